# Optimizing a Trainium2 kernel written in Bass

```python
import jax, jax.numpy as jnp
from jax import lax
import numpy as np

D_MODEL = 2048
BATCH = 1
SEQ = 16384
DEPTH = 4

HEAD_DIM = 128
N_ATTN_HEADS = 8
ATTN_WIDTH = N_ATTN_HEADS * HEAD_DIM
N_CONV_GROUPS = 8
CONV_CH = N_CONV_GROUPS * HEAD_DIM
MIX_WIDTH = ATTN_WIDTH + CONV_CH
IN_COLS = 3 * ATTN_WIDTH + 2 * CONV_CH
CONV_K = 31
MOBA_BLOCK = 256
MOBA_TOPK = 3
Q_CHUNK = 128
D_FF = -(-8 * D_MODEL // (3 * 256)) * 256
N_MOD = 6
RMS_EPS = 1e-6
LN_EPS = 1e-5
NEG_INF = -1e30

kernel_name = 'hybrid_conv_moba_block'


def rms_norm(x, g):
    x32 = x.astype(jnp.float32)
    y = x32 * lax.rsqrt(jnp.mean(x32 * x32, axis=-1, keepdims=True) + RMS_EPS)
    return (y * g.astype(jnp.float32)).astype(x.dtype)


def layer_norm(x, g, b):
    x32 = x.astype(jnp.float32)
    mu = jnp.mean(x32, axis=-1, keepdims=True)
    var = jnp.mean(jnp.square(x32 - mu), axis=-1, keepdims=True)
    y = (x32 - mu) * lax.rsqrt(var + LN_EPS) * g.astype(jnp.float32) + b.astype(jnp.float32)
    return y.astype(x.dtype)


def causal_depthwise_conv(u, w, b):
    k = w.shape[0]
    y = lax.conv_general_dilated(
        u, w[:, None, :].astype(u.dtype), window_strides=(1,),
        padding=((k - 1, 0),), dimension_numbers=('NWC', 'WIO', 'NWC'),
        feature_group_count=u.shape[-1])
    return y + b.astype(u.dtype)


def moba_attention(q, k, v):
    B, H, S, Dh = q.shape
    nb = -(-S // MOBA_BLOCK)
    n_sel = min(MOBA_TOPK, nb)
    pad = nb * MOBA_BLOCK - S
    kb = jnp.pad(k, ((0, 0), (0, 0), (0, pad), (0, 0))).reshape(B, H, nb, MOBA_BLOCK, Dh)
    vb = jnp.pad(v, ((0, 0), (0, 0), (0, pad), (0, 0))).reshape(B, H, nb, MOBA_BLOCK, Dh)
    k_mean = jnp.mean(kb.astype(jnp.float32), axis=3)
    n_chunks = S // Q_CHUNK
    q_chunks = q.reshape(B, H, n_chunks, Q_CHUNK, Dh).transpose(2, 0, 1, 3, 4)
    scale = HEAD_DIM ** -0.5
    b_idx = jnp.arange(B)[:, None, None]
    h_idx = jnp.arange(H)[None, :, None]
    blk_ids = jnp.arange(nb)

    def one_chunk(args):
        qc, ci = args
        q_pos = ci * Q_CHUNK + jnp.arange(Q_CHUNK)
        blk = (ci * Q_CHUNK) // MOBA_BLOCK
        gate = jnp.einsum('bhqd,bhnd->bhqn', qc.astype(jnp.float32), k_mean)
        gate = jnp.where(blk_ids < blk, gate, NEG_INF)
        _, sel = lax.top_k(gate, n_sel)
        k_own = lax.dynamic_index_in_dim(kb, blk, axis=2, keepdims=False)
        v_own = lax.dynamic_index_in_dim(vb, blk, axis=2, keepdims=False)
        k_pos = blk * MOBA_BLOCK + jnp.arange(MOBA_BLOCK)
        s_own = jnp.einsum('bhqd,bhkd->bhqk', qc, k_own,
                           preferred_element_type=jnp.float32) * scale
        s_own = jnp.where(k_pos[None, :] <= q_pos[:, None], s_own, NEG_INF)
        scores = [s_own]
        for j in range(n_sel):
            k_sel = kb[b_idx, h_idx, sel[..., j]]
            s_j = jnp.einsum('bhqd,bhqkd->bhqk', qc, k_sel,
                             preferred_element_type=jnp.float32) * scale
            scores.append(jnp.where(j < blk, s_j, NEG_INF))
        p = jax.nn.softmax(jnp.concatenate(scores, axis=-1), axis=-1).astype(v.dtype)
        p = p.reshape(B, H, Q_CHUNK, n_sel + 1, MOBA_BLOCK)
        out = jnp.einsum('bhqk,bhkd->bhqd', p[..., 0, :], v_own,
                         preferred_element_type=jnp.float32)
        for j in range(n_sel):
            v_sel = vb[b_idx, h_idx, sel[..., j]]
            out = out + jnp.einsum('bhqk,bhqkd->bhqd', p[..., j + 1, :], v_sel,
                                   preferred_element_type=jnp.float32)
        return out.astype(q.dtype)

    out = lax.map(one_chunk, (q_chunks, jnp.arange(n_chunks)))
    return out.transpose(1, 2, 0, 3, 4).reshape(B, H, S, Dh)


def hybrid_mixer(h, w_in, conv_w, conv_b, conv_ln_g, conv_ln_b, w_out):
    B, S, _ = h.shape
    proj = h @ w_in
    q, k, v, a, g = jnp.split(
        proj, [ATTN_WIDTH, 2 * ATTN_WIDTH, 3 * ATTN_WIDTH, 3 * ATTN_WIDTH + CONV_CH], axis=-1)

    def heads(t):
        return t.reshape(B, S, N_ATTN_HEADS, HEAD_DIM).transpose(0, 2, 1, 3)

    attn = moba_attention(heads(q), heads(k), heads(v))
    attn = attn.transpose(0, 2, 1, 3).reshape(B, S, ATTN_WIDTH)
    u = a * jax.nn.sigmoid(g)
    u = causal_depthwise_conv(u, conv_w, conv_b)
    u = jax.nn.silu(layer_norm(u, conv_ln_g, conv_ln_b))
    mix = jnp.concatenate([attn, u], axis=-1)
    return mix @ w_out


def swiglu_ffn(h, w_gate, w_up, w_down):
    return (jax.nn.silu(h @ w_gate) * (h @ w_up)) @ w_down


def setup_inputs(seed: int = 0) -> dict:
    key = jax.random.key(seed)
    ks = jax.random.split(key, 18)

    def nrm(k, shape, s):
        return jax.random.normal(k, shape, jnp.float32) * s

    L, D = DEPTH, D_MODEL
    return {
        'x': nrm(ks[0], (BATCH, SEQ, D), 1.0),
        'c': nrm(ks[1], (BATCH, D), 1.0),
        'ada_w': nrm(ks[2], (L, D, N_MOD * D), 0.5 * D ** -0.5),
        'ada_b': nrm(ks[3], (L, N_MOD * D), 0.02),
        'mix_pre_g': 1.0 + nrm(ks[4], (L, D), 0.02),
        'mix_post_g': 1.0 + nrm(ks[5], (L, D), 0.02),
        'w_in': nrm(ks[6], (L, D, IN_COLS), D ** -0.5),
        'conv_w': nrm(ks[7], (L, CONV_K, CONV_CH), CONV_K ** -0.5),
        'conv_b': nrm(ks[8], (L, CONV_CH), 0.02),
        'conv_ln_g': 1.0 + nrm(ks[9], (L, CONV_CH), 0.02),
        'conv_ln_b': nrm(ks[10], (L, CONV_CH), 0.02),
        'w_out': nrm(ks[11], (L, MIX_WIDTH, D), MIX_WIDTH ** -0.5),
        'ffn_pre_g': 1.0 + nrm(ks[12], (L, D), 0.02),
        'ffn_post_g': 1.0 + nrm(ks[13], (L, D), 0.02),
        'w_gate': nrm(ks[14], (L, D, D_FF), D ** -0.5),
        'w_up': nrm(ks[15], (L, D, D_FF), D ** -0.5),
        'w_down': nrm(ks[16], (L, D_FF, D), D_FF ** -0.5),
    }


def reference(x, c, ada_w, ada_b, mix_pre_g, mix_post_g, w_in, conv_w, conv_b,
              conv_ln_g, conv_ln_b, w_out, ffn_pre_g, ffn_post_g, w_gate, w_up, w_down):
    c_act = jax.nn.silu(c)
    for l in range(DEPTH):
        mod = c_act @ ada_w[l] + ada_b[l]
        sh1, sc1, g1, sh2, sc2, g2 = jnp.split(mod, N_MOD, axis=-1)
        h = rms_norm(x, mix_pre_g[l]) * (1.0 + sc1[:, None, :]) + sh1[:, None, :]
        y = hybrid_mixer(h, w_in[l], conv_w[l], conv_b[l], conv_ln_g[l], conv_ln_b[l], w_out[l])
        x = x + g1[:, None, :] * rms_norm(y, mix_post_g[l])
        h = rms_norm(x, ffn_pre_g[l]) * (1.0 + sc2[:, None, :]) + sh2[:, None, :]
        y = swiglu_ffn(h, w_gate[l], w_up[l], w_down[l])
        x = x + g2[:, None, :] * rms_norm(y, ffn_post_g[l])
    return x
```

```python
import os
import numpy as np
from contextlib import ExitStack
import concourse.bass as bass
import concourse.mybir as mybir
from concourse.bass_utils import run_bass_kernel_spmd

F32 = mybir.dt.float32
BF16 = mybir.dt.bfloat16
AF = mybir.ActivationFunctionType
ALU = mybir.AluOpType
AX = mybir.AxisListType

NCORES = 8
D = 2048
SEQ = 16384
L_FULL = 4
TOK = SEQ // NCORES
NT = 4
DC = D // 128
DFF = 5632
FC = DFF // 128
INC = 5120
CONVK = 31
HALO = 30
NH = 8
RMS_EPS = 1e-6
LN_EPS = 1e-5
SCALE = 128 ** -0.5
MASKV = 30000.0
NEG = -1e30


class Tok:
    __slots__ = ("name", "w", "r", "chan")

    def __init__(self, name):
        self.name = name
        self.w = {}
        self.r = {}
        self.chan = None


class Chan:
    __slots__ = ("sem", "val")

    def __init__(self, sem):
        self.sem = sem
        self.val = 0


class Eng:
    def __init__(self, name, q, sem):
        self.name = name
        self.q = q
        self.sem = sem
        self.cnt = 0
        self.seen = {}
        self.pr = []
        self.pw = []


class _DummyIns:
    def then_inc(self, *a, **k):
        return self


class _DummyEng:
    def __getattr__(self, name):
        def f(*a, **k):
            return _DummyIns()
        return f


class Sched:
    def __init__(self, nc, es, sems):
        self.nc = nc
        self.es = es
        self.engs = {}
        self.chans = []
        self.allchans = []
        self.nsem = 0
        self.sems = sems

    def new_sem(self, name):
        i = self.nsem
        self.nsem += 1
        if i >= len(self.sems):
            self.sems.append(self.es.enter_context(self.nc.semaphore(name)))
        return self.sems[i]

    def add_engine(self, name, q):
        e = Eng(name, q, self.new_sem("prog_" + name))
        self.engs[name] = e
        return e

    def chan(self, name, barrier=True):
        c = Chan(self.new_sem("ch_" + name))
        self.allchans.append(c)
        if barrier:
            self.chans.append(c)
        return c

    def tok(self, name, chan=False):
        t = Tok(name)
        if chan:
            t.chan = self.chan(name)
        return t

    @staticmethod
    def _merge(d, ev):
        s, v = ev
        k = id(s)
        if k not in d or d[k][1] < v:
            d[k] = (s, v)

    def _wait(self, e, ev):
        s, v = ev
        if s is e.sem and e.name in ("pe", "sp"):
            return
        k = id(s)
        if e.seen.get(k, 0) >= v:
            return
        e.q.wait_ge(s, v)
        e.seen[k] = v

    def _deps(self, e, reads, writes):
        for t in reads:
            for ev in t.w.values():
                self._wait(e, ev)
        for t in writes:
            for ev in t.w.values():
                self._wait(e, ev)
            for ev in t.r.values():
                self._wait(e, ev)

    def _commit(self, ev, reads, writes):
        for t in reads:
            self._merge(t.r, ev)
        for t in writes:
            t.w = {}
            t.r = {}
            self._merge(t.w, ev)

    def op(self, ename, fn, r=(), w=(), inc=True):
        e = self.engs[ename]
        self._deps(e, r, w)
        ins = fn(e.q)
        if inc:
            e.cnt += 1
            ins.then_inc(e.sem, 1)
            ev = (e.sem, e.cnt)
            self._commit(ev, list(r) + e.pr, list(w) + e.pw)
            e.pr = []
            e.pw = []
        else:
            e.pr += list(r)
            e.pw += list(w)
        return ins

    def dma(self, ename, out, in_, r=(), w=(), chan=None):
        e = self.engs[ename]
        self._deps(e, r, w)
        chan.val += 16
        e.q.dma_start(out=out, in_=in_).then_inc(chan.sem, 16)
        ev = (chan.sem, chan.val)
        self._commit(ev, r, w)
        return ev

    def collective(self, ename, kind, ins, outs, r=(), w=(), chan=None):
        e = self.engs[ename]
        self._deps(e, r, w)
        chan.val += 1
        e.q.collective_compute(kind, ALU.bypass, replica_groups=[list(range(NCORES))],
                               ins=ins, outs=outs).then_inc(chan.sem, 1)
        ev = (chan.sem, chan.val)
        self._commit(ev, r, w)
        return ev

    def barrier(self, full=False):
        evs = [(e.sem, e.cnt) for e in self.engs.values() if e.cnt > 0]
        evs += [(c.sem, c.val) for c in (self.allchans if full else self.chans) if c.val > 0]
        for e in self.engs.values():
            for ev in evs:
                self._wait(e, ev)


class _Stop(Exception):
    pass


def build_program(kind, debug=False, stop=None):
    nc = bass.Bass("TRN2", target_bir_lowering=False)
    L = 1

    def din(name, shape, dt=F32):
        return nc.dram_tensor(name, list(shape), dt, kind="ExternalInput")

    def dout(name, shape, dt=F32):
        return nc.dram_tensor(name, list(shape), dt, kind="ExternalOutput")

    def dsc(name, shape, dt):
        return nc.dram_tensor(name, list(shape), dt)

    LM = L_FULL
    if kind == "F":
        L = L_FULL
        xT_in = din("xT_in", [DC, 128, TOK])
        cT_in = din("cT", [128, DC])
        adaw_in = din("adaw", [LM, D, 1536])
        adab_in = din("adab", [128, LM * 12])
        vecs_in = din("vecs", [L, 128, 4 * DC])
        cvec_in = din("cvec", [L, 128, 8 * 34])
        w_in_d = din("w_in", [L, D, INC])
        w_out_d = din("w_out", [L, D, D])
        w_gate_d = din("w_gate", [L, D, DFF])
        w_up_d = din("w_up", [L, D, DFF])
        w_down_d = din("w_down", [L, DFF, D])
        ident_in = din("ident", [128, 128])
        emat_in = din("emat", [64, 64 * 128])
        cmask_in = din("cmask", [128, 4 * 512])
        gmask_in = din("gmask", [128, NT * 256])
        hsel_in = din("hsel", [128, 8])
        outT = dout("outT", [DC, 128, TOK])
        xT_s = dsc("xT_s", [DC, 128, TOK], F32)
        qT_s = dsc("qT_s", [NH, 128, TOK], BF16)
        kT_loc = dsc("kT_loc", [NH * 128, TOK], BF16)
        kT_all = dsc("kT_all", [NCORES * NH * 128, TOK], BF16)
        V_loc = dsc("V_loc", [NH * TOK, 128], BF16)
        V_all = dsc("V_all", [NCORES * NH * TOK, 128], BF16)
        sm_loc = dsc("sm_loc", [128, 1024], BF16)
        sm_all = dsc("sm_all", [NCORES * 128, 1024], BF16)
        mod_loc = dsc("mod_loc", [128, 512], F32)
        mod_all = dsc("mod_all", [NCORES * 128, 512], F32)
        uT_s = dsc("uT_s", [8, 128, TOK], BF16)
        h2T_s = dsc("h2T_s", [DC, 128, TOK], BF16)
        actT_s = dsc("actT_s", [FC, 128, TOK], BF16)
        wb_in = [dsc(f"wb_in{l}", [D, INC], BF16) for l in range(L)]
        wb_out = [dsc(f"wb_out{l}", [D, D], BF16) for l in range(L)]
        wb_gate = [dsc(f"wb_gate{l}", [D, DFF], BF16) for l in range(L)]
        wb_up = [dsc(f"wb_up{l}", [D, DFF], BF16) for l in range(L)]
        wb_down = [dsc(f"wb_down{l}", [DFF, D], BF16) for l in range(L)]
    elif kind == "M":
        cT_in = din("cT", [128, DC])
        adaw_in = din("adaw", [LM, D, 1536])
        adab_in = din("adab", [128, LM * 12])
        mod_out = dout("mod_out", [128, LM * 12])
    else:
        xT_in = din("xT_in", [DC, 128, TOK])
        modT_in = din("modT", [128, NCORES * 12])
        vecs_in = din("vecs", [128, 4 * DC])
    if kind == "A":
        w_in_d = din("w_in", [D, INC])
        qT_s = dout("qT_s", [NH, 128, TOK], BF16)
        kT_loc = dout("kT_loc", [NH * 128, TOK], BF16)
        V_loc = dout("V_loc", [NH * TOK, 128], BF16)
        sm_loc = dout("sm_loc", [128, 1024], BF16)
        uT_s = dout("uT_s", [8, 128, TOK], BF16)
        wb_in = [dsc("wb_in0", [D, INC], BF16)]
    if kind == "B":
        cvec_in = din("cvec", [128, 8 * 34])
        w_out_d = din("w_out", [D, D])
        w_gate_d = din("w_gate", [D, DFF])
        w_up_d = din("w_up", [D, DFF])
        w_down_d = din("w_down", [DFF, D])
        ident_in = din("ident", [128, 128])
        emat_in = din("emat", [64, 64 * 128])
        cmask_in = din("cmask", [128, 4 * 512])
        gmask_in = din("gmask", [128, NT * 256])
        hsel_in = din("hsel", [128, 8])
        qT_s = din("qT_s", [NH, 128, TOK], BF16)
        kT_loc = din("kT_loc", [NH * 128, TOK], BF16)
        V_loc = din("V_loc", [NH * TOK, 128], BF16)
        kT_all = din("kT_all", [NCORES * NH * 128, TOK], BF16)
        V_all = din("V_all", [NCORES * NH * TOK, 128], BF16)
        sm_all = din("sm_all", [NCORES * 128, 1024], BF16)
        uT_s = din("uT_s", [8, 128, TOK], BF16)
        outT = dout("outT", [DC, 128, TOK])
        xT_s = dsc("xT_s", [DC, 128, TOK], F32)
        h2T_s = dsc("h2T_s", [DC, 128, TOK], BF16)
        actT_s = dsc("actT_s", [FC, 128, TOK], BF16)
        wb_out = [dsc("wb_out0", [D, D], BF16)]
        wb_gate = [dsc("wb_gate0", [D, DFF], BF16)]
        wb_up = [dsc("wb_up0", [D, DFF], BF16)]
        wb_down = [dsc("wb_down0", [DFF, D], BF16)]
    dbg = {}
    if debug and kind == "B":
        dbg["mixT"] = dout("dbg_mixT", [DC, 128, TOK], BF16)
    if debug and kind == "A":
        dbg["hT"] = dout("dbg_hT", [DC, 128, TOK], BF16)

    with ExitStack() as es:
        def sb(name, shape, dt):
            return es.enter_context(nc.sbuf_tensor("s_" + name, list(shape), dt))

        def pst(name, shape, dt):
            return es.enter_context(nc.psum_tensor("p_" + name, list(shape), dt))

        bufA = sb("bufA", [128, DC, TOK], BF16)
        bufB0 = sb("bufB0", [128, DC, 512], F32)
        bufB1 = sb("bufB1", [128, DC, 512], F32)
        wring = [sb(f"wring{i}", [128, 8192], BF16) for i in range(3)]
        b0b = bufB0[:].rearrange("p j n -> p (j n)").bitcast(BF16)
        b1b = bufB1[:].rearrange("p j n -> p (j n)").bitcast(BF16)
        b0f = bufB0[:].rearrange("p j n -> p (j n)")
        b1f = bufB1[:].rearrange("p j n -> p (j n)")
        stage = [b0b[:, i * TOK:(i + 1) * TOK] for i in range(3)]
        emat = b0b[0:64, 0:8192].rearrange("p (a b) -> p a b", a=64)
        cmask = b0b[:, 8192:10240].rearrange("p (a b) -> p a b", a=4)
        gmask = b0f[:, 5120:6144].rearrange("p (a b) -> p a b", a=NT)
        qTh = [b0b[:, 12288 + i * TOK:12288 + (i + 1) * TOK] for i in range(2)]
        pT = [b1b[:, i * 512:(i + 1) * 512] for i in range(4)]
        nmT = [b1b[0:64, 2048 + i * 512:2048 + (i + 1) * 512] for i in range(2)]
        kown = b1b[:, 3072:3584]
        vown = b1b[:, 3584:4096].rearrange("p (k d) -> p k d", k=4)
        gm = b1f[:, 2048:2304].rearrange("p (a b) -> p a b", a=4)
        max8 = b1f[:, 2304:2336].rearrange("p (a b) -> p a b", a=4)
        thr = b1f[:, 2336:2340]
        nm = b1b[:, 4736:4992].rearrange("p (a b) -> p a b", a=4)
        kmh = b1b[:, 4992:5056]
        hall = wring[0][:, 0:NCORES * 960].rearrange("p (r n) -> p r n", r=NCORES)
        dwm = wring[1][:, 0:CONVK * 128].rearrange("p (j n) -> p j n", j=CONVK)
        upad = [wring[1][:, 4096 + i * 544:4096 + i * 544 + HALO + 512] for i in range(2)]
        halo_sel = wring[1][:, 5632:5632 + NT * 240].rearrange("p (t n) -> p t n", t=NT)
        sqr = [sb(f"sqr{i}", [128, 512], F32) for i in range(3)]
        rstd_t = sb("rstd", [128, 512], F32)
        mean_t = sb("mean", [128, 512], F32)
        tmp_t = [sb(f"tmpf{i}", [128, 512], F32) for i in range(2)]
        ident_f = sb("ident_f", [128, 128], F32)
        ident_b = sb("ident_b", [128, 128], BF16)
        ones_f = sb("ones_f", [128, 128], F32)
        ones_b = sb("ones_b", [128, 128], BF16)
        hsel = sb("hsel", [128, 8], F32)
        epsr = sb("epsr", [128, 1], F32)
        epsl = sb("epsl", [128, 1], F32)
        cT = sb("cT", [128, DC], F32)
        cact = sb("cact", [128, DC], F32)
        adab = sb("adab", [128, LM * 12], F32)
        modp = sb("modp", [128, LM * 12], F32)
        modT = sb("modT", [128, NCORES, LM * 12 if kind == "F" else 12], F32)
        vecs = sb("vecs", [128, 1, 4, DC], F32)
        cvec = sb("cvec", [128, 1, 8, 34], F32)
        gsc1 = sb("gsc1", [128, DC], F32)
        gsc2 = sb("gsc2", [128, DC], F32)
        gp1 = sb("gp1", [128, DC], F32)
        gp2 = sb("gp2", [128, DC], F32)
        sh1 = sb("sh1", [128, DC], F32)
        sh2 = sb("sh2", [128, DC], F32)
        kms = sb("kms", [128, NH, 8], F32)
        smst = sb("smst", [128, 1024], BF16)
        kmT = sb("kmT", [128, NCORES, 64], BF16)

        ps = [pst(f"ps{i}", [128, 512], F32) for i in range(7)]
        psb = pst("psb", [64, 512], BF16)

        sems = []
        block = es.enter_context(nc.Block())

        def program(S, pe, act, dve, pool, sp):
            S.add_engine("pe", pe)
            S.add_engine("act", act)
            S.add_engine("dve", dve)
            S.add_engine("pool", pool)
            S.add_engine("sp", sp)

            T = {}

            def tk(name, chan=False):
                if name not in T:
                    T[name] = S.tok(name, chan)
                return T[name]

            tA = tk("bufA", True)
            tB0 = tk("bufB0", True)
            tB1 = tk("bufB1", True)
            tW = [tk(f"wring{i}", True) for i in range(3)]
            tSt = [tk(f"stage{i}", True) for i in range(3)]
            tSq = [tk(f"sqr{i}") for i in range(3)]
            tPs = [tk(f"ps{i}") for i in range(7)]
            tPsb = tk("psb")
            tRstd = tk("rstd")
            tMean = tk("mean")
            tTmp = [tk("tmpf0"), tk("tmpf1")]
            tConst = tk("const", True)
            tMisc = tk("misc", True)
            tSm = tk("smst", True)
            tHall = tk("hall", True)
            tUp = [tk("upad0", True), tk("upad1", True)]
            tDw = tk("dwm")
            tNmT = [tk("nmT0"), tk("nmT1")]
            tPT = [tk(f"pT{i}") for i in range(4)]
            tQ = [tk("qTh0", True), tk("qTh1", True)]
            tOwn = tk("own", True)
            tGate = tk("gate")
            tKms = tk("kms")
            tAtc = tk("attc", True)
            dX = tk("d_xT")
            dQ = tk("d_qT")
            dKl = tk("d_kTloc")
            dKa = tk("d_kTall")
            dVl = tk("d_Vloc")
            dVa = tk("d_Vall")
            dSl = tk("d_smloc")
            dSa = tk("d_small")
            dU = tk("d_uT")
            dH2 = tk("d_h2T")
            dAct = tk("d_actT")
            dWb = [tk(f"d_wb{l}") for l in range(L)]
            dMl = tk("d_modloc")
            dMa = tk("d_modall")
            dOut = tk("d_out")
            dDbg = tk("d_dbg")
            chCast = S.chan("cast", barrier=False)
            chCC = S.chan("cc")

            def cast_weights(l):
                if kind == "A":
                    lst = ((w_in_d, wb_in, D),)
                elif kind == "B":
                    lst = ((w_out_d, wb_out, D), (w_gate_d, wb_gate, D), (w_up_d, wb_up, D), (w_down_d, wb_down, DFF))
                else:
                    lst = ((w_in_d, wb_in, D), (w_out_d, wb_out, D), (w_gate_d, wb_gate, D), (w_up_d, wb_up, D), (w_down_d, wb_down, DFF))
                for (src, dst, rows) in lst:
                    nsp = 8
                    rs = rows // nsp
                    sap = src.ap()[l] if kind == "F" else src.ap()
                    for i in range(nsp):
                        S.dma("pool", dst[l].ap()[i * rs:(i + 1) * rs, :], sap[i * rs:(i + 1) * rs, :],
                              r=(), w=(dWb[l],), chan=chCast)

            wslot = [0]

            def load_wblock(wdram, c0, ncols, kch, l):
                i = wslot[0] % 3
                wslot[0] += 1
                view = wring[i][:, 0:kch * ncols].rearrange("p (k n) -> p k n", k=kch)
                S.dma("sp", view, wdram.ap()[:, c0:c0 + ncols].rearrange("(k p) n -> p k n", p=128),
                      r=(dWb[l],), w=(tW[i],), chan=tW[i].chan)
                return view, tW[i]

            psrot = [0]

            def next_ps(lo=0, hi=7):
                i = lo + psrot[0] % (hi - lo)
                psrot[0] += 1
                return ps[i], tPs[i]

            sqrot = [0]
            strot = [0]

            def next_stage():
                i = strot[0] % 3
                strot[0] += 1
                return stage[i], tSt[i]

            evrot = [0]

            def evac_copy(out, in_, r, w):
                evrot[0] += 1
                if evrot[0] % 2:
                    S.op("act", lambda q: q.activation(out=out, in_=in_, func=AF.Identity), r=r, w=w)
                else:
                    S.op("dve", lambda q: q.tensor_copy(out=out, in_=in_), r=r, w=w)

            def sumsq_accum(src_ap, src_tok, first, last, acc_ps, acc_tok):
                i = sqrot[0] % 3
                sqrot[0] += 1
                S.op("act", lambda q: q.activation(out=sqr[i][:], in_=src_ap, func=AF.Square), r=(src_tok,), w=(tSq[i],))
                S.op("pe", lambda q: q.matmul(acc_ps[:], lhsT=ones_f[:], rhs=sqr[i][:], start=first, stop=last),
                     r=(tSq[i], tConst), w=(acc_tok,), inc=True)

            def rstd_from(acc_ps, acc_tok, n, eps):
                ept = epsr if eps == RMS_EPS else epsl
                S.op("act", lambda q: q.activation(out=rstd_t[:], in_=acc_ps[:], func=AF.Sqrt, bias=ept[:, 0:1], scale=1.0 / n),
                     r=(acc_tok, tConst), w=(tRstd,))
                S.op("dve", lambda q: q.reciprocal(out=rstd_t[:], in_=rstd_t[:]), r=(tRstd,), w=(tRstd,))

            def prenorm(xt, xtok, gsc, shv, dst_fn, dst_tok):
                acc, acct = ps[6], tPs[6]
                for j in range(DC):
                    sumsq_accum(xt[:, j, :], xtok, j == 0, j == DC - 1, acc, acct)
                rstd_from(acc, acct, D, RMS_EPS)
                S.op("dve", lambda q: q.tensor_tensor(out=xt, in0=xt, in1=rstd_t[:].unsqueeze(1).to_broadcast([128, DC, 512]),
                                                       op=ALU.mult), r=(xtok, tRstd), w=(xtok,))
                for j in range(DC):
                    S.op("act", lambda q, j=j: q.activation(out=dst_fn(j), in_=xt[:, j, :], func=AF.Identity,
                                                             bias=shv[:, j:j + 1], scale=gsc[:, j:j + 1]),
                         r=(xtok, tMisc), w=(dst_tok,))

            def post_residual(yt, ytok, xt, xtok, gp):
                rstd_from(ps[6], tPs[6], D, RMS_EPS)
                for j in range(DC):
                    S.op("dve", lambda q, j=j: q.scalar_tensor_tensor(out=yt[:, j, :], in0=yt[:, j, :], scalar=gp[:, j:j + 1],
                                                                      in1=rstd_t[:], op0=ALU.mult, op1=ALU.mult),
                         r=(ytok, tRstd, tMisc), w=(ytok,))
                S.op("dve", lambda q: q.tensor_tensor(out=xt, in0=xt, in1=yt, op=ALU.add), r=(xtok, ytok), w=(xtok,))

            cc = tConst.chan
            S.op("dve", lambda q: q.memset(ones_f[:], 1.0), w=(tConst,))
            S.op("dve", lambda q: q.memset(ones_b[:], 1.0), w=(tConst,))
            S.op("dve", lambda q: q.memset(epsr[:], RMS_EPS), w=(tConst,))
            S.op("dve", lambda q: q.memset(epsl[:], LN_EPS), w=(tConst,))
            if kind == "F":
                cast_weights(0)
            if kind in ("M", "F"):
                S.dma("sp", cT[:], cT_in.ap(), w=(tConst,), chan=cc)
                S.dma("sp", adab[:], adab_in.ap(), w=(tConst,), chan=cc)
                S.op("act", lambda q: q.activation(out=cact[:], in_=cT[:], func=AF.Silu), r=(tConst,), w=(tMisc,))
                for l in range(LM):
                    for half in range(3):
                        buf, bt = (bufB0, tB0) if (l * 3 + half) % 2 == 0 else (bufB1, tB1)
                        S.dma("sp", buf[:], adaw_in.ap()[l, :, half * 512:(half + 1) * 512].rearrange("(k p) n -> p k n", p=128),
                              w=(bt,), chan=bt.chan)
                        for mm in range(4):
                            col = l * 12 + half * 4 + mm
                            for k in range(DC):
                                S.op("pe", lambda q, k=k, mm=mm, col=col, buf=buf: q.matmul(
                                    ps[0][:, col:col + 1], lhsT=buf[:, k, mm * 128:(mm + 1) * 128], rhs=cact[:, k:k + 1],
                                    start=(k == 0), stop=(k == DC - 1)), r=(bt, tMisc), w=(tPs[0],), inc=(k == DC - 1))
                S.op("dve", lambda q: q.tensor_tensor(out=modp[:], in0=ps[0][:, 0:LM * 12], in1=adab[:], op=ALU.add),
                     r=(tPs[0], tConst), w=(tMisc,))
                if kind == "M":
                    S.dma("sp", mod_out.ap(), modp[:], r=(tMisc,), w=(dOut,), chan=tMisc.chan)
                    S.barrier(full=True)
                    return
                S.dma("sp", mod_loc.ap()[:, 0:LM * 12], modp[:], r=(tMisc,), w=(dMl,), chan=tMisc.chan)
                S.collective("pool", "AllGather", [mod_loc.ap()], [mod_all.ap()], r=(dMl,), w=(dMa,), chan=chCC)
                S.dma("sp", modT[:], mod_all.ap()[:, 0:LM * 12].rearrange("(r p) n -> p r n", p=128), r=(dMa,), w=(tMisc,), chan=tMisc.chan)
                S.barrier()
            xsrc = [xT_in]
            if kind != "F":
                S.dma("sp", modT[:].rearrange("p r n -> p (r n)"), modT_in.ap(), w=(tMisc,), chan=tMisc.chan)
                S.dma("sp", vecs[:].rearrange("p a b c -> p (a b c)"), vecs_in.ap(), w=(tConst,), chan=cc)
                cast_weights(0)
            if kind in ("B", "F"):
                S.dma("sp", ident_f[:], ident_in.ap(), w=(tConst,), chan=cc)
                S.dma("pool", ident_b[:], ident_in.ap(), w=(tConst,), chan=cc)
                S.dma("sp", hsel[:], hsel_in.ap(), w=(tConst,), chan=cc)
                if kind == "B":
                    S.dma("sp", cvec[:].rearrange("p a b c -> p (a b c)"), cvec_in.ap(), w=(tConst,), chan=cc)

            def ckpt(k):
                if stop == k:
                    S.barrier(full=True)
                    raise _Stop()

            ckpt(0)

            def mod_chunk(l, m):
                r_, mm = divmod(m, 12)
                if kind == "F":
                    return modT[:, r_, l * 12 + mm:l * 12 + mm + 1]
                return modT[:, r_, mm:mm + 1]

            def layer_vectors(l):
                for j in range(DC):
                    for (dst, gidx, scm, shdst, shm, gpd, gm_, pidx) in (
                            (gsc1, 0, 16 + j, sh1, 0 + j, gp1, 32 + j, 1),
                            (gsc2, 2, 64 + j, sh2, 48 + j, gp2, 80 + j, 3)):
                        S.op("dve", lambda q, dst=dst, gidx=gidx, scm=scm: q.scalar_tensor_tensor(
                            out=dst[:, j:j + 1], in0=mod_chunk(l, scm), scalar=1.0, in1=vecs[:, 0, gidx, j:j + 1],
                            op0=ALU.add, op1=ALU.mult), r=(tMisc, tConst), w=(tMisc,))
                        S.op("dve", lambda q, shdst=shdst, shm=shm: q.tensor_copy(out=shdst[:, j:j + 1], in_=mod_chunk(l, shm)),
                             r=(tMisc,), w=(tMisc,))
                        S.op("dve", lambda q, gpd=gpd, gm_=gm_, pidx=pidx: q.tensor_tensor(
                            out=gpd[:, j:j + 1], in0=mod_chunk(l, gm_), in1=vecs[:, 0, pidx, j:j + 1], op=ALU.mult),
                            r=(tMisc, tConst), w=(tMisc,))

            def proj_block(wv, wtok, jj, t, pbank, ptok):
                for k in range(DC):
                    S.op("pe", lambda q, k=k: q.matmul(pbank[:], lhsT=wv[:, k, jj * 128:(jj + 1) * 128],
                                                        rhs=bufA[:, k, t * 512:(t + 1) * 512],
                                                        start=(k == 0), stop=(k == DC - 1)),
                         r=(wtok, tA), w=(ptok,), inc=(k == DC - 1))


            def ph2(l):
                S.op("dve", lambda q: q.memset(smst[:], 0.0), w=(tSm,))
                S.op("dve", lambda q: q.memset(kms[:].rearrange("p a b -> p (a b)"), 0.0), w=(tKms,))

                for blk in range(4):
                    wv, wtok = load_wblock(wb_in[l], blk * 512, 512, DC, l)
                    for jj in range(4):
                        ch = blk * 4 + jj
                        st, stt = next_stage()
                        for t in range(NT):
                            pb, pt_ = next_ps(0, 6)
                            proj_block(wv, wtok, jj, t, pb, pt_)
                            if ch >= 8:
                                h = ch - 8
                                for b2 in range(2):
                                    S.op("act", lambda q, pb=pb, st=st, t=t, b2=b2, h=h: q.activation(
                                        out=st[:, t * 512 + b2 * 256:t * 512 + (b2 + 1) * 256], in_=pb[:, b2 * 256:(b2 + 1) * 256],
                                        func=AF.Identity, accum_out=kms[:, h, 2 * t + b2:2 * t + b2 + 1]), r=(pt_,), w=(stt, tKms))
                            else:
                                S.op("act", lambda q, pb=pb, st=st, t=t: q.activation(out=st[:, t * 512:(t + 1) * 512], in_=pb[:], func=AF.Identity),
                                     r=(pt_,), w=(stt,))
                        if os.environ.get("KSKIP_ST"):
                            pass
                        elif ch < 8:
                            S.dma("pool", qT_s.ap()[ch], st, r=(stt,), w=(dQ,), chan=stt.chan)
                        else:
                            S.dma("pool", kT_loc.ap()[(ch - 8) * 128:(ch - 7) * 128, :], st, r=(stt,), w=(dKl,), chan=stt.chan)
                S.op("dve", lambda q: q.tensor_copy(out=smst[:, 0:64], in_=kms[:].rearrange("p a b -> p (a b)")),
                     r=(tKms,), w=(tSm,))
                ckpt(10)
                for bv in range(2):
                    wv, wtok = load_wblock(wb_in[l], 2048 + bv * 512, 512, DC, l)
                    for tc4 in range(4):
                        st, stt = next_stage()
                        for tcc in range(4):
                            tc = tc4 * 4 + tcc
                            pb, pt_ = next_ps(0, 6)
                            for k in range(DC):
                                S.op("pe", lambda q, k=k, tc=tc: q.matmul(pb[:], lhsT=bufA[:, k, tc * 128:(tc + 1) * 128],
                                                                           rhs=wv[:, k, :], start=(k == 0), stop=(k == DC - 1)),
                                     r=(wtok, tA), w=(pt_,), inc=(k == DC - 1))
                            evac_copy(st[:, tcc * 512:(tcc + 1) * 512], pb[:], (pt_,), (stt,))
                        for hh in range(4):
                            dst = V_loc.ap().rearrange("(h c p) d -> p c h d", h=NH, p=128)[:, tc4 * 4:(tc4 + 1) * 4, bv * 4 + hh, :]
                            S.dma("pool", dst, st.rearrange("p (c h d) -> p c h d", c=4, h=4)[:, :, hh, :], r=(stt,), w=(dVl,), chan=stt.chan)
                ckpt(11)
                for bb in range(2):
                    wa, wat = load_wblock(wb_in[l], 3072 + bb * 512, 512, DC, l)
                    wg, wgt = load_wblock(wb_in[l], 4096 + bb * 512, 512, DC, l)
                    for jj in range(4):
                        i = bb * 4 + jj
                        st, stt = next_stage()
                        for t in range(NT):
                            pg, pgt = next_ps(0, 6)
                            proj_block(wg, wgt, jj, t, pg, pgt)
                            pa, pat = next_ps(0, 6)
                            proj_block(wa, wat, jj, t, pa, pat)
                            sg, sgt = tmp_t[t % 2], tTmp[t % 2]
                            S.op("act", lambda q, pg=pg, sg=sg: q.activation(out=sg[:], in_=pg[:], func=AF.Sigmoid), r=(pgt,), w=(sgt,))
                            S.op("dve", lambda q, pa=pa, sg=sg, st=st, t=t: q.tensor_tensor(
                                out=st[:, t * 512:(t + 1) * 512], in0=pa[:], in1=sg[:], op=ALU.mult), r=(pat, sgt), w=(stt,))
                            S.op("dve", lambda q, st=st, t=t, i=i: q.tensor_copy(
                                out=smst[:, 64 + (t * 8 + i) * HALO:64 + (t * 8 + i + 1) * HALO],
                                in_=st[:, t * 512 + 512 - HALO:(t + 1) * 512]), r=(stt,), w=(tSm,))
                        S.dma("pool", uT_s.ap()[i], st, r=(stt,), w=(dU,), chan=stt.chan)
                S.dma("pool", sm_loc.ap(), smst[:], r=(tSm,), w=(dSl,), chan=tSm.chan)

            for l in range(L):
                last = (l == L - 1)
                if kind == "F":
                    S.barrier()
                    S.dma("sp", vecs[:].rearrange("p a b c -> p (a b c)"), vecs_in.ap()[l], w=(tConst,), chan=cc)
                    S.dma("sp", cvec[:].rearrange("p a b c -> p (a b c)"), cvec_in.ap()[l], w=(tConst,), chan=cc)
                layer_vectors(l)
                S.barrier()
                for t in range(NT if kind in ("A", "F") else 0):
                    buf, bt = (bufB0, tB0) if t % 2 == 0 else (bufB1, tB1)
                    S.dma("sp", buf[:], xsrc[0].ap()[:, :, t * 512:(t + 1) * 512].rearrange("j p n -> p j n"),
                          r=(dX,), w=(bt,), chan=bt.chan)
                    prenorm(buf[:], bt, gsc1, sh1, lambda j, t=t: bufA[:, j, t * 512:(t + 1) * 512], tA)
                if debug and kind == "A":
                    S.dma("sp", dbg["hT"].ap().rearrange("j p n -> p j n"), bufA[:], r=(tA,), w=(dDbg,), chan=tA.chan)
                S.barrier()
                ckpt(1)
                if kind == "A":
                    ph2(l)
                    S.barrier(full=True)
                    return
                if kind == "F":
                    ph2(l)
                    S.collective("pool", "AllGather", [kT_loc.ap()], [kT_all.ap()], r=(dKl,), w=(dKa,), chan=chCC)
                    S.collective("pool", "AllGather", [V_loc.ap().rearrange("(a b) d -> a (b d)", b=8)],
                                 [V_all.ap().rearrange("(a b) d -> a (b d)", b=8)], r=(dVl,), w=(dVa,), chan=chCC)
                    S.collective("pool", "AllGather", [sm_loc.ap()], [sm_all.ap()], r=(dSl,), w=(dSa,), chan=chCC)
                    if l + 1 < L:
                        cast_weights(l + 1)
                S.barrier()
                ckpt(3)
                S.dma("sp", hall, sm_all.ap()[:, 64:1024].rearrange("(r p) n -> p r n", p=128), r=(dSa,), w=(tHall,), chan=tHall.chan)
                S.dma("sp", kmT[:], sm_all.ap()[:, 0:64].rearrange("(r p) n -> p r n", p=128), r=(dSa,), w=(tHall,), chan=tHall.chan)
                for t in range(NT):
                    first = True
                    for r_ in range(8):
                        tp = t if r_ < 7 else t - 1
                        if tp < 0:
                            continue
                        src = hall[:, r_, tp * 240:(tp + 1) * 240]
                        if first:
                            S.op("dve", lambda q, src=src, r_=r_: q.tensor_scalar(out=halo_sel[:, t, :], in0=src, scalar1=hsel[:, r_:r_ + 1],
                                                                               scalar2=None, op0=ALU.mult), r=(tHall, tConst), w=(tMisc,))
                            first = False
                        else:
                            S.op("dve", lambda q, src=src, r_=r_: q.scalar_tensor_tensor(out=halo_sel[:, t, :], in0=src, scalar=hsel[:, r_:r_ + 1],
                                                                                      in1=halo_sel[:, t, :], op0=ALU.mult, op1=ALU.add),
                                 r=(tHall, tConst, tMisc), w=(tMisc,))
                cT_all = [bufB0, bufB1]
                ui = 0
                for i in range(8):
                    for j in range(CONVK):
                        S.op("dve", lambda q, j=j, i=i: q.tensor_scalar(out=dwm[:, j, :], in0=ident_b[:], scalar1=cvec[:, 0, i, j:j + 1],
                                                                        scalar2=None, op0=ALU.mult), r=(tConst,), w=(tDw,))
                    for t in range(NT):
                        up, upt = upad[ui % 2], tUp[ui % 2]
                        ui += 1
                        S.dma("sp", up[:, HALO:HALO + 512], uT_s.ap()[i, :, t * 512:(t + 1) * 512], r=(dU,), w=(upt,), chan=upt.chan)
                        S.op("dve", lambda q, up=up, t=t, i=i: q.tensor_copy(out=up[:, 0:HALO], in_=halo_sel[:, t, i * HALO:(i + 1) * HALO]),
                             r=(tMisc,), w=(upt,))
                        pb, pt_ = next_ps(0, 6)
                        for j in range(CONVK):
                            S.op("pe", lambda q, j=j, up=up: q.matmul(pb[:], lhsT=dwm[:, j, :], rhs=up[:, j:j + 512],
                                                                       start=(j == 0), stop=(j == CONVK - 1)),
                                 r=(tDw, upt), w=(pt_,), inc=(j == CONVK - 1))
                        cb, cbt = (bufB0, tB0) if i < 4 else (bufB1, tB1)
                        S.op("act", lambda q, pb=pb, cb=cb, i=i, t=t: q.activation(out=cb[:, (i % 4) * 4 + t, :], in_=pb[:], func=AF.Identity,
                                                                               bias=cvec[:, 0, i, 31:32], scale=1.0), r=(pt_, tConst), w=(cbt,))
                for t in range(NT):
                    for i in range(8):
                        cb, cbt = (bufB0, tB0) if i < 4 else (bufB1, tB1)
                        src = cb[:, (i % 4) * 4 + t, :]
                        S.op("pe", lambda q, src=src, i=i: q.matmul(ps[5][:], lhsT=ones_f[:], rhs=src, start=(i == 0), stop=(i == 7)),
                             r=(cbt, tConst), w=(tPs[5],), inc=(i == 7))
                        sumsq_accum(src, cbt, i == 0, i == 7, ps[6], tPs[6])
                    S.op("dve", lambda q: q.tensor_scalar(out=mean_t[:], in0=ps[5][:], scalar1=1.0 / 1024, scalar2=None, op0=ALU.mult),
                         r=(tPs[5],), w=(tMean,))
                    S.op("dve", lambda q: q.tensor_tensor(out=tmp_t[0][:], in0=mean_t[:], in1=mean_t[:], op=ALU.mult), r=(tMean,), w=(tTmp[0],))
                    S.op("dve", lambda q: q.scalar_tensor_tensor(out=rstd_t[:], in0=ps[6][:], scalar=1.0 / 1024, in1=tmp_t[0][:],
                                                                  op0=ALU.mult, op1=ALU.subtract), r=(tPs[6], tTmp[0]), w=(tRstd,))
                    S.op("act", lambda q: q.activation(out=rstd_t[:], in_=rstd_t[:], func=AF.Sqrt, bias=epsl[:, 0:1], scale=1.0),
                         r=(tRstd, tConst), w=(tRstd,))
                    S.op("dve", lambda q: q.reciprocal(out=rstd_t[:], in_=rstd_t[:]), r=(tRstd,), w=(tRstd,))
                    for i in range(8):
                        cb, cbt = (bufB0, tB0) if i < 4 else (bufB1, tB1)
                        src = cb[:, (i % 4) * 4 + t, :]
                        S.op("dve", lambda q, src=src: q.tensor_tensor(out=src, in0=src, in1=mean_t[:], op=ALU.subtract), r=(cbt, tMean), w=(cbt,))
                        S.op("dve", lambda q, src=src: q.tensor_tensor(out=src, in0=src, in1=rstd_t[:], op=ALU.mult), r=(cbt, tRstd), w=(cbt,))
                        S.op("act", lambda q, src=src, i=i, t=t: q.activation(out=bufA[:, 8 + i, t * 512:(t + 1) * 512], in_=src, func=AF.Silu,
                                                                              bias=cvec[:, 0, i, 33:34], scale=cvec[:, 0, i, 32:33]),
                             r=(cbt, tConst), w=(tA,))
                S.barrier()
                ckpt(4)
                NSL = 16
                S.dma("pool", emat.rearrange("p a b -> p (a b)"), emat_in.ap(), w=(tAtc,), chan=tAtc.chan)
                S.dma("pool", cmask.rearrange("p a b -> p (a b)"), cmask_in.ap(), w=(tAtc,), chan=tAtc.chan)
                S.dma("sp", gmask.rearrange("p a b -> p (a b)"), gmask_in.ap(), w=(tAtc,), chan=tAtc.chan)
                kring = [wring[0][:, s * 512:(s + 1) * 512] for s in range(NSL)]
                vring = [wring[1][:, s * 512:(s + 1) * 512].rearrange("p (k d) -> p k d", k=4) for s in range(NSL)]
                tKV = [tk(f"kv{s}", True) for s in range(NSL)]
                kvrot = 0
                prot = 0
                nmrot = 0
                srot = 0
                for h in range(NH):
                    qt, qtt = qTh[h % 2], tQ[h % 2]
                    S.dma("sp", qt, qT_s.ap()[h], r=(dQ,), w=(qtt,), chan=qtt.chan)
                    S.op("dve", lambda q, h=h: q.tensor_copy(out=kmh.rearrange("p (r b) -> p r b", r=8), in_=kmT[:, :, h * 8:(h + 1) * 8]),
                         r=(tHall,), w=(tGate,))
                    for t in range(NT):
                        for c_ in range(4):
                            S.op("pe", lambda q, c_=c_: q.matmul(ps[5][:, c_ * 64:(c_ + 1) * 64], lhsT=qt[:, t * 512 + c_ * 128:t * 512 + (c_ + 1) * 128],
                                                                  rhs=kmh, start=True, stop=True), r=(qtt, tGate), w=(tPs[5],), inc=(c_ == 3))
                        S.op("dve", lambda q: q.tensor_tensor(out=gm.rearrange("p a b -> p (a b)"), in0=ps[5][:, 0:256], in1=gmask[:, t, :], op=ALU.add),
                             r=(tPs[5], tAtc), w=(tGate,))
                        for c_ in range(4):
                            S.op("dve", lambda q, c_=c_: q.max(out=max8[:, c_, :], in_=gm[:, c_, :]), r=(tGate,), w=(tGate,))
                        S.op("dve", lambda q: q.tensor_scalar(out=thr, in0=max8[:, :, 2], scalar1=-1e29, scalar2=None, op0=ALU.max), r=(tGate,), w=(tGate,))
                        for c_ in range(4):
                            S.op("dve", lambda q, c_=c_: q.tensor_scalar(out=nm[:, c_, :], in0=gm[:, c_, :], scalar1=thr[:, c_:c_ + 1], scalar2=1.0,
                                                                         op0=ALU.is_ge, op1=ALU.subtract), r=(tGate,), w=(tGate,))
                        for c_ in range(4):
                            S.op("pe", lambda q, c_=c_: q.transpose(out=psb[:, c_ * 128:(c_ + 1) * 128], in_=nm[:, c_, :], identity=ident_b[:]),
                                 r=(tGate, tConst), w=(tPsb,), inc=(c_ == 3))
                        nmt, nmtt = nmT[nmrot % 2], tNmT[nmrot % 2]
                        nmrot += 1
                        S.op("act", lambda q, nmt=nmt: q.activation(out=nmt, in_=psb[:], func=AF.Identity), r=(tPsb,), w=(nmtt,))
                        visits = []
                        S.dma("sp", kown, kT_loc.ap()[h * 128:(h + 1) * 128, t * 512:(t + 1) * 512], r=(dKl,), w=(tOwn,), chan=tOwn.chan)
                        S.dma("sp", vown, V_loc.ap()[h * TOK + t * 512:h * TOK + (t + 1) * 512, :].rearrange("(k p) d -> p k d", p=128),
                              r=(dVl,), w=(tOwn,), chan=tOwn.chan)
                        for kt in range(4):
                            visits.append(("own", kown[:, kt * 128:(kt + 1) * 128], vown[:, kt, :], tOwn, ident_b[:], cmask[:, kt, :], (tConst, tAtc)))
                        for tp in range(t + 1):
                            for r_ in range(NCORES):
                                s = kvrot % NSL
                                kvrot += 1
                                visits.append(("load", s, r_, tp))
                                for kt in range(4):
                                    jblk = r_ * 8 + 2 * tp + kt // 2
                                    visits.append(("past", kring[s][:, kt * 128:(kt + 1) * 128], vring[s][:, kt, :], tKV[s],
                                                   emat[:, jblk, :], nmt, (tAtc, nmtt)))
                        comp = [v for v in visits if v[0] != "load"]
                        nvis = len(comp)
                        pend = []
                        ci = 0

                        def emit_pv(item, idx):
                            (vv, pt_i) = item
                            S.op("pe", lambda q: q.matmul(ps[3][:], lhsT=vv[2], rhs=pT[pt_i], start=(idx == 0), stop=(idx == nvis - 1)),
                                 r=(vv[3], tPT[pt_i]), w=(tPs[3],), inc=False)
                            S.op("pe", lambda q: q.matmul(ps[4][:], lhsT=ones_b[:], rhs=pT[pt_i], start=(idx == 0), stop=(idx == nvis - 1)),
                                 r=(tPT[pt_i], tConst), w=(tPs[4],), inc=True)

                        done = 0
                        for v in visits:
                            if v[0] == "load":
                                _, s, r_, tp = v
                                S.dma("sp", kring[s], kT_all.ap()[(r_ * NH + h) * 128:(r_ * NH + h + 1) * 128, tp * 512:(tp + 1) * 512],
                                      r=(dKa,), w=(tKV[s],), chan=tKV[s].chan)
                                base = (r_ * NH + h) * TOK + tp * 512
                                S.dma("sp", vring[s], V_all.ap()[base:base + 512, :].rearrange("(k p) d -> p k d", p=128),
                                      r=(dVa,), w=(tKV[s],), chan=tKV[s].chan)
                                continue
                            sb_i = srot % 3
                            srot += 1
                            pss, psst = ps[sb_i], tPs[sb_i]
                            S.op("pe", lambda q, v=v, pss=pss: q.matmul(pss[:], lhsT=v[1], rhs=qt[:, t * 512:(t + 1) * 512], start=True, stop=False),
                                 r=(v[3], qtt), w=(psst,), inc=False)
                            S.op("pe", lambda q, v=v, pss=pss: q.matmul(pss[:], lhsT=v[4], rhs=v[5], start=False, stop=True),
                                 r=v[6], w=(psst,), inc=True)
                            pi = prot % 4
                            prot += 1
                            S.op("act", lambda q, pss=pss, pi=pi: q.activation(out=pT[pi], in_=pss[:], func=AF.Exp, scale=SCALE),
                                 r=(psst,), w=(tPT[pi],))
                            pend.append((v, pi))
                            if len(pend) > 2:
                                emit_pv(pend.pop(0), done)
                                done += 1
                        while pend:
                            emit_pv(pend.pop(0), done)
                            done += 1
                        S.op("dve", lambda q: q.reciprocal(out=tmp_t[0][:], in_=ps[4][:]), r=(tPs[4],), w=(tTmp[0],))
                        S.op("dve", lambda q, h=h, t=t: q.tensor_tensor(out=bufA[:, h, t * 512:(t + 1) * 512], in0=ps[3][:], in1=tmp_t[0][:], op=ALU.mult),
                             r=(tPs[3], tTmp[0]), w=(tA,))
                if debug:
                    S.dma("sp", dbg["mixT"].ap().rearrange("j p n -> p j n"), bufA[:], r=(tA,), w=(dDbg,), chan=tA.chan)
                S.barrier()
                ckpt(5)
                for t in range(NT):
                    S.dma("sp", bufB1[:], xsrc[0].ap()[:, :, t * 512:(t + 1) * 512].rearrange("j p n -> p j n"), r=(dX,), w=(tB1,), chan=tB1.chan)
                    for blk in range(4):
                        wv, wtok = load_wblock(wb_out[l], blk * 512, 512, DC, l)
                        for jj in range(4):
                            j = blk * 4 + jj
                            pb, pt_ = next_ps(0, 6)
                            for k in range(DC):
                                S.op("pe", lambda q, k=k, jj=jj: q.matmul(pb[:], lhsT=wv[:, k, jj * 128:(jj + 1) * 128], rhs=bufA[:, k, t * 512:(t + 1) * 512],
                                                                          start=(k == 0), stop=(k == DC - 1)), r=(wtok, tA), w=(pt_,), inc=(k == DC - 1))
                            S.op("act", lambda q, pb=pb, j=j: q.activation(out=bufB0[:, j, :], in_=pb[:], func=AF.Identity), r=(pt_,), w=(tB0,))
                            sumsq_accum(bufB0[:, j, :], tB0, j == 0, j == DC - 1, ps[6], tPs[6])
                    post_residual(bufB0[:], tB0, bufB1[:], tB1, gp1)
                    S.dma("pool", xT_s.ap()[:, :, t * 512:(t + 1) * 512].rearrange("j p n -> p j n"), bufB1[:], r=(tB1,), w=(dX,), chan=tB1.chan)
                    h2v = bufB0[:].rearrange("p j n -> p (j n)").bitcast(BF16)[:, 0:DC * 512].rearrange("p (j n) -> p j n", j=DC)
                    prenorm(bufB1[:], tB1, gsc2, sh2, lambda j: h2v[:, j, :], tB0)
                    S.dma("pool", h2T_s.ap()[:, :, t * 512:(t + 1) * 512].rearrange("j p n -> p j n"), h2v, r=(tB0,), w=(dH2,), chan=tB0.chan)
                xsrc[0] = xT_s
                S.barrier()
                ckpt(6)
                S.dma("sp", bufA[:], h2T_s.ap().rearrange("j p n -> p j n"), r=(dH2,), w=(tA,), chan=tA.chan)
                for fb in range(FC // 4):
                    wg, wgt = load_wblock(wb_gate[l], fb * 512, 512, DC, l)
                    wu, wut = load_wblock(wb_up[l], fb * 512, 512, DC, l)
                    for jj in range(4):
                        f = fb * 4 + jj
                        st, stt = next_stage()
                        for t in range(NT):
                            pg, pgt = next_ps(0, 6)
                            proj_block(wg, wgt, jj, t, pg, pgt)
                            pu, put = next_ps(0, 6)
                            proj_block(wu, wut, jj, t, pu, put)
                            sg, sgt = tmp_t[t % 2], tTmp[t % 2]
                            S.op("act", lambda q, pg=pg, sg=sg: q.activation(out=sg[:], in_=pg[:], func=AF.Silu), r=(pgt,), w=(sgt,))
                            S.op("dve", lambda q, pu=pu, sg=sg, st=st, t=t: q.tensor_tensor(out=st[:, t * 512:(t + 1) * 512], in0=pu[:], in1=sg[:], op=ALU.mult),
                                 r=(put, sgt), w=(stt,))
                        S.dma("pool", actT_s.ap()[f], st, r=(stt,), w=(dAct,), chan=stt.chan)
                S.barrier()
                ckpt(7)
                actv = bufA[:].rearrange("p j n -> p (j n)")[:, 0:FC * 512].rearrange("p (f n) -> p f n", f=FC)
                for t in range(NT):
                    S.dma("sp", actv, actT_s.ap()[:, :, t * 512:(t + 1) * 512].rearrange("f p n -> p f n"), r=(dAct,), w=(tA,), chan=tA.chan)
                    S.dma("sp", bufB1[:], xT_s.ap()[:, :, t * 512:(t + 1) * 512].rearrange("j p n -> p j n"), r=(dX,), w=(tB1,), chan=tB1.chan)
                    for j in range(DC):
                        wv, wtok = load_wblock(wb_down[l], j * 128, 128, FC, l)
                        pb, pt_ = next_ps(0, 6)
                        for k in range(FC):
                            S.op("pe", lambda q, k=k: q.matmul(pb[:], lhsT=wv[:, k, :], rhs=actv[:, k, :], start=(k == 0), stop=(k == FC - 1)),
                                 r=(wtok, tA), w=(pt_,), inc=(k == FC - 1))
                        S.op("act", lambda q, pb=pb, j=j: q.activation(out=bufB0[:, j, :], in_=pb[:], func=AF.Identity), r=(pt_,), w=(tB0,))
                        sumsq_accum(bufB0[:, j, :], tB0, j == 0, j == DC - 1, ps[6], tPs[6])
                    post_residual(bufB0[:], tB0, bufB1[:], tB1, gp2)
                    dst = outT if (kind == "B" or last) else xT_s
                    S.dma("pool", dst.ap()[:, :, t * 512:(t + 1) * 512].rearrange("j p n -> p j n"), bufB1[:], r=(tB1,),
                          w=(dOut if (kind == "B" or last) else dX,), chan=tB1.chan)
                S.barrier()
            S.barrier(full=True)

        def run_pass(live, q):
            S = Sched(nc, es, sems)
            qs = {n: (q if n == live else _DummyEng()) for n in ("pe", "act", "dve", "pool", "sp")}
            try:
                program(S, qs["pe"], qs["act"], qs["dve"], qs["pool"], qs["sp"])
            except _Stop:
                pass

        @block.tensor
        def _(q):
            run_pass("pe", q)

        @block.scalar
        def _(q):
            run_pass("act", q)

        @block.vector
        def _(q):
            run_pass("dve", q)

        @block.gpsimd
        def _(q):
            run_pass("pool", q)

        @block.sync
        def _(q):
            run_pass("sp", q)
    return nc


def _consts(core):
    ident = np.eye(128, dtype=np.float32)
    emat = np.zeros((64, 64, 128), np.float32)
    for n in range(64):
        emat[n, n, :] = MASKV
    k = np.arange(128)[:, None]
    q = np.arange(128)[None, :]
    tri = np.where(k <= q, 0.0, -MASKV).astype(np.float32)
    full = np.zeros((128, 128), np.float32)
    ninf = np.full((128, 128), -MASKV, np.float32)
    cm = np.stack([
        np.concatenate([tri, full, ninf, ninf], 1),
        np.concatenate([ninf, tri, ninf, ninf], 1),
        np.concatenate([ninf, ninf, tri, full], 1),
        np.concatenate([ninf, ninf, ninf, tri], 1)], 0)
    cmask = np.ascontiguousarray(cm.transpose(1, 0, 2)).reshape(128, 4 * 512)
    jj = np.arange(64)
    r_ = jj // 8
    lb = jj % 8
    nglob = 2 * (8 * (lb // 2) + r_) + (lb % 2)
    gmask = np.zeros((NT, 4, 64), np.float32)
    for t in range(NT):
        g = 8 * t + core
        for c_ in range(4):
            blk = 2 * g + (1 if c_ >= 2 else 0)
            gmask[t, c_, :] = np.where(nglob < blk, 0.0, NEG)
    gmask = np.broadcast_to(gmask.reshape(1, NT * 256), (128, NT * 256)).copy()
    hs = np.zeros((8,), np.float32)
    if core == 0:
        hs[7] = 1.0
    else:
        hs[core - 1] = 1.0
    hsel = np.broadcast_to(hs[None, :], (128, 8)).copy()
    return ident, emat.reshape(64, 64 * 128), cmask, gmask, hsel


def _fm(v):
    sh = v.shape
    v = v.reshape(sh[:-1] + (sh[-1] // 128, 128))
    return np.ascontiguousarray(np.moveaxis(v, -1, 0))


_CACHE = {}


def _prog(kind, debug=False, stop=None):
    key = (kind, debug, stop)
    if key not in _CACHE:
        _CACHE[key] = build_program(kind, debug, stop)
    return _CACHE[key]


def _kernel_unfused(x, c, ada_w, ada_b, mix_pre_g, mix_post_g, w_in, conv_w, conv_b, conv_ln_g, conv_ln_b, w_out,
                    ffn_pre_g, ffn_post_g, w_gate, w_up, w_down, _depth=L_FULL, _debug=False, _stop=None):
    L = _depth
    cores = list(range(NCORES))
    f32 = lambda a: np.ascontiguousarray(np.asarray(a, dtype=np.float32))
    x = f32(x).reshape(SEQ, D)
    ada_w = f32(ada_w)
    ada_b = f32(ada_b)
    dbg = {}
    cT = _fm(f32(c).reshape(D))
    in_maps = []
    for core in cores:
        aw = np.ascontiguousarray(ada_w[:, :, core * 1536:(core + 1) * 1536])
        ab = ada_b[:, core * 1536:(core + 1) * 1536].reshape(L_FULL, 12, 128)
        ab = np.ascontiguousarray(ab.transpose(2, 0, 1)).reshape(128, L_FULL * 12)
        in_maps.append({"cT": cT, "adaw": aw, "adab": ab})
    res = run_bass_kernel_spmd(_prog("M"), in_maps, core_ids=cores)
    modall = np.stack([np.asarray(res.results[r]["mod_out"]).reshape(128, L_FULL, 12) for r in cores], axis=1)
    xt = x.reshape(NT, NCORES, 512, D)
    xTs = [np.ascontiguousarray(xt[:, core].reshape(TOK, D).T).reshape(DC, 128, TOK) for core in cores]
    consts = [_consts(core) for core in cores]
    for l in range(L):
        modT = np.ascontiguousarray(modall[:, :, l, :]).reshape(128, NCORES * 12)
        vecs = np.stack([_fm(f32(v)[l]) for v in (mix_pre_g, mix_post_g, ffn_pre_g, ffn_post_g)], axis=1)
        vecs = np.ascontiguousarray(vecs).reshape(128, 4 * DC)
        cv = np.concatenate([f32(conv_w)[l], f32(conv_b)[l][None], f32(conv_ln_g)[l][None], f32(conv_ln_b)[l][None]], axis=0)
        cvec = np.ascontiguousarray(cv.reshape(34, 8, 128).transpose(2, 1, 0)).reshape(128, 8 * 34)
        wi = f32(w_in[l])
        in_maps = [{"xT_in": xTs[core], "modT": modT, "vecs": vecs, "w_in": wi} for core in cores]
        resA = run_bass_kernel_spmd(_prog("A", _debug), in_maps, core_ids=cores).results
        if _debug:
            dbg[("A", l)] = resA
        kT_all = np.concatenate([np.asarray(resA[r]["kT_loc"]) for r in cores], axis=0)
        V_all = np.concatenate([np.asarray(resA[r]["V_loc"]) for r in cores], axis=0)
        sm_all = np.concatenate([np.asarray(resA[r]["sm_loc"]) for r in cores], axis=0)
        wo, wg, wu, wd = f32(w_out[l]), f32(w_gate[l]), f32(w_up[l]), f32(w_down[l])
        in_maps = []
        for core in cores:
            ident, emat, cmask, gmask, hsel = consts[core]
            in_maps.append({"xT_in": xTs[core], "modT": modT, "vecs": vecs, "cvec": cvec,
                            "w_out": wo, "w_gate": wg, "w_up": wu, "w_down": wd,
                            "ident": ident, "emat": emat, "cmask": cmask, "gmask": gmask, "hsel": hsel,
                            "qT_s": np.asarray(resA[core]["qT_s"]), "kT_loc": np.asarray(resA[core]["kT_loc"]),
                            "V_loc": np.asarray(resA[core]["V_loc"]), "kT_all": kT_all, "V_all": V_all, "sm_all": sm_all,
                            "uT_s": np.asarray(resA[core]["uT_s"])})
        resB = run_bass_kernel_spmd(_prog("B", _debug, _stop), in_maps, core_ids=cores).results
        if _debug:
            dbg[("B", l)] = resB
        xTs = [np.asarray(resB[core]["outT"]).reshape(DC, 128, TOK) for core in cores]
    out = np.empty((NT, NCORES, 512, D), np.float32)
    for core in cores:
        out[:, core] = xTs[core].reshape(D, TOK).T.reshape(NT, 512, D)
    if _debug:
        _kernel_unfused._last = dbg
        _kernel_unfused._mod = modall
    return out.reshape(1, SEQ, D)


def kernel(x, c, ada_w, ada_b, mix_pre_g, mix_post_g, w_in, conv_w, conv_b, conv_ln_g, conv_ln_b, w_out,
           ffn_pre_g, ffn_post_g, w_gate, w_up, w_down):
    L = L_FULL
    cores = list(range(NCORES))
    f32 = lambda a: np.ascontiguousarray(np.asarray(a, dtype=np.float32))
    x = f32(x).reshape(SEQ, D)
    ada_w = f32(ada_w)
    ada_b = f32(ada_b)
    cT = _fm(f32(c).reshape(D))
    vecs = np.stack([np.stack([_fm(f32(v)[l]) for v in (mix_pre_g, mix_post_g, ffn_pre_g, ffn_post_g)], axis=1).reshape(128, 4 * DC)
                     for l in range(L)], axis=0)
    cvs = []
    for l in range(L):
        cv = np.concatenate([f32(conv_w)[l], f32(conv_b)[l][None], f32(conv_ln_g)[l][None], f32(conv_ln_b)[l][None]], axis=0)
        cvs.append(np.ascontiguousarray(cv.reshape(34, 8, 128).transpose(2, 1, 0)).reshape(128, 8 * 34))
    cvec = np.stack(cvs, axis=0)
    shared = {"cT": cT, "vecs": np.ascontiguousarray(vecs), "cvec": np.ascontiguousarray(cvec),
              "w_in": f32(w_in), "w_out": f32(w_out), "w_gate": f32(w_gate), "w_up": f32(w_up), "w_down": f32(w_down)}
    xt = x.reshape(NT, NCORES, 512, D)
    in_maps = []
    for core in cores:
        ident, emat, cmask, gmask, hsel = _consts(core)
        xT = np.ascontiguousarray(xt[:, core].reshape(TOK, D).T).reshape(DC, 128, TOK)
        aw = np.ascontiguousarray(ada_w[:, :, core * 1536:(core + 1) * 1536])
        ab = ada_b[:, core * 1536:(core + 1) * 1536].reshape(L, 12, 128)
        ab = np.ascontiguousarray(ab.transpose(2, 0, 1)).reshape(128, L * 12)
        m = dict(shared)
        m.update({"xT_in": xT, "adaw": aw, "adab": ab, "ident": ident, "emat": emat, "cmask": cmask, "gmask": gmask, "hsel": hsel})
        in_maps.append(m)
    res = run_bass_kernel_spmd(_prog("F"), in_maps, core_ids=cores)
    out = np.empty((NT, NCORES, 512, D), np.float32)
    for core in cores:
        oT = np.asarray(res.results[core]["outT"]).reshape(D, TOK)
        out[:, core] = oT.T.reshape(NT, 512, D)
    return out.reshape(1, SEQ, D)
```

```python
import os
import numpy as np
from contextlib import ExitStack
import concourse.bass as bass
import concourse.mybir as mybir
from concourse.bass_utils import run_bass_kernel_spmd

F32 = mybir.dt.float32
BF16 = mybir.dt.bfloat16
AF = mybir.ActivationFunctionType
ALU = mybir.AluOpType
AX = mybir.AxisListType

NCORES = 8
D = 2048
SEQ = 16384
L_FULL = 4
TOK = SEQ // NCORES
NT = 4
DC = D // 128
DFF = 5632
FC = DFF // 128
INC = 5120
CONVK = 31
HALO = 30
NH = 8
RMS_EPS = 1e-6
LN_EPS = 1e-5
SCALE = 128 ** -0.5
MASKV = 30000.0
NEG = -1e30


class Tok:
    __slots__ = ("name", "w", "r", "chan")

    def __init__(self, name):
        self.name = name
        self.w = {}
        self.r = {}
        self.chan = None


class Chan:
    __slots__ = ("sem", "val")

    def __init__(self, sem):
        self.sem = sem
        self.val = 0


class Eng:
    def __init__(self, name, q, sem):
        self.name = name
        self.q = q
        self.sem = sem
        self.cnt = 0
        self.seen = {}
        self.pr = []
        self.pw = []


class _DummyIns:
    def then_inc(self, *a, **k):
        return self


class _DummyEng:
    def __getattr__(self, name):
        def f(*a, **k):
            return _DummyIns()
        return f


class Sched:
    def __init__(self, nc, es, sems):
        self.nc = nc
        self.es = es
        self.engs = {}
        self.chans = []
        self.allchans = []
        self.nsem = 0
        self.sems = sems

    def new_sem(self, name):
        i = self.nsem
        self.nsem += 1
        if i >= len(self.sems):
            self.sems.append(self.es.enter_context(self.nc.semaphore(name)))
        return self.sems[i]

    def add_engine(self, name, q):
        e = Eng(name, q, self.new_sem("prog_" + name))
        self.engs[name] = e
        return e

    def chan(self, name, barrier=True):
        c = Chan(self.new_sem("ch_" + name))
        self.allchans.append(c)
        if barrier:
            self.chans.append(c)
        return c

    def tok(self, name, chan=False):
        t = Tok(name)
        if chan:
            t.chan = self.chan(name)
        return t

    @staticmethod
    def _merge(d, ev):
        s, v = ev
        k = id(s)
        if k not in d or d[k][1] < v:
            d[k] = (s, v)

    def _wait(self, e, ev):
        s, v = ev
        if s is e.sem and e.name in ("pe", "sp"):
            return
        k = id(s)
        if e.seen.get(k, 0) >= v:
            return
        e.q.wait_ge(s, v)
        e.seen[k] = v

    def _deps(self, e, reads, writes):
        for t in reads:
            for ev in t.w.values():
                self._wait(e, ev)
        for t in writes:
            for ev in t.w.values():
                self._wait(e, ev)
            for ev in t.r.values():
                self._wait(e, ev)

    def _commit(self, ev, reads, writes):
        for t in reads:
            self._merge(t.r, ev)
        for t in writes:
            t.w = {}
            t.r = {}
            self._merge(t.w, ev)

    def op(self, ename, fn, r=(), w=(), inc=True):
        e = self.engs[ename]
        self._deps(e, r, w)
        ins = fn(e.q)
        if inc:
            e.cnt += 1
            ins.then_inc(e.sem, 1)
            ev = (e.sem, e.cnt)
            self._commit(ev, list(r) + e.pr, list(w) + e.pw)
            e.pr = []
            e.pw = []
        else:
            e.pr += list(r)
            e.pw += list(w)
        return ins

    def dma(self, ename, out, in_, r=(), w=(), chan=None):
        e = self.engs[ename]
        self._deps(e, r, w)
        chan.val += 16
        e.q.dma_start(out=out, in_=in_).then_inc(chan.sem, 16)
        ev = (chan.sem, chan.val)
        self._commit(ev, r, w)
        return ev

    def collective(self, ename, kind, ins, outs, r=(), w=(), chan=None):
        e = self.engs[ename]
        self._deps(e, r, w)
        chan.val += 1
        e.q.collective_compute(kind, ALU.bypass, replica_groups=[list(range(NCORES))],
                               ins=ins, outs=outs).then_inc(chan.sem, 1)
        ev = (chan.sem, chan.val)
        self._commit(ev, r, w)
        return ev

    def barrier(self, full=False):
        evs = [(e.sem, e.cnt) for e in self.engs.values() if e.cnt > 0]
        evs += [(c.sem, c.val) for c in (self.allchans if full else self.chans) if c.val > 0]
        for e in self.engs.values():
            for ev in evs:
                self._wait(e, ev)


class _Stop(Exception):
    pass


def build_program(kind, debug=False, stop=None):
    nc = bass.Bass("TRN2", target_bir_lowering=False)
    L = 1

    def din(name, shape, dt=F32):
        return nc.dram_tensor(name, list(shape), dt, kind="ExternalInput")

    def dout(name, shape, dt=F32):
        return nc.dram_tensor(name, list(shape), dt, kind="ExternalOutput")

    def dsc(name, shape, dt):
        return nc.dram_tensor(name, list(shape), dt)

    LM = L_FULL
    if kind == "F":
        L = L_FULL
        xT_in = din("xT_in", [DC, 128, TOK])
        cT_in = din("cT", [128, DC])
        adaw_in = din("adaw", [LM, D, 1536])
        adab_in = din("adab", [128, LM * 12])
        vecs_in = din("vecs", [L, 128, 4 * DC])
        cvec_in = din("cvec", [L, 128, 8 * 34])
        w_in_d = din("w_in", [L, D, INC])
        w_out_d = din("w_out", [L, D, D])
        w_gate_d = din("w_gate", [L, D, DFF])
        w_up_d = din("w_up", [L, D, DFF])
        w_down_d = din("w_down", [L, DFF, D])
        ident_in = din("ident", [128, 128])
        emat_in = din("emat", [64, 64 * 128])
        cmask_in = din("cmask", [128, 4 * 512])
        gmask_in = din("gmask", [128, NT * 256])
        hsel_in = din("hsel", [128, 8])
        outT = dout("outT", [DC, 128, TOK])
        xT_s = dsc("xT_s", [DC, 128, TOK], F32)
        qT_s = dsc("qT_s", [NH, 128, TOK], BF16)
        kT_loc = dsc("kT_loc", [NH * 128, TOK], BF16)
        kT_all = dsc("kT_all", [NCORES * NH * 128, TOK], BF16)
        V_loc = dsc("V_loc", [NH * TOK, 128], BF16)
        V_all = dsc("V_all", [NCORES * NH * TOK, 128], BF16)
        sm_loc = dsc("sm_loc", [128, 1024], BF16)
        sm_all = dsc("sm_all", [NCORES * 128, 1024], BF16)
        mod_loc = dsc("mod_loc", [128, 512], F32)
        mod_all = dsc("mod_all", [NCORES * 128, 512], F32)
        uT_s = dsc("uT_s", [8, 128, TOK], BF16)
        h2T_s = dsc("h2T_s", [DC, 128, TOK], BF16)
        actT_s = dsc("actT_s", [FC, 128, TOK], BF16)
        wb_in = [dsc(f"wb_in{l}", [D, INC], BF16) for l in range(L)]
        wb_out = [dsc(f"wb_out{l}", [D, D], BF16) for l in range(L)]
        wb_gate = [dsc(f"wb_gate{l}", [D, DFF], BF16) for l in range(L)]
        wb_up = [dsc(f"wb_up{l}", [D, DFF], BF16) for l in range(L)]
        wb_down = [dsc(f"wb_down{l}", [DC, 128, FC * 128], BF16) for l in range(L)]
    elif kind == "M":
        cT_in = din("cT", [128, DC])
        adaw_in = din("adaw", [LM, D, 1536])
        adab_in = din("adab", [128, LM * 12])
        mod_out = dout("mod_out", [128, LM * 12])
    else:
        xT_in = din("xT_in", [DC, 128, TOK])
        modT_in = din("modT", [128, NCORES * 12])
        vecs_in = din("vecs", [128, 4 * DC])
    if kind == "A":
        w_in_d = din("w_in", [D, INC])
        qT_s = dout("qT_s", [NH, 128, TOK], BF16)
        kT_loc = dout("kT_loc", [NH * 128, TOK], BF16)
        V_loc = dout("V_loc", [NH * TOK, 128], BF16)
        sm_loc = dout("sm_loc", [128, 1024], BF16)
        uT_s = dout("uT_s", [8, 128, TOK], BF16)
        wb_in = [dsc("wb_in0", [D, INC], BF16)]
    if kind == "B":
        cvec_in = din("cvec", [128, 8 * 34])
        w_out_d = din("w_out", [D, D])
        w_gate_d = din("w_gate", [D, DFF])
        w_up_d = din("w_up", [D, DFF])
        w_down_d = din("w_down", [DFF, D])
        ident_in = din("ident", [128, 128])
        emat_in = din("emat", [64, 64 * 128])
        cmask_in = din("cmask", [128, 4 * 512])
        gmask_in = din("gmask", [128, NT * 256])
        hsel_in = din("hsel", [128, 8])
        qT_s = din("qT_s", [NH, 128, TOK], BF16)
        kT_loc = din("kT_loc", [NH * 128, TOK], BF16)
        V_loc = din("V_loc", [NH * TOK, 128], BF16)
        kT_all = din("kT_all", [NCORES * NH * 128, TOK], BF16)
        V_all = din("V_all", [NCORES * NH * TOK, 128], BF16)
        sm_all = din("sm_all", [NCORES * 128, 1024], BF16)
        uT_s = din("uT_s", [8, 128, TOK], BF16)
        outT = dout("outT", [DC, 128, TOK])
        xT_s = dsc("xT_s", [DC, 128, TOK], F32)
        h2T_s = dsc("h2T_s", [DC, 128, TOK], BF16)
        actT_s = dsc("actT_s", [FC, 128, TOK], BF16)
        wb_out = [dsc("wb_out0", [D, D], BF16)]
        wb_gate = [dsc("wb_gate0", [D, DFF], BF16)]
        wb_up = [dsc("wb_up0", [D, DFF], BF16)]
        wb_down = [dsc("wb_down0", [DC, 128, FC * 128], BF16)]
    dbg = {}
    if debug and kind == "B":
        dbg["mixT"] = dout("dbg_mixT", [DC, 128, TOK], BF16)
    if debug and kind == "A":
        dbg["hT"] = dout("dbg_hT", [DC, 128, TOK], BF16)

    with ExitStack() as es:
        def sb(name, shape, dt):
            return es.enter_context(nc.sbuf_tensor("s_" + name, list(shape), dt))

        def pst(name, shape, dt):
            return es.enter_context(nc.psum_tensor("p_" + name, list(shape), dt))

        bufA = sb("bufA", [128, DC, TOK], BF16)
        bufB0 = sb("bufB0", [128, DC, 512], F32)
        bufB1 = sb("bufB1", [128, DC, 512], F32)
        wring = [sb(f"wring{i}", [128, 8192], BF16) for i in range(3)]
        b0b = bufB0[:].rearrange("p j n -> p (j n)").bitcast(BF16)
        b1b = bufB1[:].rearrange("p j n -> p (j n)").bitcast(BF16)
        b0f = bufB0[:].rearrange("p j n -> p (j n)")
        b1f = bufB1[:].rearrange("p j n -> p (j n)")
        stage = [b0b[:, i * TOK:(i + 1) * TOK] for i in range(3)]
        emat = b0b[0:64, 0:8192].rearrange("p (a b) -> p a b", a=64)
        cmask = b0b[:, 8192:10240].rearrange("p (a b) -> p a b", a=4)
        gmask = b0f[:, 5120:6144].rearrange("p (a b) -> p a b", a=NT)
        qTh = [b0b[:, 12288 + i * TOK:12288 + (i + 1) * TOK] for i in range(2)]
        pT = [b1b[:, i * 512:(i + 1) * 512] for i in range(4)]
        nmT = [b1b[0:64, 2048 + i * 512:2048 + (i + 1) * 512] for i in range(2)]
        kown = b1b[:, 3072:3584]
        vown = b1b[:, 3584:4096].rearrange("p (k d) -> p k d", k=4)
        gm = b1f[:, 2048:2304].rearrange("p (a b) -> p a b", a=4)
        max8 = b1f[:, 2304:2336].rearrange("p (a b) -> p a b", a=4)
        thr = b1f[:, 2336:2340]
        nm = b1b[:, 4736:4992].rearrange("p (a b) -> p a b", a=4)
        kmh = b1b[:, 4992:5056]
        hall = wring[0][:, 0:NCORES * 960].rearrange("p (r n) -> p r n", r=NCORES)
        dwm = wring[1][:, 0:CONVK * 128].rearrange("p (j n) -> p j n", j=CONVK)
        upad = [wring[1][:, 4096 + i * 544:4096 + i * 544 + HALO + 512] for i in range(2)]
        halo_sel = wring[1][:, 5632:5632 + NT * 240].rearrange("p (t n) -> p t n", t=NT)
        sqr = [sb(f"sqr{i}", [128, 512], F32) for i in range(3)]
        rstd_t = sb("rstd", [128, 512], F32)
        mean_t = sb("mean", [128, 512], F32)
        tmp_t = [sb(f"tmpf{i}", [128, 512], F32) for i in range(2)]
        ident_f = sb("ident_f", [128, 128], F32)
        ident_b = sb("ident_b", [128, 128], BF16)
        ones_f = sb("ones_f", [128, 128], F32)
        ones_b = sb("ones_b", [128, 128], BF16)
        hsel = sb("hsel", [128, 8], F32)
        epsr = sb("epsr", [128, 1], F32)
        epsl = sb("epsl", [128, 1], F32)
        cT = sb("cT", [128, DC], F32)
        cact = sb("cact", [128, DC], F32)
        adab = sb("adab", [128, LM * 12], F32)
        modp = sb("modp", [128, LM * 12], F32)
        modT = sb("modT", [128, NCORES, LM * 12 if kind == "F" else 12], F32)
        vecs = sb("vecs", [128, 1, 4, DC], F32)
        cvec = sb("cvec", [128, 1, 8, 34], F32)
        gsc1 = sb("gsc1", [128, DC], F32)
        gsc2 = sb("gsc2", [128, DC], F32)
        gp1 = sb("gp1", [128, DC], F32)
        gp2 = sb("gp2", [128, DC], F32)
        sh1 = sb("sh1", [128, DC], F32)
        sh2 = sb("sh2", [128, DC], F32)
        kms = sb("kms", [128, NH, 8], F32)
        smst = sb("smst", [128, 1024], BF16)
        kmT = sb("kmT", [128, NCORES, 64], BF16)

        ps = [pst(f"ps{i}", [128, 512], F32) for i in range(7)]
        psb = pst("psb", [64, 512], BF16)

        sems = []
        block = es.enter_context(nc.Block())

        def program(S, pe, act, dve, pool, sp):
            S.add_engine("pe", pe)
            S.add_engine("act", act)
            S.add_engine("dve", dve)
            S.add_engine("pool", pool)
            S.add_engine("sp", sp)

            T = {}

            def tk(name, chan=False):
                if name not in T:
                    T[name] = S.tok(name, chan)
                return T[name]

            tA = tk("bufA", True)
            tB0 = tk("bufB0", True)
            tB1 = tk("bufB1", True)
            tW = [tk(f"wring{i}", True) for i in range(3)]
            tSt = [tk(f"stage{i}", True) for i in range(3)]
            tSq = [tk(f"sqr{i}") for i in range(3)]
            tPs = [tk(f"ps{i}") for i in range(7)]
            tPsb = tk("psb")
            tRstd = tk("rstd")
            tMean = tk("mean")
            tTmp = [tk("tmpf0"), tk("tmpf1")]
            tConst = tk("const", True)
            tMisc = tk("misc", True)
            tSm = tk("smst", True)
            tHall = tk("hall", True)
            tUp = [tk("upad0", True), tk("upad1", True)]
            tDw = tk("dwm")
            tNmT = [tk("nmT0"), tk("nmT1")]
            tPT = [tk(f"pT{i}") for i in range(4)]
            tQ = [tk("qTh0", True), tk("qTh1", True)]
            tOwn = tk("own", True)
            tGate = tk("gate")
            tKms = tk("kms")
            tAtc = tk("attc", True)
            dX = tk("d_xT")
            dQ = tk("d_qT")
            dKl = tk("d_kTloc")
            dKa = tk("d_kTall")
            dVl = tk("d_Vloc")
            dVa = tk("d_Vall")
            dSl = tk("d_smloc")
            dSa = tk("d_small")
            dU = tk("d_uT")
            dH2 = tk("d_h2T")
            dAct = tk("d_actT")
            dWb = [tk(f"d_wb{l}") for l in range(L)]
            dMl = tk("d_modloc")
            dMa = tk("d_modall")
            dOut = tk("d_out")
            dDbg = tk("d_dbg")
            chCast = S.chan("cast", barrier=False)
            chCC = S.chan("cc")

            def cast_weights(l):
                if kind == "A":
                    lst = ((w_in_d, wb_in, D),)
                elif kind == "B":
                    lst = ((w_out_d, wb_out, D), (w_gate_d, wb_gate, D), (w_up_d, wb_up, D), (w_down_d, wb_down, DFF))
                else:
                    lst = ((w_in_d, wb_in, D), (w_out_d, wb_out, D), (w_gate_d, wb_gate, D), (w_up_d, wb_up, D), (w_down_d, wb_down, DFF))
                for (src, dst, rows) in lst:
                    nsp = 8
                    rs = rows // nsp
                    sap = src.ap()[l] if kind == "F" else src.ap()
                    if rows == DFF:
                        for j in range(DC):
                            for kq in range(4):
                                k0, k1 = kq * 11, (kq + 1) * 11
                                S.dma("pool", dst[l].ap()[j][:, k0 * 128:k1 * 128].rearrange("p (k n) -> p k n", n=128),
                                      sap[k0 * 128:k1 * 128, j * 128:(j + 1) * 128].rearrange("(k p) n -> p k n", p=128),
                                      r=(), w=(dWb[l],), chan=chCast)
                        continue
                    for i in range(nsp):
                        S.dma("pool", dst[l].ap()[i * rs:(i + 1) * rs, :], sap[i * rs:(i + 1) * rs, :],
                              r=(), w=(dWb[l],), chan=chCast)

            wslot = [0]

            def load_wblock(wdram, c0, ncols, kch, l):
                i = wslot[0] % 3
                wslot[0] += 1
                view = wring[i][:, 0:kch * ncols].rearrange("p (k n) -> p k n", k=kch)
                S.dma("sp", view, wdram.ap()[:, c0:c0 + ncols].rearrange("(k p) n -> p k n", p=128),
                      r=(dWb[l],), w=(tW[i],), chan=tW[i].chan)
                return view, tW[i]

            psrot = [0]

            def next_ps(lo=0, hi=7):
                i = lo + psrot[0] % (hi - lo)
                psrot[0] += 1
                return ps[i], tPs[i]

            sqrot = [0]
            strot = [0]

            def next_stage():
                i = strot[0] % 3
                strot[0] += 1
                return stage[i], tSt[i]

            evrot = [0]

            def evac_copy(out, in_, r, w):
                evrot[0] += 1
                if evrot[0] % 2:
                    S.op("act", lambda q: q.activation(out=out, in_=in_, func=AF.Identity), r=r, w=w)
                else:
                    S.op("dve", lambda q: q.tensor_copy(out=out, in_=in_), r=r, w=w)

            def sumsq_accum(src_ap, src_tok, first, last, acc_ps, acc_tok):
                i = sqrot[0] % 3
                sqrot[0] += 1
                S.op("act", lambda q: q.activation(out=sqr[i][:], in_=src_ap, func=AF.Square), r=(src_tok,), w=(tSq[i],))
                S.op("pe", lambda q: q.matmul(acc_ps[:], lhsT=ones_f[:], rhs=sqr[i][:], start=first, stop=last),
                     r=(tSq[i], tConst), w=(acc_tok,), inc=True)

            def rstd_from(acc_ps, acc_tok, n, eps):
                ept = epsr if eps == RMS_EPS else epsl
                S.op("act", lambda q: q.activation(out=rstd_t[:], in_=acc_ps[:], func=AF.Sqrt, bias=ept[:, 0:1], scale=1.0 / n),
                     r=(acc_tok, tConst), w=(tRstd,))
                S.op("dve", lambda q: q.reciprocal(out=rstd_t[:], in_=rstd_t[:]), r=(tRstd,), w=(tRstd,))

            def prenorm(xt, xtok, gsc, shv, dst_fn, dst_tok):
                acc, acct = ps[6], tPs[6]
                for j in range(DC):
                    sumsq_accum(xt[:, j, :], xtok, j == 0, j == DC - 1, acc, acct)
                rstd_from(acc, acct, D, RMS_EPS)
                S.op("dve", lambda q: q.tensor_tensor(out=xt, in0=xt, in1=rstd_t[:].unsqueeze(1).to_broadcast([128, DC, 512]),
                                                       op=ALU.mult), r=(xtok, tRstd), w=(xtok,))
                for j in range(DC):
                    S.op("act", lambda q, j=j: q.activation(out=dst_fn(j), in_=xt[:, j, :], func=AF.Identity,
                                                             bias=shv[:, j:j + 1], scale=gsc[:, j:j + 1]),
                         r=(xtok, tMisc), w=(dst_tok,))

            def post_residual(yt, ytok, xt, xtok, gp):
                rstd_from(ps[6], tPs[6], D, RMS_EPS)
                for j in range(DC):
                    S.op("dve", lambda q, j=j: q.scalar_tensor_tensor(out=yt[:, j, :], in0=yt[:, j, :], scalar=gp[:, j:j + 1],
                                                                      in1=rstd_t[:], op0=ALU.mult, op1=ALU.mult),
                         r=(ytok, tRstd, tMisc), w=(ytok,))
                S.op("dve", lambda q: q.tensor_tensor(out=xt, in0=xt, in1=yt, op=ALU.add), r=(xtok, ytok), w=(xtok,))

            cc = tConst.chan
            S.op("dve", lambda q: q.memset(ones_f[:], 1.0), w=(tConst,))
            S.op("dve", lambda q: q.memset(ones_b[:], 1.0), w=(tConst,))
            S.op("dve", lambda q: q.memset(epsr[:], RMS_EPS), w=(tConst,))
            S.op("dve", lambda q: q.memset(epsl[:], LN_EPS), w=(tConst,))
            if kind == "F":
                cast_weights(0)
            if kind in ("M", "F"):
                S.dma("sp", cT[:], cT_in.ap(), w=(tConst,), chan=cc)
                S.dma("sp", adab[:], adab_in.ap(), w=(tConst,), chan=cc)
                S.op("act", lambda q: q.activation(out=cact[:], in_=cT[:], func=AF.Silu), r=(tConst,), w=(tMisc,))
                for l in range(LM):
                    for half in range(3):
                        buf, bt = (bufB0, tB0) if (l * 3 + half) % 2 == 0 else (bufB1, tB1)
                        S.dma("sp", buf[:], adaw_in.ap()[l, :, half * 512:(half + 1) * 512].rearrange("(k p) n -> p k n", p=128),
                              w=(bt,), chan=bt.chan)
                        for mm in range(4):
                            col = l * 12 + half * 4 + mm
                            for k in range(DC):
                                S.op("pe", lambda q, k=k, mm=mm, col=col, buf=buf: q.matmul(
                                    ps[0][:, col:col + 1], lhsT=buf[:, k, mm * 128:(mm + 1) * 128], rhs=cact[:, k:k + 1],
                                    start=(k == 0), stop=(k == DC - 1)), r=(bt, tMisc), w=(tPs[0],), inc=(k == DC - 1))
                S.op("dve", lambda q: q.tensor_tensor(out=modp[:], in0=ps[0][:, 0:LM * 12], in1=adab[:], op=ALU.add),
                     r=(tPs[0], tConst), w=(tMisc,))
                if kind == "M":
                    S.dma("sp", mod_out.ap(), modp[:], r=(tMisc,), w=(dOut,), chan=tMisc.chan)
                    S.barrier(full=True)
                    return
                S.dma("sp", mod_loc.ap()[:, 0:LM * 12], modp[:], r=(tMisc,), w=(dMl,), chan=tMisc.chan)
                S.collective("pool", "AllGather", [mod_loc.ap()], [mod_all.ap()], r=(dMl,), w=(dMa,), chan=chCC)
                S.dma("sp", modT[:], mod_all.ap()[:, 0:LM * 12].rearrange("(r p) n -> p r n", p=128), r=(dMa,), w=(tMisc,), chan=tMisc.chan)
                S.barrier()
            xsrc = [xT_in]
            if kind != "F":
                S.dma("sp", modT[:].rearrange("p r n -> p (r n)"), modT_in.ap(), w=(tMisc,), chan=tMisc.chan)
                S.dma("sp", vecs[:].rearrange("p a b c -> p (a b c)"), vecs_in.ap(), w=(tConst,), chan=cc)
                cast_weights(0)
            if kind in ("B", "F"):
                S.dma("sp", ident_f[:], ident_in.ap(), w=(tConst,), chan=cc)
                S.dma("pool", ident_b[:], ident_in.ap(), w=(tConst,), chan=cc)
                S.dma("sp", hsel[:], hsel_in.ap(), w=(tConst,), chan=cc)
                if kind == "B":
                    S.dma("sp", cvec[:].rearrange("p a b c -> p (a b c)"), cvec_in.ap(), w=(tConst,), chan=cc)

            def ckpt(k):
                if stop == k:
                    S.barrier(full=True)
                    raise _Stop()

            ckpt(0)

            def mod_chunk(l, m):
                r_, mm = divmod(m, 12)
                if kind == "F":
                    return modT[:, r_, l * 12 + mm:l * 12 + mm + 1]
                return modT[:, r_, mm:mm + 1]

            def layer_vectors(l):
                for j in range(DC):
                    for (dst, gidx, scm, shdst, shm, gpd, gm_, pidx) in (
                            (gsc1, 0, 16 + j, sh1, 0 + j, gp1, 32 + j, 1),
                            (gsc2, 2, 64 + j, sh2, 48 + j, gp2, 80 + j, 3)):
                        S.op("dve", lambda q, dst=dst, gidx=gidx, scm=scm: q.scalar_tensor_tensor(
                            out=dst[:, j:j + 1], in0=mod_chunk(l, scm), scalar=1.0, in1=vecs[:, 0, gidx, j:j + 1],
                            op0=ALU.add, op1=ALU.mult), r=(tMisc, tConst), w=(tMisc,))
                        S.op("dve", lambda q, shdst=shdst, shm=shm: q.tensor_copy(out=shdst[:, j:j + 1], in_=mod_chunk(l, shm)),
                             r=(tMisc,), w=(tMisc,))
                        S.op("dve", lambda q, gpd=gpd, gm_=gm_, pidx=pidx: q.tensor_tensor(
                            out=gpd[:, j:j + 1], in0=mod_chunk(l, gm_), in1=vecs[:, 0, pidx, j:j + 1], op=ALU.mult),
                            r=(tMisc, tConst), w=(tMisc,))

            def proj_block(wv, wtok, jj, t, pbank, ptok):
                for k in range(DC):
                    S.op("pe", lambda q, k=k: q.matmul(pbank[:], lhsT=wv[:, k, jj * 128:(jj + 1) * 128],
                                                        rhs=bufA[:, k, t * 512:(t + 1) * 512],
                                                        start=(k == 0), stop=(k == DC - 1)),
                         r=(wtok, tA), w=(ptok,), inc=(k == DC - 1))


            def ph2(l):
                S.op("dve", lambda q: q.memset(smst[:], 0.0), w=(tSm,))
                S.op("dve", lambda q: q.memset(kms[:].rearrange("p a b -> p (a b)"), 0.0), w=(tKms,))

                for blk in range(4):
                    wv, wtok = load_wblock(wb_in[l], blk * 512, 512, DC, l)
                    for jj in range(4):
                        ch = blk * 4 + jj
                        st, stt = next_stage()
                        for t in range(NT):
                            pb, pt_ = next_ps(0, 6)
                            proj_block(wv, wtok, jj, t, pb, pt_)
                            if ch >= 8:
                                h = ch - 8
                                for b2 in range(2):
                                    S.op("act", lambda q, pb=pb, st=st, t=t, b2=b2, h=h: q.activation(
                                        out=st[:, t * 512 + b2 * 256:t * 512 + (b2 + 1) * 256], in_=pb[:, b2 * 256:(b2 + 1) * 256],
                                        func=AF.Identity, accum_out=kms[:, h, 2 * t + b2:2 * t + b2 + 1]), r=(pt_,), w=(stt, tKms))
                            else:
                                S.op("act", lambda q, pb=pb, st=st, t=t: q.activation(out=st[:, t * 512:(t + 1) * 512], in_=pb[:], func=AF.Identity),
                                     r=(pt_,), w=(stt,))
                        if os.environ.get("KSKIP_ST"):
                            pass
                        elif ch < 8:
                            S.dma("pool", qT_s.ap()[ch], st, r=(stt,), w=(dQ,), chan=stt.chan)
                        else:
                            S.dma("pool", kT_loc.ap()[(ch - 8) * 128:(ch - 7) * 128, :], st, r=(stt,), w=(dKl,), chan=stt.chan)
                S.op("dve", lambda q: q.tensor_copy(out=smst[:, 0:64], in_=kms[:].rearrange("p a b -> p (a b)")),
                     r=(tKms,), w=(tSm,))
                ckpt(10)
                for bv in range(2):
                    wv, wtok = load_wblock(wb_in[l], 2048 + bv * 512, 512, DC, l)
                    for tc4 in range(4):
                        st, stt = next_stage()
                        for tcc in range(4):
                            tc = tc4 * 4 + tcc
                            pb, pt_ = next_ps(0, 6)
                            for k in range(DC):
                                S.op("pe", lambda q, k=k, tc=tc: q.matmul(pb[:], lhsT=bufA[:, k, tc * 128:(tc + 1) * 128],
                                                                           rhs=wv[:, k, :], start=(k == 0), stop=(k == DC - 1)),
                                     r=(wtok, tA), w=(pt_,), inc=(k == DC - 1))
                            evac_copy(st[:, tcc * 512:(tcc + 1) * 512], pb[:], (pt_,), (stt,))
                        for hh in range(4):
                            dst = V_loc.ap().rearrange("(h c p) d -> p c h d", h=NH, p=128)[:, tc4 * 4:(tc4 + 1) * 4, bv * 4 + hh, :]
                            S.dma("pool", dst, st.rearrange("p (c h d) -> p c h d", c=4, h=4)[:, :, hh, :], r=(stt,), w=(dVl,), chan=stt.chan)
                ckpt(11)
                for bb in range(2):
                    wa, wat = load_wblock(wb_in[l], 3072 + bb * 512, 512, DC, l)
                    wg, wgt = load_wblock(wb_in[l], 4096 + bb * 512, 512, DC, l)
                    for jj in range(4):
                        i = bb * 4 + jj
                        st, stt = next_stage()
                        for t in range(NT):
                            pg, pgt = next_ps(0, 6)
                            proj_block(wg, wgt, jj, t, pg, pgt)
                            pa, pat = next_ps(0, 6)
                            proj_block(wa, wat, jj, t, pa, pat)
                            sg, sgt = tmp_t[t % 2], tTmp[t % 2]
                            S.op("act", lambda q, pg=pg, sg=sg: q.activation(out=sg[:], in_=pg[:], func=AF.Sigmoid), r=(pgt,), w=(sgt,))
                            S.op("dve", lambda q, pa=pa, sg=sg, st=st, t=t: q.tensor_tensor(
                                out=st[:, t * 512:(t + 1) * 512], in0=pa[:], in1=sg[:], op=ALU.mult), r=(pat, sgt), w=(stt,))
                            S.op("dve", lambda q, st=st, t=t, i=i: q.tensor_copy(
                                out=smst[:, 64 + (t * 8 + i) * HALO:64 + (t * 8 + i + 1) * HALO],
                                in_=st[:, t * 512 + 512 - HALO:(t + 1) * 512]), r=(stt,), w=(tSm,))
                        S.dma("pool", uT_s.ap()[i], st, r=(stt,), w=(dU,), chan=stt.chan)
                S.dma("pool", sm_loc.ap(), smst[:], r=(tSm,), w=(dSl,), chan=tSm.chan)

            for l in range(L):
                last = (l == L - 1)
                if kind == "F":
                    S.barrier()
                    S.dma("sp", vecs[:].rearrange("p a b c -> p (a b c)"), vecs_in.ap()[l], w=(tConst,), chan=cc)
                    S.dma("sp", cvec[:].rearrange("p a b c -> p (a b c)"), cvec_in.ap()[l], w=(tConst,), chan=cc)
                layer_vectors(l)
                S.barrier()
                for t in range(NT if kind in ("A", "F") else 0):
                    buf, bt = (bufB0, tB0) if t % 2 == 0 else (bufB1, tB1)
                    S.dma("sp", buf[:], xsrc[0].ap()[:, :, t * 512:(t + 1) * 512].rearrange("j p n -> p j n"),
                          r=(dX,), w=(bt,), chan=bt.chan)
                    prenorm(buf[:], bt, gsc1, sh1, lambda j, t=t: bufA[:, j, t * 512:(t + 1) * 512], tA)
                if debug and kind == "A":
                    S.dma("sp", dbg["hT"].ap().rearrange("j p n -> p j n"), bufA[:], r=(tA,), w=(dDbg,), chan=tA.chan)
                S.barrier()
                ckpt(1)
                if kind == "A":
                    ph2(l)
                    S.barrier(full=True)
                    return
                if kind == "F":
                    ph2(l)
                    S.collective("pool", "AllGather", [kT_loc.ap()], [kT_all.ap()], r=(dKl,), w=(dKa,), chan=chCC)
                    S.collective("pool", "AllGather", [V_loc.ap().rearrange("(a b) d -> a (b d)", b=8)],
                                 [V_all.ap().rearrange("(a b) d -> a (b d)", b=8)], r=(dVl,), w=(dVa,), chan=chCC)
                    S.collective("pool", "AllGather", [sm_loc.ap()], [sm_all.ap()], r=(dSl,), w=(dSa,), chan=chCC)
                    if l + 1 < L:
                        cast_weights(l + 1)
                S.barrier()
                ckpt(3)
                S.dma("sp", hall, sm_all.ap()[:, 64:1024].rearrange("(r p) n -> p r n", p=128), r=(dSa,), w=(tHall,), chan=tHall.chan)
                S.dma("sp", kmT[:], sm_all.ap()[:, 0:64].rearrange("(r p) n -> p r n", p=128), r=(dSa,), w=(tHall,), chan=tHall.chan)
                for t in range(NT):
                    first = True
                    for r_ in range(8):
                        tp = t if r_ < 7 else t - 1
                        if tp < 0:
                            continue
                        src = hall[:, r_, tp * 240:(tp + 1) * 240]
                        if first:
                            S.op("dve", lambda q, src=src, r_=r_: q.tensor_scalar(out=halo_sel[:, t, :], in0=src, scalar1=hsel[:, r_:r_ + 1],
                                                                               scalar2=None, op0=ALU.mult), r=(tHall, tConst), w=(tMisc,))
                            first = False
                        else:
                            S.op("dve", lambda q, src=src, r_=r_: q.scalar_tensor_tensor(out=halo_sel[:, t, :], in0=src, scalar=hsel[:, r_:r_ + 1],
                                                                                      in1=halo_sel[:, t, :], op0=ALU.mult, op1=ALU.add),
                                 r=(tHall, tConst, tMisc), w=(tMisc,))
                cT_all = [bufB0, bufB1]
                ui = 0
                for i in range(8):
                    for j in range(CONVK):
                        S.op("dve", lambda q, j=j, i=i: q.tensor_scalar(out=dwm[:, j, :], in0=ident_b[:], scalar1=cvec[:, 0, i, j:j + 1],
                                                                        scalar2=None, op0=ALU.mult), r=(tConst,), w=(tDw,))
                    for t in range(NT):
                        up, upt = upad[ui % 2], tUp[ui % 2]
                        ui += 1
                        S.dma("sp", up[:, HALO:HALO + 512], uT_s.ap()[i, :, t * 512:(t + 1) * 512], r=(dU,), w=(upt,), chan=upt.chan)
                        S.op("dve", lambda q, up=up, t=t, i=i: q.tensor_copy(out=up[:, 0:HALO], in_=halo_sel[:, t, i * HALO:(i + 1) * HALO]),
                             r=(tMisc,), w=(upt,))
                        pb, pt_ = next_ps(0, 6)
                        for j in range(CONVK):
                            S.op("pe", lambda q, j=j, up=up: q.matmul(pb[:], lhsT=dwm[:, j, :], rhs=up[:, j:j + 512],
                                                                       start=(j == 0), stop=(j == CONVK - 1)),
                                 r=(tDw, upt), w=(pt_,), inc=(j == CONVK - 1))
                        cb, cbt = (bufB0, tB0) if i < 4 else (bufB1, tB1)
                        S.op("act", lambda q, pb=pb, cb=cb, i=i, t=t: q.activation(out=cb[:, (i % 4) * 4 + t, :], in_=pb[:], func=AF.Identity,
                                                                               bias=cvec[:, 0, i, 31:32], scale=1.0), r=(pt_, tConst), w=(cbt,))
                for t in range(NT):
                    for i in range(8):
                        cb, cbt = (bufB0, tB0) if i < 4 else (bufB1, tB1)
                        src = cb[:, (i % 4) * 4 + t, :]
                        S.op("pe", lambda q, src=src, i=i: q.matmul(ps[5][:], lhsT=ones_f[:], rhs=src, start=(i == 0), stop=(i == 7)),
                             r=(cbt, tConst), w=(tPs[5],), inc=(i == 7))
                        sumsq_accum(src, cbt, i == 0, i == 7, ps[6], tPs[6])
                    S.op("dve", lambda q: q.tensor_scalar(out=mean_t[:], in0=ps[5][:], scalar1=1.0 / 1024, scalar2=None, op0=ALU.mult),
                         r=(tPs[5],), w=(tMean,))
                    S.op("dve", lambda q: q.tensor_tensor(out=tmp_t[0][:], in0=mean_t[:], in1=mean_t[:], op=ALU.mult), r=(tMean,), w=(tTmp[0],))
                    S.op("dve", lambda q: q.scalar_tensor_tensor(out=rstd_t[:], in0=ps[6][:], scalar=1.0 / 1024, in1=tmp_t[0][:],
                                                                  op0=ALU.mult, op1=ALU.subtract), r=(tPs[6], tTmp[0]), w=(tRstd,))
                    S.op("act", lambda q: q.activation(out=rstd_t[:], in_=rstd_t[:], func=AF.Sqrt, bias=epsl[:, 0:1], scale=1.0),
                         r=(tRstd, tConst), w=(tRstd,))
                    S.op("dve", lambda q: q.reciprocal(out=rstd_t[:], in_=rstd_t[:]), r=(tRstd,), w=(tRstd,))
                    for i in range(8):
                        cb, cbt = (bufB0, tB0) if i < 4 else (bufB1, tB1)
                        src = cb[:, (i % 4) * 4 + t, :]
                        S.op("dve", lambda q, src=src: q.tensor_tensor(out=src, in0=src, in1=mean_t[:], op=ALU.subtract), r=(cbt, tMean), w=(cbt,))
                        S.op("dve", lambda q, src=src: q.tensor_tensor(out=src, in0=src, in1=rstd_t[:], op=ALU.mult), r=(cbt, tRstd), w=(cbt,))
                        S.op("act", lambda q, src=src, i=i, t=t: q.activation(out=bufA[:, 8 + i, t * 512:(t + 1) * 512], in_=src, func=AF.Silu,
                                                                              bias=cvec[:, 0, i, 33:34], scale=cvec[:, 0, i, 32:33]),
                             r=(cbt, tConst), w=(tA,))
                S.barrier()
                ckpt(4)
                NSL = 16
                S.dma("pool", emat.rearrange("p a b -> p (a b)"), emat_in.ap(), w=(tAtc,), chan=tAtc.chan)
                S.dma("pool", cmask.rearrange("p a b -> p (a b)"), cmask_in.ap(), w=(tAtc,), chan=tAtc.chan)
                S.dma("sp", gmask.rearrange("p a b -> p (a b)"), gmask_in.ap(), w=(tAtc,), chan=tAtc.chan)
                kring = [wring[0][:, s * 512:(s + 1) * 512] for s in range(NSL)]
                vring = [wring[1][:, s * 512:(s + 1) * 512].rearrange("p (k d) -> p k d", k=4) for s in range(NSL)]
                tKV = [tk(f"kv{s}", True) for s in range(NSL)]
                kvrot = 0
                prot = 0
                nmrot = 0
                srot = 0
                for h in range(NH):
                    qt, qtt = qTh[h % 2], tQ[h % 2]
                    S.dma("sp", qt, qT_s.ap()[h], r=(dQ,), w=(qtt,), chan=qtt.chan)
                    S.op("dve", lambda q, h=h: q.tensor_copy(out=kmh.rearrange("p (r b) -> p r b", r=8), in_=kmT[:, :, h * 8:(h + 1) * 8]),
                         r=(tHall,), w=(tGate,))
                    for t in range(NT):
                        for c_ in range(4):
                            S.op("pe", lambda q, c_=c_: q.matmul(ps[5][:, c_ * 64:(c_ + 1) * 64], lhsT=qt[:, t * 512 + c_ * 128:t * 512 + (c_ + 1) * 128],
                                                                  rhs=kmh, start=True, stop=True), r=(qtt, tGate), w=(tPs[5],), inc=(c_ == 3))
                        S.op("dve", lambda q: q.tensor_tensor(out=gm.rearrange("p a b -> p (a b)"), in0=ps[5][:, 0:256], in1=gmask[:, t, :], op=ALU.add),
                             r=(tPs[5], tAtc), w=(tGate,))
                        for c_ in range(4):
                            S.op("dve", lambda q, c_=c_: q.max(out=max8[:, c_, :], in_=gm[:, c_, :]), r=(tGate,), w=(tGate,))
                        S.op("dve", lambda q: q.tensor_scalar(out=thr, in0=max8[:, :, 2], scalar1=-1e29, scalar2=None, op0=ALU.max), r=(tGate,), w=(tGate,))
                        for c_ in range(4):
                            S.op("dve", lambda q, c_=c_: q.tensor_scalar(out=nm[:, c_, :], in0=gm[:, c_, :], scalar1=thr[:, c_:c_ + 1], scalar2=1.0,
                                                                         op0=ALU.is_ge, op1=ALU.subtract), r=(tGate,), w=(tGate,))
                        for c_ in range(4):
                            S.op("pe", lambda q, c_=c_: q.transpose(out=psb[:, c_ * 128:(c_ + 1) * 128], in_=nm[:, c_, :], identity=ident_b[:]),
                                 r=(tGate, tConst), w=(tPsb,), inc=(c_ == 3))
                        nmt, nmtt = nmT[nmrot % 2], tNmT[nmrot % 2]
                        nmrot += 1
                        S.op("act", lambda q, nmt=nmt: q.activation(out=nmt, in_=psb[:], func=AF.Identity), r=(tPsb,), w=(nmtt,))
                        visits = []
                        S.dma("sp", kown, kT_loc.ap()[h * 128:(h + 1) * 128, t * 512:(t + 1) * 512], r=(dKl,), w=(tOwn,), chan=tOwn.chan)
                        S.dma("sp", vown, V_loc.ap()[h * TOK + t * 512:h * TOK + (t + 1) * 512, :].rearrange("(k p) d -> p k d", p=128),
                              r=(dVl,), w=(tOwn,), chan=tOwn.chan)
                        for kt in range(4):
                            visits.append(("own", kown[:, kt * 128:(kt + 1) * 128], vown[:, kt, :], tOwn, ident_b[:], cmask[:, kt, :], (tConst, tAtc)))
                        for tp in range(t + 1):
                            for r_ in range(NCORES):
                                s = kvrot % NSL
                                kvrot += 1
                                visits.append(("load", s, r_, tp))
                                for kt in range(4):
                                    jblk = r_ * 8 + 2 * tp + kt // 2
                                    visits.append(("past", kring[s][:, kt * 128:(kt + 1) * 128], vring[s][:, kt, :], tKV[s],
                                                   emat[:, jblk, :], nmt, (tAtc, nmtt)))
                        comp = [v for v in visits if v[0] != "load"]
                        nvis = len(comp)
                        pend = []
                        ci = 0

                        def emit_pv(item, idx):
                            (vv, pt_i) = item
                            S.op("pe", lambda q: q.matmul(ps[3][:], lhsT=vv[2], rhs=pT[pt_i], start=(idx == 0), stop=(idx == nvis - 1)),
                                 r=(vv[3], tPT[pt_i]), w=(tPs[3],), inc=False)
                            S.op("pe", lambda q: q.matmul(ps[4][:], lhsT=ones_b[:], rhs=pT[pt_i], start=(idx == 0), stop=(idx == nvis - 1)),
                                 r=(tPT[pt_i], tConst), w=(tPs[4],), inc=True)

                        done = 0
                        for v in visits:
                            if v[0] == "load":
                                _, s, r_, tp = v
                                S.dma("sp", kring[s], kT_all.ap()[(r_ * NH + h) * 128:(r_ * NH + h + 1) * 128, tp * 512:(tp + 1) * 512],
                                      r=(dKa,), w=(tKV[s],), chan=tKV[s].chan)
                                base = (r_ * NH + h) * TOK + tp * 512
                                S.dma("sp", vring[s], V_all.ap()[base:base + 512, :].rearrange("(k p) d -> p k d", p=128),
                                      r=(dVa,), w=(tKV[s],), chan=tKV[s].chan)
                                continue
                            sb_i = srot % 3
                            srot += 1
                            pss, psst = ps[sb_i], tPs[sb_i]
                            S.op("pe", lambda q, v=v, pss=pss: q.matmul(pss[:], lhsT=v[1], rhs=qt[:, t * 512:(t + 1) * 512], start=True, stop=False),
                                 r=(v[3], qtt), w=(psst,), inc=False)
                            S.op("pe", lambda q, v=v, pss=pss: q.matmul(pss[:], lhsT=v[4], rhs=v[5], start=False, stop=True),
                                 r=v[6], w=(psst,), inc=True)
                            pi = prot % 4
                            prot += 1
                            S.op("act", lambda q, pss=pss, pi=pi: q.activation(out=pT[pi], in_=pss[:], func=AF.Exp, scale=SCALE),
                                 r=(psst,), w=(tPT[pi],))
                            pend.append((v, pi))
                            if len(pend) > 2:
                                emit_pv(pend.pop(0), done)
                                done += 1
                        while pend:
                            emit_pv(pend.pop(0), done)
                            done += 1
                        S.op("dve", lambda q: q.reciprocal(out=tmp_t[0][:], in_=ps[4][:]), r=(tPs[4],), w=(tTmp[0],))
                        S.op("dve", lambda q, h=h, t=t: q.tensor_tensor(out=bufA[:, h, t * 512:(t + 1) * 512], in0=ps[3][:], in1=tmp_t[0][:], op=ALU.mult),
                             r=(tPs[3], tTmp[0]), w=(tA,))
                if debug:
                    S.dma("sp", dbg["mixT"].ap().rearrange("j p n -> p j n"), bufA[:], r=(tA,), w=(dDbg,), chan=tA.chan)
                S.barrier()
                ckpt(5)
                for t in range(NT):
                    S.dma("sp", bufB1[:], xsrc[0].ap()[:, :, t * 512:(t + 1) * 512].rearrange("j p n -> p j n"), r=(dX,), w=(tB1,), chan=tB1.chan)
                    for blk in range(4):
                        wv, wtok = load_wblock(wb_out[l], blk * 512, 512, DC, l)
                        for jj in range(4):
                            j = blk * 4 + jj
                            pb, pt_ = next_ps(0, 6)
                            for k in range(DC):
                                S.op("pe", lambda q, k=k, jj=jj: q.matmul(pb[:], lhsT=wv[:, k, jj * 128:(jj + 1) * 128], rhs=bufA[:, k, t * 512:(t + 1) * 512],
                                                                          start=(k == 0), stop=(k == DC - 1)), r=(wtok, tA), w=(pt_,), inc=(k == DC - 1))
                            S.op("act", lambda q, pb=pb, j=j: q.activation(out=bufB0[:, j, :], in_=pb[:], func=AF.Identity), r=(pt_,), w=(tB0,))
                            sumsq_accum(bufB0[:, j, :], tB0, j == 0, j == DC - 1, ps[6], tPs[6])
                    post_residual(bufB0[:], tB0, bufB1[:], tB1, gp1)
                    S.dma("pool", xT_s.ap()[:, :, t * 512:(t + 1) * 512].rearrange("j p n -> p j n"), bufB1[:], r=(tB1,), w=(dX,), chan=tB1.chan)
                    h2v = bufB0[:].rearrange("p j n -> p (j n)").bitcast(BF16)[:, 0:DC * 512].rearrange("p (j n) -> p j n", j=DC)
                    prenorm(bufB1[:], tB1, gsc2, sh2, lambda j: h2v[:, j, :], tB0)
                    S.dma("pool", h2T_s.ap()[:, :, t * 512:(t + 1) * 512].rearrange("j p n -> p j n"), h2v, r=(tB0,), w=(dH2,), chan=tB0.chan)
                xsrc[0] = xT_s
                S.barrier()
                ckpt(6)
                S.dma("sp", bufA[:], h2T_s.ap().rearrange("j p n -> p j n"), r=(dH2,), w=(tA,), chan=tA.chan)
                for fb in range(FC // 4):
                    wg, wgt = load_wblock(wb_gate[l], fb * 512, 512, DC, l)
                    wu, wut = load_wblock(wb_up[l], fb * 512, 512, DC, l)
                    for jj in range(4):
                        f = fb * 4 + jj
                        st, stt = next_stage()
                        for t in range(NT):
                            pg, pgt = next_ps(0, 6)
                            proj_block(wg, wgt, jj, t, pg, pgt)
                            pu, put = next_ps(0, 6)
                            proj_block(wu, wut, jj, t, pu, put)
                            sg, sgt = tmp_t[t % 2], tTmp[t % 2]
                            S.op("act", lambda q, pg=pg, sg=sg: q.activation(out=sg[:], in_=pg[:], func=AF.Silu), r=(pgt,), w=(sgt,))
                            S.op("dve", lambda q, pu=pu, sg=sg, st=st, t=t: q.tensor_tensor(out=st[:, t * 512:(t + 1) * 512], in0=pu[:], in1=sg[:], op=ALU.mult),
                                 r=(put, sgt), w=(stt,))
                        S.dma("pool", actT_s.ap()[f], st, r=(stt,), w=(dAct,), chan=stt.chan)
                S.barrier()
                ckpt(7)
                actv = bufA[:].rearrange("p j n -> p (j n)")[:, 0:FC * 512].rearrange("p (f n) -> p f n", f=FC)
                for t in range(NT):
                    S.dma("sp", actv, actT_s.ap()[:, :, t * 512:(t + 1) * 512].rearrange("f p n -> p f n"), r=(dAct,), w=(tA,), chan=tA.chan)
                    S.dma("sp", bufB1[:], xT_s.ap()[:, :, t * 512:(t + 1) * 512].rearrange("j p n -> p j n"), r=(dX,), w=(tB1,), chan=tB1.chan)
                    for j in range(DC):
                        wi_ = wslot[0] % 3
                        wslot[0] += 1
                        S.dma("sp", wring[wi_][:, 0:FC * 128], wb_down[l].ap()[j], r=(dWb[l],), w=(tW[wi_],), chan=tW[wi_].chan)
                        wv, wtok = wring[wi_][:, 0:FC * 128].rearrange("p (k n) -> p k n", k=FC), tW[wi_]
                        pb, pt_ = next_ps(0, 6)
                        for k in range(FC):
                            S.op("pe", lambda q, k=k: q.matmul(pb[:], lhsT=wv[:, k, :], rhs=actv[:, k, :], start=(k == 0), stop=(k == FC - 1)),
                                 r=(wtok, tA), w=(pt_,), inc=(k == FC - 1))
                        S.op("act", lambda q, pb=pb, j=j: q.activation(out=bufB0[:, j, :], in_=pb[:], func=AF.Identity), r=(pt_,), w=(tB0,))
                        sumsq_accum(bufB0[:, j, :], tB0, j == 0, j == DC - 1, ps[6], tPs[6])
                    post_residual(bufB0[:], tB0, bufB1[:], tB1, gp2)
                    dst = outT if (kind == "B" or last) else xT_s
                    S.dma("pool", dst.ap()[:, :, t * 512:(t + 1) * 512].rearrange("j p n -> p j n"), bufB1[:], r=(tB1,),
                          w=(dOut if (kind == "B" or last) else dX,), chan=tB1.chan)
                S.barrier()
            S.barrier(full=True)

        def run_pass(live, q):
            S = Sched(nc, es, sems)
            qs = {n: (q if n == live else _DummyEng()) for n in ("pe", "act", "dve", "pool", "sp")}
            try:
                program(S, qs["pe"], qs["act"], qs["dve"], qs["pool"], qs["sp"])
            except _Stop:
                pass

        @block.tensor
        def _(q):
            run_pass("pe", q)

        @block.scalar
        def _(q):
            run_pass("act", q)

        @block.vector
        def _(q):
            run_pass("dve", q)

        @block.gpsimd
        def _(q):
            run_pass("pool", q)

        @block.sync
        def _(q):
            run_pass("sp", q)
    return nc


def _consts(core):
    ident = np.eye(128, dtype=np.float32)
    emat = np.zeros((64, 64, 128), np.float32)
    for n in range(64):
        emat[n, n, :] = MASKV
    k = np.arange(128)[:, None]
    q = np.arange(128)[None, :]
    tri = np.where(k <= q, 0.0, -MASKV).astype(np.float32)
    full = np.zeros((128, 128), np.float32)
    ninf = np.full((128, 128), -MASKV, np.float32)
    cm = np.stack([
        np.concatenate([tri, full, ninf, ninf], 1),
        np.concatenate([ninf, tri, ninf, ninf], 1),
        np.concatenate([ninf, ninf, tri, full], 1),
        np.concatenate([ninf, ninf, ninf, tri], 1)], 0)
    cmask = np.ascontiguousarray(cm.transpose(1, 0, 2)).reshape(128, 4 * 512)
    jj = np.arange(64)
    r_ = jj // 8
    lb = jj % 8
    nglob = 2 * (8 * (lb // 2) + r_) + (lb % 2)
    gmask = np.zeros((NT, 4, 64), np.float32)
    for t in range(NT):
        g = 8 * t + core
        for c_ in range(4):
            blk = 2 * g + (1 if c_ >= 2 else 0)
            gmask[t, c_, :] = np.where(nglob < blk, 0.0, NEG)
    gmask = np.broadcast_to(gmask.reshape(1, NT * 256), (128, NT * 256)).copy()
    hs = np.zeros((8,), np.float32)
    if core == 0:
        hs[7] = 1.0
    else:
        hs[core - 1] = 1.0
    hsel = np.broadcast_to(hs[None, :], (128, 8)).copy()
    return ident, emat.reshape(64, 64 * 128), cmask, gmask, hsel


def _fm(v):
    sh = v.shape
    v = v.reshape(sh[:-1] + (sh[-1] // 128, 128))
    return np.ascontiguousarray(np.moveaxis(v, -1, 0))


_CACHE = {}


def _prog(kind, debug=False, stop=None):
    key = (kind, debug, stop)
    if key not in _CACHE:
        _CACHE[key] = build_program(kind, debug, stop)
    return _CACHE[key]


def _kernel_unfused(x, c, ada_w, ada_b, mix_pre_g, mix_post_g, w_in, conv_w, conv_b, conv_ln_g, conv_ln_b, w_out,
                    ffn_pre_g, ffn_post_g, w_gate, w_up, w_down, _depth=L_FULL, _debug=False, _stop=None):
    L = _depth
    cores = list(range(NCORES))
    f32 = lambda a: np.ascontiguousarray(np.asarray(a, dtype=np.float32))
    x = f32(x).reshape(SEQ, D)
    ada_w = f32(ada_w)
    ada_b = f32(ada_b)
    dbg = {}
    cT = _fm(f32(c).reshape(D))
    in_maps = []
    for core in cores:
        aw = np.ascontiguousarray(ada_w[:, :, core * 1536:(core + 1) * 1536])
        ab = ada_b[:, core * 1536:(core + 1) * 1536].reshape(L_FULL, 12, 128)
        ab = np.ascontiguousarray(ab.transpose(2, 0, 1)).reshape(128, L_FULL * 12)
        in_maps.append({"cT": cT, "adaw": aw, "adab": ab})
    res = run_bass_kernel_spmd(_prog("M"), in_maps, core_ids=cores)
    modall = np.stack([np.asarray(res.results[r]["mod_out"]).reshape(128, L_FULL, 12) for r in cores], axis=1)
    xt = x.reshape(NT, NCORES, 512, D)
    xTs = [np.ascontiguousarray(xt[:, core].reshape(TOK, D).T).reshape(DC, 128, TOK) for core in cores]
    consts = [_consts(core) for core in cores]
    for l in range(L):
        modT = np.ascontiguousarray(modall[:, :, l, :]).reshape(128, NCORES * 12)
        vecs = np.stack([_fm(f32(v)[l]) for v in (mix_pre_g, mix_post_g, ffn_pre_g, ffn_post_g)], axis=1)
        vecs = np.ascontiguousarray(vecs).reshape(128, 4 * DC)
        cv = np.concatenate([f32(conv_w)[l], f32(conv_b)[l][None], f32(conv_ln_g)[l][None], f32(conv_ln_b)[l][None]], axis=0)
        cvec = np.ascontiguousarray(cv.reshape(34, 8, 128).transpose(2, 1, 0)).reshape(128, 8 * 34)
        wi = f32(w_in[l])
        in_maps = [{"xT_in": xTs[core], "modT": modT, "vecs": vecs, "w_in": wi} for core in cores]
        resA = run_bass_kernel_spmd(_prog("A", _debug), in_maps, core_ids=cores).results
        if _debug:
            dbg[("A", l)] = resA
        kT_all = np.concatenate([np.asarray(resA[r]["kT_loc"]) for r in cores], axis=0)
        V_all = np.concatenate([np.asarray(resA[r]["V_loc"]) for r in cores], axis=0)
        sm_all = np.concatenate([np.asarray(resA[r]["sm_loc"]) for r in cores], axis=0)
        wo, wg, wu, wd = f32(w_out[l]), f32(w_gate[l]), f32(w_up[l]), f32(w_down[l])
        in_maps = []
        for core in cores:
            ident, emat, cmask, gmask, hsel = consts[core]
            in_maps.append({"xT_in": xTs[core], "modT": modT, "vecs": vecs, "cvec": cvec,
                            "w_out": wo, "w_gate": wg, "w_up": wu, "w_down": wd,
                            "ident": ident, "emat": emat, "cmask": cmask, "gmask": gmask, "hsel": hsel,
                            "qT_s": np.asarray(resA[core]["qT_s"]), "kT_loc": np.asarray(resA[core]["kT_loc"]),
                            "V_loc": np.asarray(resA[core]["V_loc"]), "kT_all": kT_all, "V_all": V_all, "sm_all": sm_all,
                            "uT_s": np.asarray(resA[core]["uT_s"])})
        resB = run_bass_kernel_spmd(_prog("B", _debug, _stop), in_maps, core_ids=cores).results
        if _debug:
            dbg[("B", l)] = resB
        xTs = [np.asarray(resB[core]["outT"]).reshape(DC, 128, TOK) for core in cores]
    out = np.empty((NT, NCORES, 512, D), np.float32)
    for core in cores:
        out[:, core] = xTs[core].reshape(D, TOK).T.reshape(NT, 512, D)
    if _debug:
        _kernel_unfused._last = dbg
        _kernel_unfused._mod = modall
    return out.reshape(1, SEQ, D)


def _kernel_fused(x, c, ada_w, ada_b, mix_pre_g, mix_post_g, w_in, conv_w, conv_b, conv_ln_g, conv_ln_b, w_out,
                  ffn_pre_g, ffn_post_g, w_gate, w_up, w_down):
    L = L_FULL
    cores = list(range(NCORES))
    f32 = lambda a: np.ascontiguousarray(np.asarray(a, dtype=np.float32))
    x = f32(x).reshape(SEQ, D)
    ada_w = f32(ada_w)
    ada_b = f32(ada_b)
    cT = _fm(f32(c).reshape(D))
    vecs = np.stack([np.stack([_fm(f32(v)[l]) for v in (mix_pre_g, mix_post_g, ffn_pre_g, ffn_post_g)], axis=1).reshape(128, 4 * DC)
                     for l in range(L)], axis=0)
    cvs = []
    for l in range(L):
        cv = np.concatenate([f32(conv_w)[l], f32(conv_b)[l][None], f32(conv_ln_g)[l][None], f32(conv_ln_b)[l][None]], axis=0)
        cvs.append(np.ascontiguousarray(cv.reshape(34, 8, 128).transpose(2, 1, 0)).reshape(128, 8 * 34))
    cvec = np.stack(cvs, axis=0)
    shared = {"cT": cT, "vecs": np.ascontiguousarray(vecs), "cvec": np.ascontiguousarray(cvec),
              "w_in": f32(w_in), "w_out": f32(w_out), "w_gate": f32(w_gate), "w_up": f32(w_up), "w_down": f32(w_down)}
    xt = x.reshape(NT, NCORES, 512, D)
    in_maps = []
    for core in cores:
        ident, emat, cmask, gmask, hsel = _consts(core)
        xT = np.ascontiguousarray(xt[:, core].reshape(TOK, D).T).reshape(DC, 128, TOK)
        aw = np.ascontiguousarray(ada_w[:, :, core * 1536:(core + 1) * 1536])
        ab = ada_b[:, core * 1536:(core + 1) * 1536].reshape(L, 12, 128)
        ab = np.ascontiguousarray(ab.transpose(2, 0, 1)).reshape(128, L * 12)
        m = dict(shared)
        m.update({"xT_in": xT, "adaw": aw, "adab": ab, "ident": ident, "emat": emat, "cmask": cmask, "gmask": gmask, "hsel": hsel})
        in_maps.append(m)
    res = run_bass_kernel_spmd(_prog("F"), in_maps, core_ids=cores)
    out = np.empty((NT, NCORES, 512, D), np.float32)
    for core in cores:
        oT = np.asarray(res.results[core]["outT"]).reshape(D, TOK)
        out[:, core] = oT.T.reshape(NT, 512, D)
    return out.reshape(1, SEQ, D)


def kernel(x, c, ada_w, ada_b, mix_pre_g, mix_post_g, w_in, conv_w, conv_b, conv_ln_g, conv_ln_b, w_out,
           ffn_pre_g, ffn_post_g, w_gate, w_up, w_down):
    return _kernel_unfused(x, c, ada_w, ada_b, mix_pre_g, mix_post_g, w_in, conv_w, conv_b, conv_ln_g, conv_ln_b, w_out,
                           ffn_pre_g, ffn_post_g, w_gate, w_up, w_down)
```

```python
import os
import numpy as np
from contextlib import ExitStack
import concourse.bass as bass
import concourse.mybir as mybir
from concourse.bass_utils import run_bass_kernel_spmd

F32 = mybir.dt.float32
BF16 = mybir.dt.bfloat16
AF = mybir.ActivationFunctionType
ALU = mybir.AluOpType
AX = mybir.AxisListType

NCORES = 8
D = 2048
SEQ = 16384
L_FULL = 4
TOK = SEQ // NCORES
NT = 4
DC = D // 128
DFF = 5632
FC = DFF // 128
INC = 5120
CONVK = 31
HALO = 30
NH = 8
RMS_EPS = 1e-6
LN_EPS = 1e-5
SCALE = 128 ** -0.5
MASKV = 30000.0
NEG = -1e30


class Tok:
    __slots__ = ("name", "w", "r", "chan")

    def __init__(self, name):
        self.name = name
        self.w = {}
        self.r = {}
        self.chan = None


class Chan:
    __slots__ = ("sem", "val")

    def __init__(self, sem):
        self.sem = sem
        self.val = 0


class Eng:
    def __init__(self, name, q, sem):
        self.name = name
        self.q = q
        self.sem = sem
        self.cnt = 0
        self.seen = {}
        self.pr = []
        self.pw = []


class _DummyIns:
    def then_inc(self, *a, **k):
        return self


class _DummyEng:
    def __getattr__(self, name):
        def f(*a, **k):
            return _DummyIns()
        return f


class Sched:
    def __init__(self, nc, es, sems):
        self.nc = nc
        self.es = es
        self.engs = {}
        self.chans = []
        self.allchans = []
        self.nsem = 0
        self.sems = sems

    def new_sem(self, name):
        i = self.nsem
        self.nsem += 1
        if i >= len(self.sems):
            self.sems.append(self.es.enter_context(self.nc.semaphore(name)))
        return self.sems[i]

    def add_engine(self, name, q):
        e = Eng(name, q, self.new_sem("prog_" + name))
        self.engs[name] = e
        return e

    def chan(self, name, barrier=True):
        c = Chan(self.new_sem("ch_" + name))
        self.allchans.append(c)
        if barrier:
            self.chans.append(c)
        return c

    def tok(self, name, chan=False):
        t = Tok(name)
        if chan:
            t.chan = self.chan(name)
        return t

    @staticmethod
    def _merge(d, ev):
        s, v = ev
        k = id(s)
        if k not in d or d[k][1] < v:
            d[k] = (s, v)

    def _wait(self, e, ev):
        s, v = ev
        if s is e.sem and e.name in ("pe", "sp"):
            return
        k = id(s)
        if e.seen.get(k, 0) >= v:
            return
        e.q.wait_ge(s, v)
        e.seen[k] = v

    def _deps(self, e, reads, writes):
        for t in reads:
            for ev in t.w.values():
                self._wait(e, ev)
        for t in writes:
            for ev in t.w.values():
                self._wait(e, ev)
            for ev in t.r.values():
                self._wait(e, ev)

    def _commit(self, ev, reads, writes):
        for t in reads:
            self._merge(t.r, ev)
        for t in writes:
            t.w = {}
            t.r = {}
            self._merge(t.w, ev)

    def op(self, ename, fn, r=(), w=(), inc=True):
        e = self.engs[ename]
        self._deps(e, r, w)
        ins = fn(e.q)
        if inc:
            e.cnt += 1
            ins.then_inc(e.sem, 1)
            ev = (e.sem, e.cnt)
            self._commit(ev, list(r) + e.pr, list(w) + e.pw)
            e.pr = []
            e.pw = []
        else:
            e.pr += list(r)
            e.pw += list(w)
        return ins

    def dma(self, ename, out, in_, r=(), w=(), chan=None):
        e = self.engs[ename]
        self._deps(e, r, w)
        chan.val += 16
        e.q.dma_start(out=out, in_=in_).then_inc(chan.sem, 16)
        ev = (chan.sem, chan.val)
        self._commit(ev, r, w)
        return ev

    def collective(self, ename, kind, ins, outs, r=(), w=(), chan=None):
        e = self.engs[ename]
        self._deps(e, r, w)
        chan.val += 1
        e.q.collective_compute(kind, ALU.bypass, replica_groups=[list(range(NCORES))],
                               ins=ins, outs=outs).then_inc(chan.sem, 1)
        ev = (chan.sem, chan.val)
        self._commit(ev, r, w)
        return ev

    def barrier(self, full=False):
        evs = [(e.sem, e.cnt) for e in self.engs.values() if e.cnt > 0]
        evs += [(c.sem, c.val) for c in (self.allchans if full else self.chans) if c.val > 0]
        for e in self.engs.values():
            for ev in evs:
                self._wait(e, ev)


class _Stop(Exception):
    pass


def build_program(kind, debug=False, stop=None):
    nc = bass.Bass("TRN2", target_bir_lowering=False)
    L = 1

    def din(name, shape, dt=F32):
        return nc.dram_tensor(name, list(shape), dt, kind="ExternalInput")

    def dout(name, shape, dt=F32):
        return nc.dram_tensor(name, list(shape), dt, kind="ExternalOutput")

    def dsc(name, shape, dt):
        return nc.dram_tensor(name, list(shape), dt)

    LM = L_FULL
    if kind == "F":
        L = L_FULL
        xT_in = din("xT_in", [DC, 128, TOK])
        cT_in = din("cT", [128, DC])
        adaw_in = din("adaw", [LM, D, 1536])
        adab_in = din("adab", [128, LM * 12])
        vecs_in = din("vecs", [L, 128, 4 * DC])
        cvec_in = din("cvec", [L, 128, 8 * 34])
        w_in_d = din("w_in", [L, D, INC])
        w_out_d = din("w_out", [L, D, D])
        w_gate_d = din("w_gate", [L, D, DFF])
        w_up_d = din("w_up", [L, D, DFF])
        w_down_d = din("w_down", [L, DFF, D])
        ident_in = din("ident", [128, 128])
        emat_in = din("emat", [64, 64 * 128])
        cmask_in = din("cmask", [128, 4 * 512])
        gmask_in = din("gmask", [128, NT * 256])
        hsel_in = din("hsel", [128, 8])
        outT = dout("outT", [DC, 128, TOK])
        xT_s = dsc("xT_s", [DC, 128, TOK], F32)
        qT_s = dsc("qT_s", [NH, 128, TOK], BF16)
        kT_loc = dsc("kT_loc", [NH * 128, TOK], BF16)
        kT_all = dsc("kT_all", [NCORES * NH * 128, TOK], BF16)
        V_loc = dsc("V_loc", [NH * TOK, 128], BF16)
        V_all = dsc("V_all", [NCORES * NH * TOK, 128], BF16)
        sm_loc = dsc("sm_loc", [128, 1024], BF16)
        sm_all = dsc("sm_all", [NCORES * 128, 1024], BF16)
        mod_loc = dsc("mod_loc", [128, 512], F32)
        mod_all = dsc("mod_all", [NCORES * 128, 512], F32)
        uT_s = dsc("uT_s", [8, 128, TOK], BF16)
        h2T_s = dsc("h2T_s", [DC, 128, TOK], BF16)
        actT_s = dsc("actT_s", [FC, 128, TOK], BF16)
        wb_in = [dsc(f"wb_in{l}", [D, INC], BF16) for l in range(L)]
        wb_out = [dsc(f"wb_out{l}", [D, D], BF16) for l in range(L)]
        wb_gate = [dsc(f"wb_gate{l}", [D, DFF], BF16) for l in range(L)]
        wb_up = [dsc(f"wb_up{l}", [D, DFF], BF16) for l in range(L)]
        wb_down = [dsc(f"wb_down{l}", [DC, 128, FC * 128], BF16) for l in range(L)]
    elif kind == "M":
        cT_in = din("cT", [128, DC])
        adaw_in = din("adaw", [LM, D, 1536])
        adab_in = din("adab", [128, LM * 12])
        mod_out = dout("mod_out", [128, LM * 12])
    else:
        xT_in = din("xT_in", [DC, 128, TOK])
        modT_in = din("modT", [128, NCORES * 12])
        vecs_in = din("vecs", [128, 4 * DC])
    if kind == "A":
        w_in_d = din("w_in", [D, INC])
        qT_s = dout("qT_s", [NH, 128, TOK], BF16)
        kT_loc = dout("kT_loc", [NH * 128, TOK], BF16)
        V_loc = dout("V_loc", [NH * TOK, 128], BF16)
        sm_loc = dout("sm_loc", [128, 1024], BF16)
        uT_s = dout("uT_s", [8, 128, TOK], BF16)
        wb_in = [dsc("wb_in0", [D, INC], BF16)]
    if kind == "B":
        cvec_in = din("cvec", [128, 8 * 34])
        w_out_d = din("w_out", [D, D])
        w_gate_d = din("w_gate", [D, DFF])
        w_up_d = din("w_up", [D, DFF])
        w_down_d = din("w_down", [DFF, D])
        ident_in = din("ident", [128, 128])
        emat_in = din("emat", [64, 64 * 128])
        cmask_in = din("cmask", [128, 4 * 512])
        gmask_in = din("gmask", [128, NT * 256])
        hsel_in = din("hsel", [128, 8])
        qT_s = din("qT_s", [NH, 128, TOK], BF16)
        kT_loc = din("kT_loc", [NH * 128, TOK], BF16)
        V_loc = din("V_loc", [NH * TOK, 128], BF16)
        kT_all = din("kT_all", [NCORES * NH * 128, TOK], BF16)
        V_all = din("V_all", [NCORES * NH * TOK, 128], BF16)
        sm_all = din("sm_all", [NCORES * 128, 1024], BF16)
        uT_s = din("uT_s", [8, 128, TOK], BF16)
        outT = dout("outT", [DC, 128, TOK])
        xT_s = dsc("xT_s", [DC, 128, TOK], F32)
        h2T_s = dsc("h2T_s", [DC, 128, TOK], BF16)
        actT_s = dsc("actT_s", [FC, 128, TOK], BF16)
        wb_out = [dsc("wb_out0", [D, D], BF16)]
        wb_gate = [dsc("wb_gate0", [D, DFF], BF16)]
        wb_up = [dsc("wb_up0", [D, DFF], BF16)]
        wb_down = [dsc("wb_down0", [DC, 128, FC * 128], BF16)]
    dbg = {}
    if debug and kind == "B":
        dbg["mixT"] = dout("dbg_mixT", [DC, 128, TOK], BF16)
    if debug and kind == "A":
        dbg["hT"] = dout("dbg_hT", [DC, 128, TOK], BF16)

    with ExitStack() as es:
        def sb(name, shape, dt):
            return es.enter_context(nc.sbuf_tensor("s_" + name, list(shape), dt))

        def pst(name, shape, dt):
            return es.enter_context(nc.psum_tensor("p_" + name, list(shape), dt))

        bufA = sb("bufA", [128, DC, TOK], BF16)
        bufB0 = sb("bufB0", [128, DC, 512], F32)
        bufB1 = sb("bufB1", [128, DC, 512], F32)
        wring = [sb(f"wring{i}", [128, 8192], BF16) for i in range(3)]
        b0b = bufB0[:].rearrange("p j n -> p (j n)").bitcast(BF16)
        b1b = bufB1[:].rearrange("p j n -> p (j n)").bitcast(BF16)
        b0f = bufB0[:].rearrange("p j n -> p (j n)")
        b1f = bufB1[:].rearrange("p j n -> p (j n)")
        stage = [b0b[:, i * TOK:(i + 1) * TOK] for i in range(3)]
        emat = b0b[0:64, 0:8192].rearrange("p (a b) -> p a b", a=64)
        cmask = b0b[:, 8192:10240].rearrange("p (a b) -> p a b", a=4)
        gmask = b0f[:, 5120:6144].rearrange("p (a b) -> p a b", a=NT)
        qTh = [b0b[:, 12288 + i * TOK:12288 + (i + 1) * TOK] for i in range(2)]
        pT = [b1b[:, i * 512:(i + 1) * 512] for i in range(4)]
        nmT = [b1b[0:64, 2048 + i * 512:2048 + (i + 1) * 512] for i in range(2)]
        kown = b1b[:, 3072:3584]
        vown = b1b[:, 3584:4096].rearrange("p (k d) -> p k d", k=4)
        gm = b1f[:, 2048:2304].rearrange("p (a b) -> p a b", a=4)
        max8 = b1f[:, 2304:2336].rearrange("p (a b) -> p a b", a=4)
        thr = b1f[:, 2336:2340]
        nm = b1b[:, 4736:4992].rearrange("p (a b) -> p a b", a=4)
        kmh = b1b[:, 4992:5056]
        hall = wring[0][:, 0:NCORES * 960].rearrange("p (r n) -> p r n", r=NCORES)
        dwm = wring[1][:, 0:CONVK * 128].rearrange("p (j n) -> p j n", j=CONVK)
        upad = [wring[1][:, 4096 + i * 544:4096 + i * 544 + HALO + 512] for i in range(2)]
        halo_sel = wring[1][:, 5632:5632 + NT * 240].rearrange("p (t n) -> p t n", t=NT)
        sqr = [sb(f"sqr{i}", [128, 512], F32) for i in range(3)]
        rstd_t = sb("rstd", [128, 512], F32)
        mean_t = sb("mean", [128, 512], F32)
        tmp_t = [sb(f"tmpf{i}", [128, 512], F32) for i in range(2)]
        ident_f = sb("ident_f", [128, 128], F32)
        ident_b = sb("ident_b", [128, 128], BF16)
        ones_f = sb("ones_f", [128, 128], F32)
        ones_b = sb("ones_b", [128, 128], BF16)
        hsel = sb("hsel", [128, 8], F32)
        epsr = sb("epsr", [128, 1], F32)
        epsl = sb("epsl", [128, 1], F32)
        cT = sb("cT", [128, DC], F32)
        cact = sb("cact", [128, DC], F32)
        adab = sb("adab", [128, LM * 12], F32)
        modp = sb("modp", [128, LM * 12], F32)
        modT = sb("modT", [128, NCORES, LM * 12 if kind == "F" else 12], F32)
        vecs = sb("vecs", [128, 1, 4, DC], F32)
        cvec = sb("cvec", [128, 1, 8, 34], F32)
        gsc1 = sb("gsc1", [128, DC], F32)
        gsc2 = sb("gsc2", [128, DC], F32)
        gp1 = sb("gp1", [128, DC], F32)
        gp2 = sb("gp2", [128, DC], F32)
        sh1 = sb("sh1", [128, DC], F32)
        sh2 = sb("sh2", [128, DC], F32)
        kms = sb("kms", [128, NH, 8], F32)
        smst = sb("smst", [128, 1024], BF16)
        kmT = sb("kmT", [128, NCORES, 64], BF16)

        ps = [pst(f"ps{i}", [128, 512], F32) for i in range(7)]
        psb = pst("psb", [64, 512], BF16)

        sems = []
        block = es.enter_context(nc.Block())

        def program(S, pe, act, dve, pool, sp):
            S.add_engine("pe", pe)
            S.add_engine("act", act)
            S.add_engine("dve", dve)
            S.add_engine("pool", pool)
            S.add_engine("sp", sp)

            T = {}

            def tk(name, chan=False):
                if name not in T:
                    T[name] = S.tok(name, chan)
                return T[name]

            tA = tk("bufA", True)
            tB0 = tk("bufB0", True)
            tB1 = tk("bufB1", True)
            tW = [tk(f"wring{i}", True) for i in range(3)]
            tSt = [tk(f"stage{i}", True) for i in range(3)]
            tSq = [tk(f"sqr{i}") for i in range(3)]
            tPs = [tk(f"ps{i}") for i in range(7)]
            tPsb = tk("psb")
            tRstd = tk("rstd")
            tMean = tk("mean")
            tTmp = [tk("tmpf0"), tk("tmpf1")]
            tConst = tk("const", True)
            tMisc = tk("misc", True)
            tSm = tk("smst", True)
            tHall = tk("hall", True)
            tUp = [tk("upad0", True), tk("upad1", True)]
            tDw = tk("dwm")
            tNmT = [tk("nmT0"), tk("nmT1")]
            tPT = [tk(f"pT{i}") for i in range(4)]
            tQ = [tk("qTh0", True), tk("qTh1", True)]
            tOwn = tk("own", True)
            tGate = tk("gate")
            tKms = tk("kms")
            tAtc = tk("attc", True)
            dX = tk("d_xT")
            dQ = tk("d_qT")
            dKl = tk("d_kTloc")
            dKa = tk("d_kTall")
            dVl = tk("d_Vloc")
            dVa = tk("d_Vall")
            dSl = tk("d_smloc")
            dSa = tk("d_small")
            dU = tk("d_uT")
            dH2 = tk("d_h2T")
            dAct = tk("d_actT")
            dWb = [tk(f"d_wb{l}") for l in range(L)]
            dMl = tk("d_modloc")
            dMa = tk("d_modall")
            dOut = tk("d_out")
            dDbg = tk("d_dbg")
            chCast = S.chan("cast", barrier=False)
            chCC = S.chan("cc")

            def cast_weights(l):
                if kind == "A":
                    lst = ((w_in_d, wb_in, D),)
                elif kind == "B":
                    lst = ((w_out_d, wb_out, D), (w_gate_d, wb_gate, D), (w_up_d, wb_up, D), (w_down_d, wb_down, DFF))
                else:
                    lst = ((w_in_d, wb_in, D), (w_out_d, wb_out, D), (w_gate_d, wb_gate, D), (w_up_d, wb_up, D), (w_down_d, wb_down, DFF))
                for (src, dst, rows) in lst:
                    nsp = 8
                    rs = rows // nsp
                    sap = src.ap()[l] if kind == "F" else src.ap()
                    if rows == DFF:
                        for j in range(DC):
                            for kq in range(4):
                                k0, k1 = kq * 11, (kq + 1) * 11
                                S.dma("pool", dst[l].ap()[j][:, k0 * 128:k1 * 128].rearrange("p (k n) -> p k n", n=128),
                                      sap[k0 * 128:k1 * 128, j * 128:(j + 1) * 128].rearrange("(k p) n -> p k n", p=128),
                                      r=(), w=(dWb[l],), chan=chCast)
                        continue
                    for i in range(nsp):
                        S.dma("pool", dst[l].ap()[i * rs:(i + 1) * rs, :], sap[i * rs:(i + 1) * rs, :],
                              r=(), w=(dWb[l],), chan=chCast)

            wslot = [0]

            def load_wblock(wdram, c0, ncols, kch, l):
                i = wslot[0] % 3
                wslot[0] += 1
                view = wring[i][:, 0:kch * ncols].rearrange("p (k n) -> p k n", k=kch)
                S.dma("sp", view, wdram.ap()[:, c0:c0 + ncols].rearrange("(k p) n -> p k n", p=128),
                      r=(dWb[l],), w=(tW[i],), chan=tW[i].chan)
                return view, tW[i]

            psrot = [0]

            def next_ps(lo=0, hi=7):
                i = lo + psrot[0] % (hi - lo)
                psrot[0] += 1
                return ps[i], tPs[i]

            sqrot = [0]
            strot = [0]

            def next_stage():
                i = strot[0] % 3
                strot[0] += 1
                return stage[i], tSt[i]

            evrot = [0]

            def evac_copy(out, in_, r, w):
                evrot[0] += 1
                if evrot[0] % 2:
                    S.op("act", lambda q: q.activation(out=out, in_=in_, func=AF.Identity), r=r, w=w)
                else:
                    S.op("dve", lambda q: q.tensor_copy(out=out, in_=in_), r=r, w=w)

            def sumsq_accum(src_ap, src_tok, first, last, acc_ps, acc_tok):
                i = sqrot[0] % 3
                sqrot[0] += 1
                S.op("act", lambda q: q.activation(out=sqr[i][:], in_=src_ap, func=AF.Square), r=(src_tok,), w=(tSq[i],))
                S.op("pe", lambda q: q.matmul(acc_ps[:], lhsT=ones_f[:], rhs=sqr[i][:], start=first, stop=last),
                     r=(tSq[i], tConst), w=(acc_tok,), inc=True)

            def rstd_from(acc_ps, acc_tok, n, eps):
                ept = epsr if eps == RMS_EPS else epsl
                S.op("act", lambda q: q.activation(out=rstd_t[:], in_=acc_ps[:], func=AF.Sqrt, bias=ept[:, 0:1], scale=1.0 / n),
                     r=(acc_tok, tConst), w=(tRstd,))
                S.op("dve", lambda q: q.reciprocal(out=rstd_t[:], in_=rstd_t[:]), r=(tRstd,), w=(tRstd,))

            def prenorm(xt, xtok, gsc, shv, dst_fn, dst_tok):
                acc, acct = ps[6], tPs[6]
                for j in range(DC):
                    sumsq_accum(xt[:, j, :], xtok, j == 0, j == DC - 1, acc, acct)
                rstd_from(acc, acct, D, RMS_EPS)
                S.op("dve", lambda q: q.tensor_tensor(out=xt, in0=xt, in1=rstd_t[:].unsqueeze(1).to_broadcast([128, DC, 512]),
                                                       op=ALU.mult), r=(xtok, tRstd), w=(xtok,))
                for j in range(DC):
                    S.op("act", lambda q, j=j: q.activation(out=dst_fn(j), in_=xt[:, j, :], func=AF.Identity,
                                                             bias=shv[:, j:j + 1], scale=gsc[:, j:j + 1]),
                         r=(xtok, tMisc), w=(dst_tok,))

            def post_residual(yt, ytok, xt, xtok, gp):
                rstd_from(ps[6], tPs[6], D, RMS_EPS)
                for j in range(DC):
                    S.op("dve", lambda q, j=j: q.scalar_tensor_tensor(out=yt[:, j, :], in0=yt[:, j, :], scalar=gp[:, j:j + 1],
                                                                      in1=rstd_t[:], op0=ALU.mult, op1=ALU.mult),
                         r=(ytok, tRstd, tMisc), w=(ytok,))
                S.op("dve", lambda q: q.tensor_tensor(out=xt, in0=xt, in1=yt, op=ALU.add), r=(xtok, ytok), w=(xtok,))

            cc = tConst.chan
            S.op("dve", lambda q: q.memset(ones_f[:], 1.0), w=(tConst,))
            S.op("dve", lambda q: q.memset(ones_b[:], 1.0), w=(tConst,))
            S.op("dve", lambda q: q.memset(epsr[:], RMS_EPS), w=(tConst,))
            S.op("dve", lambda q: q.memset(epsl[:], LN_EPS), w=(tConst,))
            if kind == "F":
                cast_weights(0)
            if kind in ("M", "F"):
                S.dma("sp", cT[:], cT_in.ap(), w=(tConst,), chan=cc)
                S.dma("sp", adab[:], adab_in.ap(), w=(tConst,), chan=cc)
                S.op("act", lambda q: q.activation(out=cact[:], in_=cT[:], func=AF.Silu), r=(tConst,), w=(tMisc,))
                for l in range(LM):
                    for half in range(3):
                        buf, bt = (bufB0, tB0) if (l * 3 + half) % 2 == 0 else (bufB1, tB1)
                        S.dma("sp", buf[:], adaw_in.ap()[l, :, half * 512:(half + 1) * 512].rearrange("(k p) n -> p k n", p=128),
                              w=(bt,), chan=bt.chan)
                        for mm in range(4):
                            col = l * 12 + half * 4 + mm
                            for k in range(DC):
                                S.op("pe", lambda q, k=k, mm=mm, col=col, buf=buf: q.matmul(
                                    ps[0][:, col:col + 1], lhsT=buf[:, k, mm * 128:(mm + 1) * 128], rhs=cact[:, k:k + 1],
                                    start=(k == 0), stop=(k == DC - 1)), r=(bt, tMisc), w=(tPs[0],), inc=(k == DC - 1))
                S.op("dve", lambda q: q.tensor_tensor(out=modp[:], in0=ps[0][:, 0:LM * 12], in1=adab[:], op=ALU.add),
                     r=(tPs[0], tConst), w=(tMisc,))
                if kind == "M":
                    S.dma("sp", mod_out.ap(), modp[:], r=(tMisc,), w=(dOut,), chan=tMisc.chan)
                    S.barrier(full=True)
                    return
                S.dma("sp", mod_loc.ap()[:, 0:LM * 12], modp[:], r=(tMisc,), w=(dMl,), chan=tMisc.chan)
                S.collective("pool", "AllGather", [mod_loc.ap()], [mod_all.ap()], r=(dMl,), w=(dMa,), chan=chCC)
                S.dma("sp", modT[:], mod_all.ap()[:, 0:LM * 12].rearrange("(r p) n -> p r n", p=128), r=(dMa,), w=(tMisc,), chan=tMisc.chan)
                S.barrier()
            xsrc = [xT_in]
            if kind != "F":
                S.dma("sp", modT[:].rearrange("p r n -> p (r n)"), modT_in.ap(), w=(tMisc,), chan=tMisc.chan)
                S.dma("sp", vecs[:].rearrange("p a b c -> p (a b c)"), vecs_in.ap(), w=(tConst,), chan=cc)
            if kind in ("B", "F"):
                S.dma("sp", ident_f[:], ident_in.ap(), w=(tConst,), chan=cc)
                S.dma("pool", ident_b[:], ident_in.ap(), w=(tConst,), chan=cc)
                S.dma("sp", hsel[:], hsel_in.ap(), w=(tConst,), chan=cc)
                if kind == "B":
                    S.dma("sp", cvec[:].rearrange("p a b c -> p (a b c)"), cvec_in.ap(), w=(tConst,), chan=cc)
            if kind != "F":
                cast_weights(0)

            def ckpt(k):
                if stop == k:
                    S.barrier(full=True)
                    raise _Stop()

            ckpt(0)

            def mod_chunk(l, m):
                r_, mm = divmod(m, 12)
                if kind == "F":
                    return modT[:, r_, l * 12 + mm:l * 12 + mm + 1]
                return modT[:, r_, mm:mm + 1]

            def layer_vectors(l):
                for j in range(DC):
                    for (dst, gidx, scm, shdst, shm, gpd, gm_, pidx) in (
                            (gsc1, 0, 16 + j, sh1, 0 + j, gp1, 32 + j, 1),
                            (gsc2, 2, 64 + j, sh2, 48 + j, gp2, 80 + j, 3)):
                        S.op("dve", lambda q, dst=dst, gidx=gidx, scm=scm: q.scalar_tensor_tensor(
                            out=dst[:, j:j + 1], in0=mod_chunk(l, scm), scalar=1.0, in1=vecs[:, 0, gidx, j:j + 1],
                            op0=ALU.add, op1=ALU.mult), r=(tMisc, tConst), w=(tMisc,))
                        S.op("dve", lambda q, shdst=shdst, shm=shm: q.tensor_copy(out=shdst[:, j:j + 1], in_=mod_chunk(l, shm)),
                             r=(tMisc,), w=(tMisc,))
                        S.op("dve", lambda q, gpd=gpd, gm_=gm_, pidx=pidx: q.tensor_tensor(
                            out=gpd[:, j:j + 1], in0=mod_chunk(l, gm_), in1=vecs[:, 0, pidx, j:j + 1], op=ALU.mult),
                            r=(tMisc, tConst), w=(tMisc,))

            def proj_block(wv, wtok, jj, t, pbank, ptok):
                for k in range(DC):
                    S.op("pe", lambda q, k=k: q.matmul(pbank[:], lhsT=wv[:, k, jj * 128:(jj + 1) * 128],
                                                        rhs=bufA[:, k, t * 512:(t + 1) * 512],
                                                        start=(k == 0), stop=(k == DC - 1)),
                         r=(wtok, tA), w=(ptok,), inc=(k == DC - 1))


            def ph2(l):
                S.op("dve", lambda q: q.memset(smst[:], 0.0), w=(tSm,))
                S.op("dve", lambda q: q.memset(kms[:].rearrange("p a b -> p (a b)"), 0.0), w=(tKms,))

                for blk in range(4):
                    wv, wtok = load_wblock(wb_in[l], blk * 512, 512, DC, l)
                    for jj in range(4):
                        ch = blk * 4 + jj
                        st, stt = next_stage()
                        for t in range(NT):
                            pb, pt_ = next_ps(0, 6)
                            proj_block(wv, wtok, jj, t, pb, pt_)
                            if ch >= 8:
                                h = ch - 8
                                for b2 in range(2):
                                    S.op("act", lambda q, pb=pb, st=st, t=t, b2=b2, h=h: q.activation(
                                        out=st[:, t * 512 + b2 * 256:t * 512 + (b2 + 1) * 256], in_=pb[:, b2 * 256:(b2 + 1) * 256],
                                        func=AF.Identity, accum_out=kms[:, h, 2 * t + b2:2 * t + b2 + 1]), r=(pt_,), w=(stt, tKms))
                            else:
                                S.op("act", lambda q, pb=pb, st=st, t=t: q.activation(out=st[:, t * 512:(t + 1) * 512], in_=pb[:], func=AF.Identity),
                                     r=(pt_,), w=(stt,))
                        if os.environ.get("KSKIP_ST"):
                            pass
                        elif ch < 8:
                            S.dma("pool", qT_s.ap()[ch], st, r=(stt,), w=(dQ,), chan=stt.chan)
                        else:
                            S.dma("pool", kT_loc.ap()[(ch - 8) * 128:(ch - 7) * 128, :], st, r=(stt,), w=(dKl,), chan=stt.chan)
                S.op("dve", lambda q: q.tensor_copy(out=smst[:, 0:64], in_=kms[:].rearrange("p a b -> p (a b)")),
                     r=(tKms,), w=(tSm,))
                ckpt(10)
                for bv in range(2):
                    wv, wtok = load_wblock(wb_in[l], 2048 + bv * 512, 512, DC, l)
                    for tc4 in range(4):
                        st, stt = next_stage()
                        for tcc in range(4):
                            tc = tc4 * 4 + tcc
                            pb, pt_ = next_ps(0, 6)
                            for k in range(DC):
                                S.op("pe", lambda q, k=k, tc=tc: q.matmul(pb[:], lhsT=bufA[:, k, tc * 128:(tc + 1) * 128],
                                                                           rhs=wv[:, k, :], start=(k == 0), stop=(k == DC - 1)),
                                     r=(wtok, tA), w=(pt_,), inc=(k == DC - 1))
                            evac_copy(st[:, tcc * 512:(tcc + 1) * 512], pb[:], (pt_,), (stt,))
                        for hh in range(4):
                            dst = V_loc.ap().rearrange("(h c p) d -> p c h d", h=NH, p=128)[:, tc4 * 4:(tc4 + 1) * 4, bv * 4 + hh, :]
                            S.dma("pool", dst, st.rearrange("p (c h d) -> p c h d", c=4, h=4)[:, :, hh, :], r=(stt,), w=(dVl,), chan=stt.chan)
                ckpt(11)
                for bb in range(2):
                    wa, wat = load_wblock(wb_in[l], 3072 + bb * 512, 512, DC, l)
                    wg, wgt = load_wblock(wb_in[l], 4096 + bb * 512, 512, DC, l)
                    for jj in range(4):
                        i = bb * 4 + jj
                        st, stt = next_stage()
                        for t in range(NT):
                            pg, pgt = next_ps(0, 6)
                            proj_block(wg, wgt, jj, t, pg, pgt)
                            pa, pat = next_ps(0, 6)
                            proj_block(wa, wat, jj, t, pa, pat)
                            sg, sgt = tmp_t[t % 2], tTmp[t % 2]
                            S.op("act", lambda q, pg=pg, sg=sg: q.activation(out=sg[:], in_=pg[:], func=AF.Sigmoid), r=(pgt,), w=(sgt,))
                            S.op("dve", lambda q, pa=pa, sg=sg, st=st, t=t: q.tensor_tensor(
                                out=st[:, t * 512:(t + 1) * 512], in0=pa[:], in1=sg[:], op=ALU.mult), r=(pat, sgt), w=(stt,))
                            S.op("dve", lambda q, st=st, t=t, i=i: q.tensor_copy(
                                out=smst[:, 64 + (t * 8 + i) * HALO:64 + (t * 8 + i + 1) * HALO],
                                in_=st[:, t * 512 + 512 - HALO:(t + 1) * 512]), r=(stt,), w=(tSm,))
                        S.dma("pool", uT_s.ap()[i], st, r=(stt,), w=(dU,), chan=stt.chan)
                S.dma("pool", sm_loc.ap(), smst[:], r=(tSm,), w=(dSl,), chan=tSm.chan)

            for l in range(L):
                last = (l == L - 1)
                if kind == "F":
                    S.barrier()
                    S.dma("sp", vecs[:].rearrange("p a b c -> p (a b c)"), vecs_in.ap()[l], w=(tConst,), chan=cc)
                    S.dma("sp", cvec[:].rearrange("p a b c -> p (a b c)"), cvec_in.ap()[l], w=(tConst,), chan=cc)
                layer_vectors(l)
                S.barrier()
                for t in range(NT if kind in ("A", "F") else 0):
                    buf, bt = (bufB0, tB0) if t % 2 == 0 else (bufB1, tB1)
                    S.dma("sp", buf[:], xsrc[0].ap()[:, :, t * 512:(t + 1) * 512].rearrange("j p n -> p j n"),
                          r=(dX,), w=(bt,), chan=bt.chan)
                    prenorm(buf[:], bt, gsc1, sh1, lambda j, t=t: bufA[:, j, t * 512:(t + 1) * 512], tA)
                if debug and kind == "A":
                    S.dma("sp", dbg["hT"].ap().rearrange("j p n -> p j n"), bufA[:], r=(tA,), w=(dDbg,), chan=tA.chan)
                S.barrier()
                ckpt(1)
                if kind == "A":
                    ph2(l)
                    S.barrier(full=True)
                    return
                if kind == "F":
                    ph2(l)
                    S.collective("pool", "AllGather", [kT_loc.ap()], [kT_all.ap()], r=(dKl,), w=(dKa,), chan=chCC)
                    S.collective("pool", "AllGather", [V_loc.ap().rearrange("(a b) d -> a (b d)", b=8)],
                                 [V_all.ap().rearrange("(a b) d -> a (b d)", b=8)], r=(dVl,), w=(dVa,), chan=chCC)
                    S.collective("pool", "AllGather", [sm_loc.ap()], [sm_all.ap()], r=(dSl,), w=(dSa,), chan=chCC)
                    if l + 1 < L:
                        cast_weights(l + 1)
                S.barrier()
                ckpt(3)
                S.dma("sp", hall, sm_all.ap()[:, 64:1024].rearrange("(r p) n -> p r n", p=128), r=(dSa,), w=(tHall,), chan=tHall.chan)
                S.dma("sp", kmT[:], sm_all.ap()[:, 0:64].rearrange("(r p) n -> p r n", p=128), r=(dSa,), w=(tHall,), chan=tHall.chan)
                for t in range(NT):
                    first = True
                    for r_ in range(8):
                        tp = t if r_ < 7 else t - 1
                        if tp < 0:
                            continue
                        src = hall[:, r_, tp * 240:(tp + 1) * 240]
                        if first:
                            S.op("dve", lambda q, src=src, r_=r_: q.tensor_scalar(out=halo_sel[:, t, :], in0=src, scalar1=hsel[:, r_:r_ + 1],
                                                                               scalar2=None, op0=ALU.mult), r=(tHall, tConst), w=(tMisc,))
                            first = False
                        else:
                            S.op("dve", lambda q, src=src, r_=r_: q.scalar_tensor_tensor(out=halo_sel[:, t, :], in0=src, scalar=hsel[:, r_:r_ + 1],
                                                                                      in1=halo_sel[:, t, :], op0=ALU.mult, op1=ALU.add),
                                 r=(tHall, tConst, tMisc), w=(tMisc,))
                cT_all = [bufB0, bufB1]
                ui = 0
                for i in range(8):
                    for j in range(CONVK):
                        S.op("dve", lambda q, j=j, i=i: q.tensor_scalar(out=dwm[:, j, :], in0=ident_b[:], scalar1=cvec[:, 0, i, j:j + 1],
                                                                        scalar2=None, op0=ALU.mult), r=(tConst,), w=(tDw,))
                    for t in range(NT):
                        up, upt = upad[ui % 2], tUp[ui % 2]
                        ui += 1
                        S.dma("sp", up[:, HALO:HALO + 512], uT_s.ap()[i, :, t * 512:(t + 1) * 512], r=(dU,), w=(upt,), chan=upt.chan)
                        S.op("dve", lambda q, up=up, t=t, i=i: q.tensor_copy(out=up[:, 0:HALO], in_=halo_sel[:, t, i * HALO:(i + 1) * HALO]),
                             r=(tMisc,), w=(upt,))
                        pb, pt_ = next_ps(0, 6)
                        for j in range(CONVK):
                            S.op("pe", lambda q, j=j, up=up: q.matmul(pb[:], lhsT=dwm[:, j, :], rhs=up[:, j:j + 512],
                                                                       start=(j == 0), stop=(j == CONVK - 1)),
                                 r=(tDw, upt), w=(pt_,), inc=(j == CONVK - 1))
                        cb, cbt = (bufB0, tB0) if i < 4 else (bufB1, tB1)
                        S.op("act", lambda q, pb=pb, cb=cb, i=i, t=t: q.activation(out=cb[:, (i % 4) * 4 + t, :], in_=pb[:], func=AF.Identity,
                                                                               bias=cvec[:, 0, i, 31:32], scale=1.0), r=(pt_, tConst), w=(cbt,))
                for t in range(NT):
                    for i in range(8):
                        cb, cbt = (bufB0, tB0) if i < 4 else (bufB1, tB1)
                        src = cb[:, (i % 4) * 4 + t, :]
                        S.op("pe", lambda q, src=src, i=i: q.matmul(ps[5][:], lhsT=ones_f[:], rhs=src, start=(i == 0), stop=(i == 7)),
                             r=(cbt, tConst), w=(tPs[5],), inc=(i == 7))
                        sumsq_accum(src, cbt, i == 0, i == 7, ps[6], tPs[6])
                    S.op("dve", lambda q: q.tensor_scalar(out=mean_t[:], in0=ps[5][:], scalar1=1.0 / 1024, scalar2=None, op0=ALU.mult),
                         r=(tPs[5],), w=(tMean,))
                    S.op("dve", lambda q: q.tensor_tensor(out=tmp_t[0][:], in0=mean_t[:], in1=mean_t[:], op=ALU.mult), r=(tMean,), w=(tTmp[0],))
                    S.op("dve", lambda q: q.scalar_tensor_tensor(out=rstd_t[:], in0=ps[6][:], scalar=1.0 / 1024, in1=tmp_t[0][:],
                                                                  op0=ALU.mult, op1=ALU.subtract), r=(tPs[6], tTmp[0]), w=(tRstd,))
                    S.op("act", lambda q: q.activation(out=rstd_t[:], in_=rstd_t[:], func=AF.Sqrt, bias=epsl[:, 0:1], scale=1.0),
                         r=(tRstd, tConst), w=(tRstd,))
                    S.op("dve", lambda q: q.reciprocal(out=rstd_t[:], in_=rstd_t[:]), r=(tRstd,), w=(tRstd,))
                    for i in range(8):
                        cb, cbt = (bufB0, tB0) if i < 4 else (bufB1, tB1)
                        src = cb[:, (i % 4) * 4 + t, :]
                        S.op("dve", lambda q, src=src: q.tensor_tensor(out=src, in0=src, in1=mean_t[:], op=ALU.subtract), r=(cbt, tMean), w=(cbt,))
                        S.op("dve", lambda q, src=src: q.tensor_tensor(out=src, in0=src, in1=rstd_t[:], op=ALU.mult), r=(cbt, tRstd), w=(cbt,))
                        S.op("act", lambda q, src=src, i=i, t=t: q.activation(out=bufA[:, 8 + i, t * 512:(t + 1) * 512], in_=src, func=AF.Silu,
                                                                              bias=cvec[:, 0, i, 33:34], scale=cvec[:, 0, i, 32:33]),
                             r=(cbt, tConst), w=(tA,))
                S.barrier()
                ckpt(4)
                NSL = 16
                S.dma("pool", emat.rearrange("p a b -> p (a b)"), emat_in.ap(), w=(tAtc,), chan=tAtc.chan)
                S.dma("pool", cmask.rearrange("p a b -> p (a b)"), cmask_in.ap(), w=(tAtc,), chan=tAtc.chan)
                S.dma("sp", gmask.rearrange("p a b -> p (a b)"), gmask_in.ap(), w=(tAtc,), chan=tAtc.chan)
                kring = [wring[0][:, s * 512:(s + 1) * 512] for s in range(NSL)]
                vring = [wring[1][:, s * 512:(s + 1) * 512].rearrange("p (k d) -> p k d", k=4) for s in range(NSL)]
                tKV = [tk(f"kv{s}", True) for s in range(NSL)]
                kvrot = 0
                prot = 0
                nmrot = 0
                srot = 0
                for h in range(NH):
                    qt, qtt = qTh[h % 2], tQ[h % 2]
                    S.dma("sp", qt, qT_s.ap()[h], r=(dQ,), w=(qtt,), chan=qtt.chan)
                    S.op("dve", lambda q, h=h: q.tensor_copy(out=kmh.rearrange("p (r b) -> p r b", r=8), in_=kmT[:, :, h * 8:(h + 1) * 8]),
                         r=(tHall,), w=(tGate,))
                    for t in range(NT):
                        for c_ in range(4):
                            S.op("pe", lambda q, c_=c_: q.matmul(ps[5][:, c_ * 64:(c_ + 1) * 64], lhsT=qt[:, t * 512 + c_ * 128:t * 512 + (c_ + 1) * 128],
                                                                  rhs=kmh, start=True, stop=True), r=(qtt, tGate), w=(tPs[5],), inc=(c_ == 3))
                        S.op("dve", lambda q: q.tensor_tensor(out=gm.rearrange("p a b -> p (a b)"), in0=ps[5][:, 0:256], in1=gmask[:, t, :], op=ALU.add),
                             r=(tPs[5], tAtc), w=(tGate,))
                        for c_ in range(4):
                            S.op("dve", lambda q, c_=c_: q.max(out=max8[:, c_, :], in_=gm[:, c_, :]), r=(tGate,), w=(tGate,))
                        S.op("dve", lambda q: q.tensor_scalar(out=thr, in0=max8[:, :, 2], scalar1=-1e29, scalar2=None, op0=ALU.max), r=(tGate,), w=(tGate,))
                        for c_ in range(4):
                            S.op("dve", lambda q, c_=c_: q.tensor_scalar(out=nm[:, c_, :], in0=gm[:, c_, :], scalar1=thr[:, c_:c_ + 1], scalar2=1.0,
                                                                         op0=ALU.is_ge, op1=ALU.subtract), r=(tGate,), w=(tGate,))
                        for c_ in range(4):
                            S.op("pe", lambda q, c_=c_: q.transpose(out=psb[:, c_ * 128:(c_ + 1) * 128], in_=nm[:, c_, :], identity=ident_b[:]),
                                 r=(tGate, tConst), w=(tPsb,), inc=(c_ == 3))
                        nmt, nmtt = nmT[nmrot % 2], tNmT[nmrot % 2]
                        nmrot += 1
                        S.op("act", lambda q, nmt=nmt: q.activation(out=nmt, in_=psb[:], func=AF.Identity), r=(tPsb,), w=(nmtt,))
                        visits = []
                        S.dma("sp", kown, kT_loc.ap()[h * 128:(h + 1) * 128, t * 512:(t + 1) * 512], r=(dKl,), w=(tOwn,), chan=tOwn.chan)
                        S.dma("sp", vown, V_loc.ap()[h * TOK + t * 512:h * TOK + (t + 1) * 512, :].rearrange("(k p) d -> p k d", p=128),
                              r=(dVl,), w=(tOwn,), chan=tOwn.chan)
                        for kt in range(4):
                            visits.append(("own", kown[:, kt * 128:(kt + 1) * 128], vown[:, kt, :], tOwn, ident_b[:], cmask[:, kt, :], (tConst, tAtc)))
                        for tp in range(t + 1):
                            for r_ in range(NCORES):
                                s = kvrot % NSL
                                kvrot += 1
                                visits.append(("load", s, r_, tp))
                                for kt in range(4):
                                    jblk = r_ * 8 + 2 * tp + kt // 2
                                    visits.append(("past", kring[s][:, kt * 128:(kt + 1) * 128], vring[s][:, kt, :], tKV[s],
                                                   emat[:, jblk, :], nmt, (tAtc, nmtt)))
                        comp = [v for v in visits if v[0] != "load"]
                        nvis = len(comp)
                        pend = []
                        ci = 0

                        def emit_pv(item, idx):
                            (vv, pt_i) = item
                            S.op("pe", lambda q: q.matmul(ps[3][:], lhsT=vv[2], rhs=pT[pt_i], start=(idx == 0), stop=(idx == nvis - 1)),
                                 r=(vv[3], tPT[pt_i]), w=(tPs[3],), inc=False)
                            S.op("pe", lambda q: q.matmul(ps[4][:], lhsT=ones_b[:], rhs=pT[pt_i], start=(idx == 0), stop=(idx == nvis - 1)),
                                 r=(tPT[pt_i], tConst), w=(tPs[4],), inc=True)

                        done = 0
                        for v in visits:
                            if v[0] == "load":
                                _, s, r_, tp = v
                                S.dma("sp", kring[s], kT_all.ap()[(r_ * NH + h) * 128:(r_ * NH + h + 1) * 128, tp * 512:(tp + 1) * 512],
                                      r=(dKa,), w=(tKV[s],), chan=tKV[s].chan)
                                base = (r_ * NH + h) * TOK + tp * 512
                                S.dma("sp", vring[s], V_all.ap()[base:base + 512, :].rearrange("(k p) d -> p k d", p=128),
                                      r=(dVa,), w=(tKV[s],), chan=tKV[s].chan)
                                continue
                            sb_i = srot % 3
                            srot += 1
                            pss, psst = ps[sb_i], tPs[sb_i]
                            S.op("pe", lambda q, v=v, pss=pss: q.matmul(pss[:], lhsT=v[1], rhs=qt[:, t * 512:(t + 1) * 512], start=True, stop=False),
                                 r=(v[3], qtt), w=(psst,), inc=False)
                            S.op("pe", lambda q, v=v, pss=pss: q.matmul(pss[:], lhsT=v[4], rhs=v[5], start=False, stop=True),
                                 r=v[6], w=(psst,), inc=True)
                            pi = prot % 4
                            prot += 1
                            S.op("act", lambda q, pss=pss, pi=pi: q.activation(out=pT[pi], in_=pss[:], func=AF.Exp, scale=SCALE),
                                 r=(psst,), w=(tPT[pi],))
                            pend.append((v, pi))
                            if len(pend) > 2:
                                emit_pv(pend.pop(0), done)
                                done += 1
                        while pend:
                            emit_pv(pend.pop(0), done)
                            done += 1
                        S.op("dve", lambda q: q.reciprocal(out=tmp_t[0][:], in_=ps[4][:]), r=(tPs[4],), w=(tTmp[0],))
                        S.op("dve", lambda q, h=h, t=t: q.tensor_tensor(out=bufA[:, h, t * 512:(t + 1) * 512], in0=ps[3][:], in1=tmp_t[0][:], op=ALU.mult),
                             r=(tPs[3], tTmp[0]), w=(tA,))
                if debug:
                    S.dma("sp", dbg["mixT"].ap().rearrange("j p n -> p j n"), bufA[:], r=(tA,), w=(dDbg,), chan=tA.chan)
                S.barrier()
                ckpt(5)
                for t in range(NT):
                    S.dma("sp", bufB1[:], xsrc[0].ap()[:, :, t * 512:(t + 1) * 512].rearrange("j p n -> p j n"), r=(dX,), w=(tB1,), chan=tB1.chan)
                    for blk in range(4):
                        wv, wtok = load_wblock(wb_out[l], blk * 512, 512, DC, l)
                        for jj in range(4):
                            j = blk * 4 + jj
                            pb, pt_ = next_ps(0, 6)
                            for k in range(DC):
                                S.op("pe", lambda q, k=k, jj=jj: q.matmul(pb[:], lhsT=wv[:, k, jj * 128:(jj + 1) * 128], rhs=bufA[:, k, t * 512:(t + 1) * 512],
                                                                          start=(k == 0), stop=(k == DC - 1)), r=(wtok, tA), w=(pt_,), inc=(k == DC - 1))
                            S.op("act", lambda q, pb=pb, j=j: q.activation(out=bufB0[:, j, :], in_=pb[:], func=AF.Identity), r=(pt_,), w=(tB0,))
                            sumsq_accum(bufB0[:, j, :], tB0, j == 0, j == DC - 1, ps[6], tPs[6])
                    post_residual(bufB0[:], tB0, bufB1[:], tB1, gp1)
                    S.dma("pool", xT_s.ap()[:, :, t * 512:(t + 1) * 512].rearrange("j p n -> p j n"), bufB1[:], r=(tB1,), w=(dX,), chan=tB1.chan)
                    h2v = bufB0[:].rearrange("p j n -> p (j n)").bitcast(BF16)[:, 0:DC * 512].rearrange("p (j n) -> p j n", j=DC)
                    prenorm(bufB1[:], tB1, gsc2, sh2, lambda j: h2v[:, j, :], tB0)
                    S.dma("pool", h2T_s.ap()[:, :, t * 512:(t + 1) * 512].rearrange("j p n -> p j n"), h2v, r=(tB0,), w=(dH2,), chan=tB0.chan)
                xsrc[0] = xT_s
                S.barrier()
                ckpt(6)
                S.dma("sp", bufA[:], h2T_s.ap().rearrange("j p n -> p j n"), r=(dH2,), w=(tA,), chan=tA.chan)
                for fb in range(FC // 4):
                    wg, wgt = load_wblock(wb_gate[l], fb * 512, 512, DC, l)
                    wu, wut = load_wblock(wb_up[l], fb * 512, 512, DC, l)
                    for jj in range(4):
                        f = fb * 4 + jj
                        st, stt = next_stage()
                        for t in range(NT):
                            pg, pgt = next_ps(0, 6)
                            proj_block(wg, wgt, jj, t, pg, pgt)
                            pu, put = next_ps(0, 6)
                            proj_block(wu, wut, jj, t, pu, put)
                            sg, sgt = tmp_t[t % 2], tTmp[t % 2]
                            S.op("act", lambda q, pg=pg, sg=sg: q.activation(out=sg[:], in_=pg[:], func=AF.Silu), r=(pgt,), w=(sgt,))
                            S.op("dve", lambda q, pu=pu, sg=sg, st=st, t=t: q.tensor_tensor(out=st[:, t * 512:(t + 1) * 512], in0=pu[:], in1=sg[:], op=ALU.mult),
                                 r=(put, sgt), w=(stt,))
                        S.dma("pool", actT_s.ap()[f], st, r=(stt,), w=(dAct,), chan=stt.chan)
                S.barrier()
                ckpt(7)
                actv = bufA[:].rearrange("p j n -> p (j n)")[:, 0:FC * 512].rearrange("p (f n) -> p f n", f=FC)
                for t in range(NT):
                    S.dma("sp", actv, actT_s.ap()[:, :, t * 512:(t + 1) * 512].rearrange("f p n -> p f n"), r=(dAct,), w=(tA,), chan=tA.chan)
                    S.dma("sp", bufB1[:], xT_s.ap()[:, :, t * 512:(t + 1) * 512].rearrange("j p n -> p j n"), r=(dX,), w=(tB1,), chan=tB1.chan)
                    for j in range(DC):
                        wi_ = wslot[0] % 3
                        wslot[0] += 1
                        S.dma("sp", wring[wi_][:, 0:FC * 128], wb_down[l].ap()[j], r=(dWb[l],), w=(tW[wi_],), chan=tW[wi_].chan)
                        wv, wtok = wring[wi_][:, 0:FC * 128].rearrange("p (k n) -> p k n", k=FC), tW[wi_]
                        pb, pt_ = next_ps(0, 6)
                        for k in range(FC):
                            S.op("pe", lambda q, k=k: q.matmul(pb[:], lhsT=wv[:, k, :], rhs=actv[:, k, :], start=(k == 0), stop=(k == FC - 1)),
                                 r=(wtok, tA), w=(pt_,), inc=(k == FC - 1))
                        S.op("act", lambda q, pb=pb, j=j: q.activation(out=bufB0[:, j, :], in_=pb[:], func=AF.Identity), r=(pt_,), w=(tB0,))
                        sumsq_accum(bufB0[:, j, :], tB0, j == 0, j == DC - 1, ps[6], tPs[6])
                    post_residual(bufB0[:], tB0, bufB1[:], tB1, gp2)
                    dst = outT if (kind == "B" or last) else xT_s
                    S.dma("pool", dst.ap()[:, :, t * 512:(t + 1) * 512].rearrange("j p n -> p j n"), bufB1[:], r=(tB1,),
                          w=(dOut if (kind == "B" or last) else dX,), chan=tB1.chan)
                S.barrier()
            S.barrier(full=True)

        def run_pass(live, q):
            S = Sched(nc, es, sems)
            qs = {n: (q if n == live else _DummyEng()) for n in ("pe", "act", "dve", "pool", "sp")}
            try:
                program(S, qs["pe"], qs["act"], qs["dve"], qs["pool"], qs["sp"])
            except _Stop:
                pass

        @block.tensor
        def _(q):
            run_pass("pe", q)

        @block.scalar
        def _(q):
            run_pass("act", q)

        @block.vector
        def _(q):
            run_pass("dve", q)

        @block.gpsimd
        def _(q):
            run_pass("pool", q)

        @block.sync
        def _(q):
            run_pass("sp", q)
    return nc


def _consts(core):
    ident = np.eye(128, dtype=np.float32)
    emat = np.zeros((64, 64, 128), np.float32)
    for n in range(64):
        emat[n, n, :] = MASKV
    k = np.arange(128)[:, None]
    q = np.arange(128)[None, :]
    tri = np.where(k <= q, 0.0, -MASKV).astype(np.float32)
    full = np.zeros((128, 128), np.float32)
    ninf = np.full((128, 128), -MASKV, np.float32)
    cm = np.stack([
        np.concatenate([tri, full, ninf, ninf], 1),
        np.concatenate([ninf, tri, ninf, ninf], 1),
        np.concatenate([ninf, ninf, tri, full], 1),
        np.concatenate([ninf, ninf, ninf, tri], 1)], 0)
    cmask = np.ascontiguousarray(cm.transpose(1, 0, 2)).reshape(128, 4 * 512)
    jj = np.arange(64)
    r_ = jj // 8
    lb = jj % 8
    nglob = 2 * (8 * (lb // 2) + r_) + (lb % 2)
    gmask = np.zeros((NT, 4, 64), np.float32)
    for t in range(NT):
        g = 8 * t + core
        for c_ in range(4):
            blk = 2 * g + (1 if c_ >= 2 else 0)
            gmask[t, c_, :] = np.where(nglob < blk, 0.0, NEG)
    gmask = np.broadcast_to(gmask.reshape(1, NT * 256), (128, NT * 256)).copy()
    hs = np.zeros((8,), np.float32)
    if core == 0:
        hs[7] = 1.0
    else:
        hs[core - 1] = 1.0
    hsel = np.broadcast_to(hs[None, :], (128, 8)).copy()
    return ident, emat.reshape(64, 64 * 128), cmask, gmask, hsel


def _fm(v):
    sh = v.shape
    v = v.reshape(sh[:-1] + (sh[-1] // 128, 128))
    return np.ascontiguousarray(np.moveaxis(v, -1, 0))


_CACHE = {}


def _prog(kind, debug=False, stop=None):
    key = (kind, debug, stop)
    if key not in _CACHE:
        _CACHE[key] = build_program(kind, debug, stop)
    return _CACHE[key]


def _kernel_unfused(x, c, ada_w, ada_b, mix_pre_g, mix_post_g, w_in, conv_w, conv_b, conv_ln_g, conv_ln_b, w_out,
                    ffn_pre_g, ffn_post_g, w_gate, w_up, w_down, _depth=L_FULL, _debug=False, _stop=None):
    L = _depth
    cores = list(range(NCORES))
    f32 = lambda a: np.ascontiguousarray(np.asarray(a, dtype=np.float32))
    x = f32(x).reshape(SEQ, D)
    ada_w = f32(ada_w)
    ada_b = f32(ada_b)
    dbg = {}
    cT = _fm(f32(c).reshape(D))
    in_maps = []
    for core in cores:
        aw = np.ascontiguousarray(ada_w[:, :, core * 1536:(core + 1) * 1536])
        ab = ada_b[:, core * 1536:(core + 1) * 1536].reshape(L_FULL, 12, 128)
        ab = np.ascontiguousarray(ab.transpose(2, 0, 1)).reshape(128, L_FULL * 12)
        in_maps.append({"cT": cT, "adaw": aw, "adab": ab})
    res = run_bass_kernel_spmd(_prog("M"), in_maps, core_ids=cores)
    modall = np.stack([np.asarray(res.results[r]["mod_out"]).reshape(128, L_FULL, 12) for r in cores], axis=1)
    xt = x.reshape(NT, NCORES, 512, D)
    xTs = [np.ascontiguousarray(xt[:, core].reshape(TOK, D).T).reshape(DC, 128, TOK) for core in cores]
    consts = [_consts(core) for core in cores]
    for l in range(L):
        modT = np.ascontiguousarray(modall[:, :, l, :]).reshape(128, NCORES * 12)
        vecs = np.stack([_fm(f32(v)[l]) for v in (mix_pre_g, mix_post_g, ffn_pre_g, ffn_post_g)], axis=1)
        vecs = np.ascontiguousarray(vecs).reshape(128, 4 * DC)
        cv = np.concatenate([f32(conv_w)[l], f32(conv_b)[l][None], f32(conv_ln_g)[l][None], f32(conv_ln_b)[l][None]], axis=0)
        cvec = np.ascontiguousarray(cv.reshape(34, 8, 128).transpose(2, 1, 0)).reshape(128, 8 * 34)
        wi = f32(w_in[l])
        in_maps = [{"xT_in": xTs[core], "modT": modT, "vecs": vecs, "w_in": wi} for core in cores]
        resA = run_bass_kernel_spmd(_prog("A", _debug), in_maps, core_ids=cores).results
        if _debug:
            dbg[("A", l)] = resA
        kT_all = np.concatenate([np.asarray(resA[r]["kT_loc"]) for r in cores], axis=0)
        V_all = np.concatenate([np.asarray(resA[r]["V_loc"]) for r in cores], axis=0)
        sm_all = np.concatenate([np.asarray(resA[r]["sm_loc"]) for r in cores], axis=0)
        wo, wg, wu, wd = f32(w_out[l]), f32(w_gate[l]), f32(w_up[l]), f32(w_down[l])
        in_maps = []
        for core in cores:
            ident, emat, cmask, gmask, hsel = consts[core]
            in_maps.append({"xT_in": xTs[core], "modT": modT, "vecs": vecs, "cvec": cvec,
                            "w_out": wo, "w_gate": wg, "w_up": wu, "w_down": wd,
                            "ident": ident, "emat": emat, "cmask": cmask, "gmask": gmask, "hsel": hsel,
                            "qT_s": np.asarray(resA[core]["qT_s"]), "kT_loc": np.asarray(resA[core]["kT_loc"]),
                            "V_loc": np.asarray(resA[core]["V_loc"]), "kT_all": kT_all, "V_all": V_all, "sm_all": sm_all,
                            "uT_s": np.asarray(resA[core]["uT_s"])})
        resB = run_bass_kernel_spmd(_prog("B", _debug, _stop), in_maps, core_ids=cores).results
        if _debug:
            dbg[("B", l)] = resB
        xTs = [np.asarray(resB[core]["outT"]).reshape(DC, 128, TOK) for core in cores]
    out = np.empty((NT, NCORES, 512, D), np.float32)
    for core in cores:
        out[:, core] = xTs[core].reshape(D, TOK).T.reshape(NT, 512, D)
    if _debug:
        _kernel_unfused._last = dbg
        _kernel_unfused._mod = modall
    return out.reshape(1, SEQ, D)


def _kernel_fused(x, c, ada_w, ada_b, mix_pre_g, mix_post_g, w_in, conv_w, conv_b, conv_ln_g, conv_ln_b, w_out,
                  ffn_pre_g, ffn_post_g, w_gate, w_up, w_down):
    L = L_FULL
    cores = list(range(NCORES))
    f32 = lambda a: np.ascontiguousarray(np.asarray(a, dtype=np.float32))
    x = f32(x).reshape(SEQ, D)
    ada_w = f32(ada_w)
    ada_b = f32(ada_b)
    cT = _fm(f32(c).reshape(D))
    vecs = np.stack([np.stack([_fm(f32(v)[l]) for v in (mix_pre_g, mix_post_g, ffn_pre_g, ffn_post_g)], axis=1).reshape(128, 4 * DC)
                     for l in range(L)], axis=0)
    cvs = []
    for l in range(L):
        cv = np.concatenate([f32(conv_w)[l], f32(conv_b)[l][None], f32(conv_ln_g)[l][None], f32(conv_ln_b)[l][None]], axis=0)
        cvs.append(np.ascontiguousarray(cv.reshape(34, 8, 128).transpose(2, 1, 0)).reshape(128, 8 * 34))
    cvec = np.stack(cvs, axis=0)
    shared = {"cT": cT, "vecs": np.ascontiguousarray(vecs), "cvec": np.ascontiguousarray(cvec),
              "w_in": f32(w_in), "w_out": f32(w_out), "w_gate": f32(w_gate), "w_up": f32(w_up), "w_down": f32(w_down)}
    xt = x.reshape(NT, NCORES, 512, D)
    in_maps = []
    for core in cores:
        ident, emat, cmask, gmask, hsel = _consts(core)
        xT = np.ascontiguousarray(xt[:, core].reshape(TOK, D).T).reshape(DC, 128, TOK)
        aw = np.ascontiguousarray(ada_w[:, :, core * 1536:(core + 1) * 1536])
        ab = ada_b[:, core * 1536:(core + 1) * 1536].reshape(L, 12, 128)
        ab = np.ascontiguousarray(ab.transpose(2, 0, 1)).reshape(128, L * 12)
        m = dict(shared)
        m.update({"xT_in": xT, "adaw": aw, "adab": ab, "ident": ident, "emat": emat, "cmask": cmask, "gmask": gmask, "hsel": hsel})
        in_maps.append(m)
    res = run_bass_kernel_spmd(_prog("F"), in_maps, core_ids=cores)
    out = np.empty((NT, NCORES, 512, D), np.float32)
    for core in cores:
        oT = np.asarray(res.results[core]["outT"]).reshape(D, TOK)
        out[:, core] = oT.T.reshape(NT, 512, D)
    return out.reshape(1, SEQ, D)


def kernel(x, c, ada_w, ada_b, mix_pre_g, mix_post_g, w_in, conv_w, conv_b, conv_ln_g, conv_ln_b, w_out,
           ffn_pre_g, ffn_post_g, w_gate, w_up, w_down):
    return _kernel_unfused(x, c, ada_w, ada_b, mix_pre_g, mix_post_g, w_in, conv_w, conv_b, conv_ln_g, conv_ln_b, w_out,
                           ffn_pre_g, ffn_post_g, w_gate, w_up, w_down)
```

```python
import os
import numpy as np
import ml_dtypes
from contextlib import ExitStack
import concourse.bass as bass
import concourse.mybir as mybir
from concourse.bass_utils import run_bass_kernel_spmd

F32 = mybir.dt.float32
BF16 = mybir.dt.bfloat16
AF = mybir.ActivationFunctionType
ALU = mybir.AluOpType
AX = mybir.AxisListType

NCORES = 8
D = 2048
SEQ = 16384
L_FULL = 4
TOK = SEQ // NCORES
NT = 4
DC = D // 128
DFF = 5632
FC = DFF // 128
INC = 5120
CONVK = 31
HALO = 30
NH = 8
RMS_EPS = 1e-6
LN_EPS = 1e-5
SCALE = 128 ** -0.5
MASKV = 30000.0
NEG = -1e30


class Tok:
    __slots__ = ("name", "w", "r", "chan")

    def __init__(self, name):
        self.name = name
        self.w = {}
        self.r = {}
        self.chan = None


class Chan:
    __slots__ = ("sem", "val")

    def __init__(self, sem):
        self.sem = sem
        self.val = 0


class Eng:
    def __init__(self, name, q, sem):
        self.name = name
        self.q = q
        self.sem = sem
        self.cnt = 0
        self.seen = {}
        self.pr = []
        self.pw = []


class _DummyIns:
    def then_inc(self, *a, **k):
        return self


class _DummyEng:
    def __getattr__(self, name):
        def f(*a, **k):
            return _DummyIns()
        return f


class Sched:
    def __init__(self, nc, es, sems):
        self.nc = nc
        self.es = es
        self.engs = {}
        self.chans = []
        self.allchans = []
        self.nsem = 0
        self.sems = sems

    def new_sem(self, name):
        i = self.nsem
        self.nsem += 1
        if i >= len(self.sems):
            self.sems.append(self.es.enter_context(self.nc.semaphore(name)))
        return self.sems[i]

    def add_engine(self, name, q):
        e = Eng(name, q, self.new_sem("prog_" + name))
        self.engs[name] = e
        return e

    def chan(self, name, barrier=True):
        c = Chan(self.new_sem("ch_" + name))
        self.allchans.append(c)
        if barrier:
            self.chans.append(c)
        return c

    def tok(self, name, chan=False):
        t = Tok(name)
        if chan:
            t.chan = self.chan(name)
        return t

    @staticmethod
    def _merge(d, ev):
        s, v = ev
        k = id(s)
        if k not in d or d[k][1] < v:
            d[k] = (s, v)

    def _wait(self, e, ev):
        s, v = ev
        if s is e.sem and e.name in ("pe", "sp"):
            return
        k = id(s)
        if e.seen.get(k, 0) >= v:
            return
        e.q.wait_ge(s, v)
        e.seen[k] = v

    def _deps(self, e, reads, writes):
        for t in reads:
            for ev in t.w.values():
                self._wait(e, ev)
        for t in writes:
            for ev in t.w.values():
                self._wait(e, ev)
            for ev in t.r.values():
                self._wait(e, ev)

    def _commit(self, ev, reads, writes):
        for t in reads:
            self._merge(t.r, ev)
        for t in writes:
            t.w = {}
            t.r = {}
            self._merge(t.w, ev)

    def op(self, ename, fn, r=(), w=(), inc=True):
        e = self.engs[ename]
        self._deps(e, r, w)
        ins = fn(e.q)
        if inc:
            e.cnt += 1
            ins.then_inc(e.sem, 1)
            ev = (e.sem, e.cnt)
            self._commit(ev, list(r) + e.pr, list(w) + e.pw)
            e.pr = []
            e.pw = []
        else:
            e.pr += list(r)
            e.pw += list(w)
        return ins

    def dma(self, ename, out, in_, r=(), w=(), chan=None):
        e = self.engs[ename]
        self._deps(e, r, w)
        chan.val += 16
        e.q.dma_start(out=out, in_=in_).then_inc(chan.sem, 16)
        ev = (chan.sem, chan.val)
        self._commit(ev, r, w)
        return ev

    def collective(self, ename, kind, ins, outs, r=(), w=(), chan=None):
        e = self.engs[ename]
        self._deps(e, r, w)
        chan.val += 1
        e.q.collective_compute(kind, ALU.bypass, replica_groups=[list(range(NCORES))],
                               ins=ins, outs=outs).then_inc(chan.sem, 1)
        ev = (chan.sem, chan.val)
        self._commit(ev, r, w)
        return ev

    def barrier(self, full=False):
        evs = [(e.sem, e.cnt) for e in self.engs.values() if e.cnt > 0]
        evs += [(c.sem, c.val) for c in (self.allchans if full else self.chans) if c.val > 0]
        for e in self.engs.values():
            for ev in evs:
                self._wait(e, ev)


class _Stop(Exception):
    pass


def build_program(kind, debug=False, stop=None):
    nc = bass.Bass("TRN2", target_bir_lowering=False)
    L = 1

    def din(name, shape, dt=F32):
        return nc.dram_tensor(name, list(shape), dt, kind="ExternalInput")

    def dout(name, shape, dt=F32):
        return nc.dram_tensor(name, list(shape), dt, kind="ExternalOutput")

    def dsc(name, shape, dt):
        return nc.dram_tensor(name, list(shape), dt)

    LM = L_FULL
    if kind == "F":
        L = L_FULL
        xT_in = din("xT_in", [DC, 128, TOK])
        cT_in = din("cT", [128, DC])
        adaw_in = din("adaw", [LM, D, 1536])
        adab_in = din("adab", [128, LM * 12])
        vecs_in = din("vecs", [L, 128, 4 * DC])
        cvec_in = din("cvec", [L, 128, 8 * 34])
        w_in_d = din("w_in", [L, D, INC])
        w_out_d = din("w_out", [L, D, D])
        w_gate_d = din("w_gate", [L, D, DFF])
        w_up_d = din("w_up", [L, D, DFF])
        w_down_d = din("w_down", [L, DFF, D])
        ident_in = din("ident", [128, 128])
        emat_in = din("emat", [64, 64 * 128], BF16)
        cmask_in = din("cmask", [128, 4 * 512], BF16)
        gmask_in = din("gmask", [128, NT * 256])
        hsel_in = din("hsel", [128, 8])
        outT = dout("outT", [DC, 128, TOK])
        xT_s = dsc("xT_s", [DC, 128, TOK], F32)
        qT_s = dsc("qT_s", [NH, 128, TOK], BF16)
        kT_loc = dsc("kT_loc", [NH * 128, TOK], BF16)
        kT_all = dsc("kT_all", [NCORES * NH * 128, TOK], BF16)
        V_loc = dsc("V_loc", [NH * TOK, 128], BF16)
        V_all = dsc("V_all", [NCORES * NH * TOK, 128], BF16)
        sm_loc = dsc("sm_loc", [128, 1024], BF16)
        sm_all = dsc("sm_all", [NCORES * 128, 1024], BF16)
        mod_loc = dsc("mod_loc", [128, 512], F32)
        mod_all = dsc("mod_all", [NCORES * 128, 512], F32)
        uT_s = dsc("uT_s", [8, 128, TOK], BF16)
        h2T_s = dsc("h2T_s", [DC, 128, TOK], BF16)
        actT_s = dsc("actT_s", [FC, 128, TOK], BF16)
        wb_in = [dsc(f"wb_in{l}", [D, INC], BF16) for l in range(L)]
        wb_out = [dsc(f"wb_out{l}", [D, D], BF16) for l in range(L)]
        wb_gate = [dsc(f"wb_gate{l}", [D, DFF], BF16) for l in range(L)]
        wb_up = [dsc(f"wb_up{l}", [D, DFF], BF16) for l in range(L)]
        wb_down = [dsc(f"wb_down{l}", [DC, 128, FC * 128], BF16) for l in range(L)]
    elif kind == "M":
        cT_in = din("cT", [128, DC])
        adaw_in = din("adaw", [LM, D, 1536])
        adab_in = din("adab", [128, LM * 12])
        mod_out = dout("mod_out", [128, LM * 12])
    else:
        xT_in = din("xT_in", [DC, 128, TOK])
        modT_in = din("modT", [128, NCORES * 12])
        vecs_in = din("vecs", [128, 4 * DC])
    if kind == "A":
        w_in_d = din("w_in", [D, INC])
        qT_s = dout("qT_s", [NH, 128, TOK], BF16)
        kT_loc = dout("kT_loc", [NH * 128, TOK], BF16)
        V_loc = dout("V_loc", [NH * TOK, 128], BF16)
        sm_loc = dout("sm_loc", [128, 1024], BF16)
        uT_s = dout("uT_s", [8, 128, TOK], BF16)
        wb_in = [dsc("wb_in0", [D, INC], BF16)]
    if kind == "B":
        cvec_in = din("cvec", [128, 8 * 34])
        w_out_d = din("w_out", [D, D])
        w_gate_d = din("w_gate", [D, DFF])
        w_up_d = din("w_up", [D, DFF])
        w_down_d = din("w_down", [DFF, D])
        ident_in = din("ident", [128, 128])
        emat_in = din("emat", [64, 64 * 128], BF16)
        cmask_in = din("cmask", [128, 4 * 512], BF16)
        gmask_in = din("gmask", [128, NT * 256])
        hsel_in = din("hsel", [128, 8])
        qT_s = din("qT_s", [NH, 128, TOK], BF16)
        kT_loc = din("kT_loc", [NH * 128, TOK], BF16)
        V_loc = din("V_loc", [NH * TOK, 128], BF16)
        kT_all = din("kT_all", [NCORES * NH * 128, TOK], BF16)
        V_all = din("V_all", [NCORES * NH * TOK, 128], BF16)
        sm_all = din("sm_all", [NCORES * 128, 1024], BF16)
        uT_s = din("uT_s", [8, 128, TOK], BF16)
        outT = dout("outT", [DC, 128, TOK])
        xT_s = dsc("xT_s", [DC, 128, TOK], F32)
        h2T_s = dsc("h2T_s", [DC, 128, TOK], BF16)
        actT_s = dsc("actT_s", [FC, 128, TOK], BF16)
        wb_out = [dsc("wb_out0", [D, D], BF16)]
        wb_gate = [dsc("wb_gate0", [D, DFF], BF16)]
        wb_up = [dsc("wb_up0", [D, DFF], BF16)]
        wb_down = [dsc("wb_down0", [DC, 128, FC * 128], BF16)]
    dbg = {}
    if debug and kind == "B":
        dbg["mixT"] = dout("dbg_mixT", [DC, 128, TOK], BF16)
    if debug and kind == "A":
        dbg["hT"] = dout("dbg_hT", [DC, 128, TOK], BF16)

    with ExitStack() as es:
        def sb(name, shape, dt):
            return es.enter_context(nc.sbuf_tensor("s_" + name, list(shape), dt))

        def pst(name, shape, dt):
            return es.enter_context(nc.psum_tensor("p_" + name, list(shape), dt))

        bufA = sb("bufA", [128, DC, TOK], BF16)
        bufB0 = sb("bufB0", [128, DC, 512], F32)
        bufB1 = sb("bufB1", [128, DC, 512], F32)
        wring = [sb(f"wring{i}", [128, 8192], BF16) for i in range(3)]
        b0b = bufB0[:].rearrange("p j n -> p (j n)").bitcast(BF16)
        b1b = bufB1[:].rearrange("p j n -> p (j n)").bitcast(BF16)
        b0f = bufB0[:].rearrange("p j n -> p (j n)")
        b1f = bufB1[:].rearrange("p j n -> p (j n)")
        stage = [b0b[:, i * TOK:(i + 1) * TOK] for i in range(3)]
        emat = b0b[0:64, 0:8192].rearrange("p (a b) -> p a b", a=64)
        cmask = b0b[:, 8192:10240].rearrange("p (a b) -> p a b", a=4)
        gmask = b0f[:, 5120:6144].rearrange("p (a b) -> p a b", a=NT)
        qTh = [b0b[:, 12288 + i * TOK:12288 + (i + 1) * TOK] for i in range(2)]
        pT = [b1b[:, i * 512:(i + 1) * 512] for i in range(4)]
        nmT = [b1b[0:64, 2048 + i * 512:2048 + (i + 1) * 512] for i in range(2)]
        kown = b1b[:, 3072:3584]
        vown = b1b[:, 3584:4096].rearrange("p (k d) -> p k d", k=4)
        gm = b1f[:, 2048:2304].rearrange("p (a b) -> p a b", a=4)
        max8 = b1f[:, 2304:2336].rearrange("p (a b) -> p a b", a=4)
        thr = b1f[:, 2336:2340]
        nm = b1b[:, 4736:4992].rearrange("p (a b) -> p a b", a=4)
        kmh = b1b[:, 4992:5056]
        hall = wring[0][:, 0:NCORES * 960].rearrange("p (r n) -> p r n", r=NCORES)
        dwm = wring[1][:, 0:CONVK * 128].rearrange("p (j n) -> p j n", j=CONVK)
        upad = [wring[1][:, 4096 + i * 544:4096 + i * 544 + HALO + 512] for i in range(2)]
        halo_sel = wring[1][:, 5632:5632 + NT * 240].rearrange("p (t n) -> p t n", t=NT)
        sqr = [sb(f"sqr{i}", [128, 512], F32) for i in range(3)]
        rstd_t = sb("rstd", [128, 512], F32)
        mean_t = sb("mean", [128, 512], F32)
        tmp_t = [sb(f"tmpf{i}", [128, 512], F32) for i in range(2)]
        ident_f = sb("ident_f", [128, 128], F32)
        ident_b = sb("ident_b", [128, 128], BF16)
        ones_f = sb("ones_f", [128, 128], F32)
        ones_b = sb("ones_b", [128, 128], BF16)
        hsel = sb("hsel", [128, 8], F32)
        epsr = sb("epsr", [128, 1], F32)
        epsl = sb("epsl", [128, 1], F32)
        cT = sb("cT", [128, DC], F32)
        cact = sb("cact", [128, DC], F32)
        adab = sb("adab", [128, LM * 12], F32)
        modp = sb("modp", [128, LM * 12], F32)
        modT = sb("modT", [128, NCORES, LM * 12 if kind == "F" else 12], F32)
        vecs = sb("vecs", [128, 1, 4, DC], F32)
        cvec = sb("cvec", [128, 1, 8, 34], F32)
        gsc1 = sb("gsc1", [128, DC], F32)
        gsc2 = sb("gsc2", [128, DC], F32)
        gp1 = sb("gp1", [128, DC], F32)
        gp2 = sb("gp2", [128, DC], F32)
        sh1 = sb("sh1", [128, DC], F32)
        sh2 = sb("sh2", [128, DC], F32)
        kms = sb("kms", [128, NH, 8], F32)
        smst = sb("smst", [128, 1024], BF16)
        kmT = sb("kmT", [128, NCORES, 64], BF16)

        ps = [pst(f"ps{i}", [128, 512], F32) for i in range(7)]
        psb = pst("psb", [64, 512], BF16)

        sems = []
        block = es.enter_context(nc.Block())

        def program(S, pe, act, dve, pool, sp):
            S.add_engine("pe", pe)
            S.add_engine("act", act)
            S.add_engine("dve", dve)
            S.add_engine("pool", pool)
            S.add_engine("sp", sp)

            T = {}

            def tk(name, chan=False):
                if name not in T:
                    T[name] = S.tok(name, chan)
                return T[name]

            tA = tk("bufA", True)
            tB0 = tk("bufB0", True)
            tB1 = tk("bufB1", True)
            tW = [tk(f"wring{i}", True) for i in range(3)]
            tSt = [tk(f"stage{i}", True) for i in range(3)]
            tSq = [tk(f"sqr{i}") for i in range(3)]
            tPs = [tk(f"ps{i}") for i in range(7)]
            tPsb = tk("psb")
            tRstd = tk("rstd")
            tMean = tk("mean")
            tTmp = [tk("tmpf0"), tk("tmpf1")]
            tConst = tk("const", True)
            tMisc = tk("misc", True)
            tSm = tk("smst", True)
            tHall = tk("hall", True)
            tUp = [tk("upad0", True), tk("upad1", True)]
            tDw = tk("dwm")
            tNmT = [tk("nmT0"), tk("nmT1")]
            tPT = [tk(f"pT{i}") for i in range(4)]
            tQ = [tk("qTh0", True), tk("qTh1", True)]
            tOwn = tk("own", True)
            tGate = tk("gate")
            tKms = tk("kms")
            tAtc = tk("attc", True)
            dX = tk("d_xT")
            dQ = tk("d_qT")
            dKl = tk("d_kTloc")
            dKa = tk("d_kTall")
            dVl = tk("d_Vloc")
            dVa = tk("d_Vall")
            dSl = tk("d_smloc")
            dSa = tk("d_small")
            dU = tk("d_uT")
            dH2 = tk("d_h2T")
            dAct = tk("d_actT")
            dWb = [tk(f"d_wb{l}") for l in range(L)]
            dMl = tk("d_modloc")
            dMa = tk("d_modall")
            dOut = tk("d_out")
            dDbg = tk("d_dbg")
            chCast = S.chan("cast", barrier=False)
            chCC = S.chan("cc")

            def cast_weights(l):
                if kind == "A":
                    lst = ((w_in_d, wb_in, D),)
                elif kind == "B":
                    lst = ((w_out_d, wb_out, D), (w_gate_d, wb_gate, D), (w_up_d, wb_up, D), (w_down_d, wb_down, DFF))
                else:
                    lst = ((w_in_d, wb_in, D), (w_out_d, wb_out, D), (w_gate_d, wb_gate, D), (w_up_d, wb_up, D), (w_down_d, wb_down, DFF))
                for (src, dst, rows) in lst:
                    nsp = 8
                    rs = rows // nsp
                    sap = src.ap()[l] if kind == "F" else src.ap()
                    if rows == DFF:
                        for j in range(DC):
                            for kq in range(4):
                                k0, k1 = kq * 11, (kq + 1) * 11
                                S.dma("pool", dst[l].ap()[j][:, k0 * 128:k1 * 128].rearrange("p (k n) -> p k n", n=128),
                                      sap[k0 * 128:k1 * 128, j * 128:(j + 1) * 128].rearrange("(k p) n -> p k n", p=128),
                                      r=(), w=(dWb[l],), chan=chCast)
                        continue
                    for i in range(nsp):
                        S.dma("pool", dst[l].ap()[i * rs:(i + 1) * rs, :], sap[i * rs:(i + 1) * rs, :],
                              r=(), w=(dWb[l],), chan=chCast)

            wslot = [0]

            def load_wblock(wdram, c0, ncols, kch, l):
                i = wslot[0] % 3
                wslot[0] += 1
                view = wring[i][:, 0:kch * ncols].rearrange("p (k n) -> p k n", k=kch)
                S.dma("sp", view, wdram.ap()[:, c0:c0 + ncols].rearrange("(k p) n -> p k n", p=128),
                      r=(dWb[l],), w=(tW[i],), chan=tW[i].chan)
                return view, tW[i]

            psrot = [0]

            def next_ps(lo=0, hi=7):
                i = lo + psrot[0] % (hi - lo)
                psrot[0] += 1
                return ps[i], tPs[i]

            sqrot = [0]
            strot = [0]

            def next_stage():
                i = strot[0] % 3
                strot[0] += 1
                return stage[i], tSt[i]

            evrot = [0]

            def evac_copy(out, in_, r, w):
                evrot[0] += 1
                if evrot[0] % 2:
                    S.op("act", lambda q: q.activation(out=out, in_=in_, func=AF.Identity), r=r, w=w)
                else:
                    S.op("dve", lambda q: q.tensor_copy(out=out, in_=in_), r=r, w=w)

            def sumsq_accum(src_ap, src_tok, first, last, acc_ps, acc_tok):
                i = sqrot[0] % 3
                sqrot[0] += 1
                S.op("act", lambda q: q.activation(out=sqr[i][:], in_=src_ap, func=AF.Square), r=(src_tok,), w=(tSq[i],))
                S.op("pe", lambda q: q.matmul(acc_ps[:], lhsT=ones_f[:], rhs=sqr[i][:], start=first, stop=last),
                     r=(tSq[i], tConst), w=(acc_tok,), inc=True)

            def rstd_from(acc_ps, acc_tok, n, eps):
                ept = epsr if eps == RMS_EPS else epsl
                S.op("act", lambda q: q.activation(out=rstd_t[:], in_=acc_ps[:], func=AF.Sqrt, bias=ept[:, 0:1], scale=1.0 / n),
                     r=(acc_tok, tConst), w=(tRstd,))
                S.op("dve", lambda q: q.reciprocal(out=rstd_t[:], in_=rstd_t[:]), r=(tRstd,), w=(tRstd,))

            def prenorm(xt, xtok, gsc, shv, dst_fn, dst_tok):
                acc, acct = ps[6], tPs[6]
                for j in range(DC):
                    sumsq_accum(xt[:, j, :], xtok, j == 0, j == DC - 1, acc, acct)
                rstd_from(acc, acct, D, RMS_EPS)
                S.op("dve", lambda q: q.tensor_tensor(out=xt, in0=xt, in1=rstd_t[:].unsqueeze(1).to_broadcast([128, DC, 512]),
                                                       op=ALU.mult), r=(xtok, tRstd), w=(xtok,))
                for j in range(DC):
                    S.op("act", lambda q, j=j: q.activation(out=dst_fn(j), in_=xt[:, j, :], func=AF.Identity,
                                                             bias=shv[:, j:j + 1], scale=gsc[:, j:j + 1]),
                         r=(xtok, tMisc), w=(dst_tok,))

            def post_residual(yt, ytok, xt, xtok, gp):
                rstd_from(ps[6], tPs[6], D, RMS_EPS)
                for j in range(DC):
                    S.op("dve", lambda q, j=j: q.scalar_tensor_tensor(out=yt[:, j, :], in0=yt[:, j, :], scalar=gp[:, j:j + 1],
                                                                      in1=rstd_t[:], op0=ALU.mult, op1=ALU.mult),
                         r=(ytok, tRstd, tMisc), w=(ytok,))
                S.op("dve", lambda q: q.tensor_tensor(out=xt, in0=xt, in1=yt, op=ALU.add), r=(xtok, ytok), w=(xtok,))

            cc = tConst.chan
            S.op("dve", lambda q: q.memset(ones_f[:], 1.0), w=(tConst,))
            S.op("dve", lambda q: q.memset(ones_b[:], 1.0), w=(tConst,))
            S.op("dve", lambda q: q.memset(epsr[:], RMS_EPS), w=(tConst,))
            S.op("dve", lambda q: q.memset(epsl[:], LN_EPS), w=(tConst,))
            if kind == "F":
                cast_weights(0)
            if kind in ("M", "F"):
                S.dma("sp", cT[:], cT_in.ap(), w=(tConst,), chan=cc)
                S.dma("sp", adab[:], adab_in.ap(), w=(tConst,), chan=cc)
                S.op("act", lambda q: q.activation(out=cact[:], in_=cT[:], func=AF.Silu), r=(tConst,), w=(tMisc,))
                for l in range(LM):
                    for half in range(3):
                        buf, bt = (bufB0, tB0) if (l * 3 + half) % 2 == 0 else (bufB1, tB1)
                        S.dma("sp", buf[:], adaw_in.ap()[l, :, half * 512:(half + 1) * 512].rearrange("(k p) n -> p k n", p=128),
                              w=(bt,), chan=bt.chan)
                        for mm in range(4):
                            col = l * 12 + half * 4 + mm
                            for k in range(DC):
                                S.op("pe", lambda q, k=k, mm=mm, col=col, buf=buf: q.matmul(
                                    ps[0][:, col:col + 1], lhsT=buf[:, k, mm * 128:(mm + 1) * 128], rhs=cact[:, k:k + 1],
                                    start=(k == 0), stop=(k == DC - 1)), r=(bt, tMisc), w=(tPs[0],), inc=(k == DC - 1))
                S.op("dve", lambda q: q.tensor_tensor(out=modp[:], in0=ps[0][:, 0:LM * 12], in1=adab[:], op=ALU.add),
                     r=(tPs[0], tConst), w=(tMisc,))
                if kind == "M":
                    S.dma("sp", mod_out.ap(), modp[:], r=(tMisc,), w=(dOut,), chan=tMisc.chan)
                    S.barrier(full=True)
                    return
                S.dma("sp", mod_loc.ap()[:, 0:LM * 12], modp[:], r=(tMisc,), w=(dMl,), chan=tMisc.chan)
                S.collective("pool", "AllGather", [mod_loc.ap()], [mod_all.ap()], r=(dMl,), w=(dMa,), chan=chCC)
                S.dma("sp", modT[:], mod_all.ap()[:, 0:LM * 12].rearrange("(r p) n -> p r n", p=128), r=(dMa,), w=(tMisc,), chan=tMisc.chan)
                S.barrier()
            xsrc = [xT_in]
            if kind != "F":
                S.dma("sp", modT[:].rearrange("p r n -> p (r n)"), modT_in.ap(), w=(tMisc,), chan=tMisc.chan)
                S.dma("sp", vecs[:].rearrange("p a b c -> p (a b c)"), vecs_in.ap(), w=(tConst,), chan=cc)
            if kind in ("B", "F"):
                S.dma("sp", ident_f[:], ident_in.ap(), w=(tConst,), chan=cc)
                S.dma("pool", ident_b[:], ident_in.ap(), w=(tConst,), chan=cc)
                S.dma("sp", hsel[:], hsel_in.ap(), w=(tConst,), chan=cc)
                if kind == "B":
                    S.dma("sp", cvec[:].rearrange("p a b c -> p (a b c)"), cvec_in.ap(), w=(tConst,), chan=cc)
            if kind != "F":
                cast_weights(0)

            def ckpt(k):
                if stop == k:
                    S.barrier(full=True)
                    raise _Stop()

            ckpt(0)

            def mod_chunk(l, m):
                r_, mm = divmod(m, 12)
                if kind == "F":
                    return modT[:, r_, l * 12 + mm:l * 12 + mm + 1]
                return modT[:, r_, mm:mm + 1]

            def layer_vectors(l):
                for j in range(DC):
                    for (dst, gidx, scm, shdst, shm, gpd, gm_, pidx) in (
                            (gsc1, 0, 16 + j, sh1, 0 + j, gp1, 32 + j, 1),
                            (gsc2, 2, 64 + j, sh2, 48 + j, gp2, 80 + j, 3)):
                        S.op("dve", lambda q, dst=dst, gidx=gidx, scm=scm: q.scalar_tensor_tensor(
                            out=dst[:, j:j + 1], in0=mod_chunk(l, scm), scalar=1.0, in1=vecs[:, 0, gidx, j:j + 1],
                            op0=ALU.add, op1=ALU.mult), r=(tMisc, tConst), w=(tMisc,))
                        S.op("dve", lambda q, shdst=shdst, shm=shm: q.tensor_copy(out=shdst[:, j:j + 1], in_=mod_chunk(l, shm)),
                             r=(tMisc,), w=(tMisc,))
                        S.op("dve", lambda q, gpd=gpd, gm_=gm_, pidx=pidx: q.tensor_tensor(
                            out=gpd[:, j:j + 1], in0=mod_chunk(l, gm_), in1=vecs[:, 0, pidx, j:j + 1], op=ALU.mult),
                            r=(tMisc, tConst), w=(tMisc,))

            def proj_block(wv, wtok, jj, t, pbank, ptok):
                for k in range(DC):
                    S.op("pe", lambda q, k=k: q.matmul(pbank[:], lhsT=wv[:, k, jj * 128:(jj + 1) * 128],
                                                        rhs=bufA[:, k, t * 512:(t + 1) * 512],
                                                        start=(k == 0), stop=(k == DC - 1)),
                         r=(wtok, tA), w=(ptok,), inc=(k == DC - 1))


            def ph2(l):
                S.op("dve", lambda q: q.memset(smst[:], 0.0), w=(tSm,))
                S.op("dve", lambda q: q.memset(kms[:].rearrange("p a b -> p (a b)"), 0.0), w=(tKms,))

                for blk in range(4):
                    wv, wtok = load_wblock(wb_in[l], blk * 512, 512, DC, l)
                    for jj in range(4):
                        ch = blk * 4 + jj
                        st, stt = next_stage()
                        for t in range(NT):
                            pb, pt_ = next_ps(0, 6)
                            proj_block(wv, wtok, jj, t, pb, pt_)
                            if ch >= 8:
                                h = ch - 8
                                for b2 in range(2):
                                    S.op("act", lambda q, pb=pb, st=st, t=t, b2=b2, h=h: q.activation(
                                        out=st[:, t * 512 + b2 * 256:t * 512 + (b2 + 1) * 256], in_=pb[:, b2 * 256:(b2 + 1) * 256],
                                        func=AF.Identity, accum_out=kms[:, h, 2 * t + b2:2 * t + b2 + 1]), r=(pt_,), w=(stt, tKms))
                            else:
                                S.op("act", lambda q, pb=pb, st=st, t=t: q.activation(out=st[:, t * 512:(t + 1) * 512], in_=pb[:], func=AF.Identity),
                                     r=(pt_,), w=(stt,))
                        if os.environ.get("KSKIP_ST"):
                            pass
                        elif ch < 8:
                            S.dma("pool", qT_s.ap()[ch], st, r=(stt,), w=(dQ,), chan=stt.chan)
                        else:
                            S.dma("pool", kT_loc.ap()[(ch - 8) * 128:(ch - 7) * 128, :], st, r=(stt,), w=(dKl,), chan=stt.chan)
                S.op("dve", lambda q: q.tensor_copy(out=smst[:, 0:64], in_=kms[:].rearrange("p a b -> p (a b)")),
                     r=(tKms,), w=(tSm,))
                ckpt(10)
                for bv in range(2):
                    wv, wtok = load_wblock(wb_in[l], 2048 + bv * 512, 512, DC, l)
                    for tc4 in range(4):
                        st, stt = next_stage()
                        for tcc in range(4):
                            tc = tc4 * 4 + tcc
                            pb, pt_ = next_ps(0, 6)
                            for k in range(DC):
                                S.op("pe", lambda q, k=k, tc=tc: q.matmul(pb[:], lhsT=bufA[:, k, tc * 128:(tc + 1) * 128],
                                                                           rhs=wv[:, k, :], start=(k == 0), stop=(k == DC - 1)),
                                     r=(wtok, tA), w=(pt_,), inc=(k == DC - 1))
                            evac_copy(st[:, tcc * 512:(tcc + 1) * 512], pb[:], (pt_,), (stt,))
                        for hh in range(4):
                            dst = V_loc.ap().rearrange("(h c p) d -> p c h d", h=NH, p=128)[:, tc4 * 4:(tc4 + 1) * 4, bv * 4 + hh, :]
                            S.dma("pool", dst, st.rearrange("p (c h d) -> p c h d", c=4, h=4)[:, :, hh, :], r=(stt,), w=(dVl,), chan=stt.chan)
                ckpt(11)
                for bb in range(2):
                    wa, wat = load_wblock(wb_in[l], 3072 + bb * 512, 512, DC, l)
                    wg, wgt = load_wblock(wb_in[l], 4096 + bb * 512, 512, DC, l)
                    for jj in range(4):
                        i = bb * 4 + jj
                        st, stt = next_stage()
                        for t in range(NT):
                            pg, pgt = next_ps(0, 6)
                            proj_block(wg, wgt, jj, t, pg, pgt)
                            pa, pat = next_ps(0, 6)
                            proj_block(wa, wat, jj, t, pa, pat)
                            sg, sgt = tmp_t[t % 2], tTmp[t % 2]
                            S.op("act", lambda q, pg=pg, sg=sg: q.activation(out=sg[:], in_=pg[:], func=AF.Sigmoid), r=(pgt,), w=(sgt,))
                            S.op("dve", lambda q, pa=pa, sg=sg, st=st, t=t: q.tensor_tensor(
                                out=st[:, t * 512:(t + 1) * 512], in0=pa[:], in1=sg[:], op=ALU.mult), r=(pat, sgt), w=(stt,))
                            S.op("dve", lambda q, st=st, t=t, i=i: q.tensor_copy(
                                out=smst[:, 64 + (t * 8 + i) * HALO:64 + (t * 8 + i + 1) * HALO],
                                in_=st[:, t * 512 + 512 - HALO:(t + 1) * 512]), r=(stt,), w=(tSm,))
                        S.dma("pool", uT_s.ap()[i], st, r=(stt,), w=(dU,), chan=stt.chan)
                S.dma("pool", sm_loc.ap(), smst[:], r=(tSm,), w=(dSl,), chan=tSm.chan)

            for l in range(L):
                last = (l == L - 1)
                if kind == "F":
                    S.barrier()
                    S.dma("sp", vecs[:].rearrange("p a b c -> p (a b c)"), vecs_in.ap()[l], w=(tConst,), chan=cc)
                    S.dma("sp", cvec[:].rearrange("p a b c -> p (a b c)"), cvec_in.ap()[l], w=(tConst,), chan=cc)
                layer_vectors(l)
                S.barrier()
                for t in range(NT if kind in ("A", "F") else 0):
                    buf, bt = (bufB0, tB0) if t % 2 == 0 else (bufB1, tB1)
                    S.dma("sp", buf[:], xsrc[0].ap()[:, :, t * 512:(t + 1) * 512].rearrange("j p n -> p j n"),
                          r=(dX,), w=(bt,), chan=bt.chan)
                    prenorm(buf[:], bt, gsc1, sh1, lambda j, t=t: bufA[:, j, t * 512:(t + 1) * 512], tA)
                if debug and kind == "A":
                    S.dma("sp", dbg["hT"].ap().rearrange("j p n -> p j n"), bufA[:], r=(tA,), w=(dDbg,), chan=tA.chan)
                S.barrier()
                ckpt(1)
                if kind == "A":
                    ph2(l)
                    S.barrier(full=True)
                    return
                if kind == "F":
                    ph2(l)
                    S.collective("pool", "AllGather", [kT_loc.ap()], [kT_all.ap()], r=(dKl,), w=(dKa,), chan=chCC)
                    S.collective("pool", "AllGather", [V_loc.ap().rearrange("(a b) d -> a (b d)", b=8)],
                                 [V_all.ap().rearrange("(a b) d -> a (b d)", b=8)], r=(dVl,), w=(dVa,), chan=chCC)
                    S.collective("pool", "AllGather", [sm_loc.ap()], [sm_all.ap()], r=(dSl,), w=(dSa,), chan=chCC)
                    if l + 1 < L:
                        cast_weights(l + 1)
                S.barrier()
                ckpt(3)
                S.dma("sp", hall, sm_all.ap()[:, 64:1024].rearrange("(r p) n -> p r n", p=128), r=(dSa,), w=(tHall,), chan=tHall.chan)
                S.dma("sp", kmT[:], sm_all.ap()[:, 0:64].rearrange("(r p) n -> p r n", p=128), r=(dSa,), w=(tHall,), chan=tHall.chan)
                for t in range(NT):
                    first = True
                    for r_ in range(8):
                        tp = t if r_ < 7 else t - 1
                        if tp < 0:
                            continue
                        src = hall[:, r_, tp * 240:(tp + 1) * 240]
                        if first:
                            S.op("dve", lambda q, src=src, r_=r_: q.tensor_scalar(out=halo_sel[:, t, :], in0=src, scalar1=hsel[:, r_:r_ + 1],
                                                                               scalar2=None, op0=ALU.mult), r=(tHall, tConst), w=(tMisc,))
                            first = False
                        else:
                            S.op("dve", lambda q, src=src, r_=r_: q.scalar_tensor_tensor(out=halo_sel[:, t, :], in0=src, scalar=hsel[:, r_:r_ + 1],
                                                                                      in1=halo_sel[:, t, :], op0=ALU.mult, op1=ALU.add),
                                 r=(tHall, tConst, tMisc), w=(tMisc,))
                cT_all = [bufB0, bufB1]
                ui = 0
                for i in range(8):
                    for j in range(CONVK):
                        S.op("dve", lambda q, j=j, i=i: q.tensor_scalar(out=dwm[:, j, :], in0=ident_b[:], scalar1=cvec[:, 0, i, j:j + 1],
                                                                        scalar2=None, op0=ALU.mult), r=(tConst,), w=(tDw,))
                    for t in range(NT):
                        up, upt = upad[ui % 2], tUp[ui % 2]
                        ui += 1
                        S.dma("sp", up[:, HALO:HALO + 512], uT_s.ap()[i, :, t * 512:(t + 1) * 512], r=(dU,), w=(upt,), chan=upt.chan)
                        S.op("dve", lambda q, up=up, t=t, i=i: q.tensor_copy(out=up[:, 0:HALO], in_=halo_sel[:, t, i * HALO:(i + 1) * HALO]),
                             r=(tMisc,), w=(upt,))
                        pb, pt_ = next_ps(0, 6)
                        for j in range(CONVK):
                            S.op("pe", lambda q, j=j, up=up: q.matmul(pb[:], lhsT=dwm[:, j, :], rhs=up[:, j:j + 512],
                                                                       start=(j == 0), stop=(j == CONVK - 1)),
                                 r=(tDw, upt), w=(pt_,), inc=(j == CONVK - 1))
                        cb, cbt = (bufB0, tB0) if i < 4 else (bufB1, tB1)
                        S.op("act", lambda q, pb=pb, cb=cb, i=i, t=t: q.activation(out=cb[:, (i % 4) * 4 + t, :], in_=pb[:], func=AF.Identity,
                                                                               bias=cvec[:, 0, i, 31:32], scale=1.0), r=(pt_, tConst), w=(cbt,))
                for t in range(NT):
                    for i in range(8):
                        cb, cbt = (bufB0, tB0) if i < 4 else (bufB1, tB1)
                        src = cb[:, (i % 4) * 4 + t, :]
                        S.op("pe", lambda q, src=src, i=i: q.matmul(ps[5][:], lhsT=ones_f[:], rhs=src, start=(i == 0), stop=(i == 7)),
                             r=(cbt, tConst), w=(tPs[5],), inc=(i == 7))
                        sumsq_accum(src, cbt, i == 0, i == 7, ps[6], tPs[6])
                    S.op("dve", lambda q: q.tensor_scalar(out=mean_t[:], in0=ps[5][:], scalar1=1.0 / 1024, scalar2=None, op0=ALU.mult),
                         r=(tPs[5],), w=(tMean,))
                    S.op("dve", lambda q: q.tensor_tensor(out=tmp_t[0][:], in0=mean_t[:], in1=mean_t[:], op=ALU.mult), r=(tMean,), w=(tTmp[0],))
                    S.op("dve", lambda q: q.scalar_tensor_tensor(out=rstd_t[:], in0=ps[6][:], scalar=1.0 / 1024, in1=tmp_t[0][:],
                                                                  op0=ALU.mult, op1=ALU.subtract), r=(tPs[6], tTmp[0]), w=(tRstd,))
                    S.op("act", lambda q: q.activation(out=rstd_t[:], in_=rstd_t[:], func=AF.Sqrt, bias=epsl[:, 0:1], scale=1.0),
                         r=(tRstd, tConst), w=(tRstd,))
                    S.op("dve", lambda q: q.reciprocal(out=rstd_t[:], in_=rstd_t[:]), r=(tRstd,), w=(tRstd,))
                    for i in range(8):
                        cb, cbt = (bufB0, tB0) if i < 4 else (bufB1, tB1)
                        src = cb[:, (i % 4) * 4 + t, :]
                        S.op("dve", lambda q, src=src: q.tensor_tensor(out=src, in0=src, in1=mean_t[:], op=ALU.subtract), r=(cbt, tMean), w=(cbt,))
                        S.op("dve", lambda q, src=src: q.tensor_tensor(out=src, in0=src, in1=rstd_t[:], op=ALU.mult), r=(cbt, tRstd), w=(cbt,))
                        S.op("act", lambda q, src=src, i=i, t=t: q.activation(out=bufA[:, 8 + i, t * 512:(t + 1) * 512], in_=src, func=AF.Silu,
                                                                              bias=cvec[:, 0, i, 33:34], scale=cvec[:, 0, i, 32:33]),
                             r=(cbt, tConst), w=(tA,))
                S.barrier()
                ckpt(4)
                NSL = 16
                S.dma("sp", emat.rearrange("p a b -> p (a b)"), emat_in.ap(), w=(tAtc,), chan=tAtc.chan)
                S.dma("sp", cmask.rearrange("p a b -> p (a b)"), cmask_in.ap(), w=(tAtc,), chan=tAtc.chan)
                S.dma("sp", gmask.rearrange("p a b -> p (a b)"), gmask_in.ap(), w=(tAtc,), chan=tAtc.chan)
                kring = [wring[0][:, s * 512:(s + 1) * 512] for s in range(NSL)]
                vring = [wring[1][:, s * 512:(s + 1) * 512].rearrange("p (k d) -> p k d", k=4) for s in range(NSL)]
                tKV = [tk(f"kv{s}", True) for s in range(NSL)]
                kvrot = 0
                prot = 0
                nmrot = 0
                srot = 0
                for h in range(NH):
                    qt, qtt = qTh[h % 2], tQ[h % 2]
                    S.dma("sp", qt, qT_s.ap()[h], r=(dQ,), w=(qtt,), chan=qtt.chan)
                    S.op("dve", lambda q, h=h: q.tensor_copy(out=kmh.rearrange("p (r b) -> p r b", r=8), in_=kmT[:, :, h * 8:(h + 1) * 8]),
                         r=(tHall,), w=(tGate,))
                    for t in range(NT):
                        for c_ in range(4):
                            S.op("pe", lambda q, c_=c_: q.matmul(ps[5][:, c_ * 64:(c_ + 1) * 64], lhsT=qt[:, t * 512 + c_ * 128:t * 512 + (c_ + 1) * 128],
                                                                  rhs=kmh, start=True, stop=True), r=(qtt, tGate), w=(tPs[5],), inc=(c_ == 3))
                        S.op("dve", lambda q: q.tensor_tensor(out=gm.rearrange("p a b -> p (a b)"), in0=ps[5][:, 0:256], in1=gmask[:, t, :], op=ALU.add),
                             r=(tPs[5], tAtc), w=(tGate,))
                        for c_ in range(4):
                            S.op("dve", lambda q, c_=c_: q.max(out=max8[:, c_, :], in_=gm[:, c_, :]), r=(tGate,), w=(tGate,))
                        S.op("dve", lambda q: q.tensor_scalar(out=thr, in0=max8[:, :, 2], scalar1=-1e29, scalar2=None, op0=ALU.max), r=(tGate,), w=(tGate,))
                        for c_ in range(4):
                            S.op("dve", lambda q, c_=c_: q.tensor_scalar(out=nm[:, c_, :], in0=gm[:, c_, :], scalar1=thr[:, c_:c_ + 1], scalar2=1.0,
                                                                         op0=ALU.is_ge, op1=ALU.subtract), r=(tGate,), w=(tGate,))
                        for c_ in range(4):
                            S.op("pe", lambda q, c_=c_: q.transpose(out=psb[:, c_ * 128:(c_ + 1) * 128], in_=nm[:, c_, :], identity=ident_b[:]),
                                 r=(tGate, tConst), w=(tPsb,), inc=(c_ == 3))
                        nmt, nmtt = nmT[nmrot % 2], tNmT[nmrot % 2]
                        nmrot += 1
                        S.op("act", lambda q, nmt=nmt: q.activation(out=nmt, in_=psb[:], func=AF.Identity), r=(tPsb,), w=(nmtt,))
                        visits = []
                        S.dma("sp", kown, kT_loc.ap()[h * 128:(h + 1) * 128, t * 512:(t + 1) * 512], r=(dKl,), w=(tOwn,), chan=tOwn.chan)
                        S.dma("sp", vown, V_loc.ap()[h * TOK + t * 512:h * TOK + (t + 1) * 512, :].rearrange("(k p) d -> p k d", p=128),
                              r=(dVl,), w=(tOwn,), chan=tOwn.chan)
                        for kt in range(4):
                            visits.append(("own", kown[:, kt * 128:(kt + 1) * 128], vown[:, kt, :], tOwn, ident_b[:], cmask[:, kt, :], (tConst, tAtc)))
                        for tp in range(t + 1):
                            for r_ in range(NCORES):
                                s = kvrot % NSL
                                kvrot += 1
                                visits.append(("load", s, r_, tp))
                                for kt in range(4):
                                    jblk = r_ * 8 + 2 * tp + kt // 2
                                    visits.append(("past", kring[s][:, kt * 128:(kt + 1) * 128], vring[s][:, kt, :], tKV[s],
                                                   emat[:, jblk, :], nmt, (tAtc, nmtt)))
                        comp = [v for v in visits if v[0] != "load"]
                        nvis = len(comp)
                        pend = []
                        ci = 0

                        def emit_pv(item, idx):
                            (vv, pt_i) = item
                            S.op("pe", lambda q: q.matmul(ps[3][:], lhsT=vv[2], rhs=pT[pt_i], start=(idx == 0), stop=(idx == nvis - 1)),
                                 r=(vv[3], tPT[pt_i]), w=(tPs[3],), inc=False)
                            S.op("pe", lambda q: q.matmul(ps[4][:], lhsT=ones_b[:], rhs=pT[pt_i], start=(idx == 0), stop=(idx == nvis - 1)),
                                 r=(tPT[pt_i], tConst), w=(tPs[4],), inc=True)

                        done = 0
                        for v in visits:
                            if v[0] == "load":
                                _, s, r_, tp = v
                                S.dma("sp", kring[s], kT_all.ap()[(r_ * NH + h) * 128:(r_ * NH + h + 1) * 128, tp * 512:(tp + 1) * 512],
                                      r=(dKa,), w=(tKV[s],), chan=tKV[s].chan)
                                base = (r_ * NH + h) * TOK + tp * 512
                                S.dma("sp", vring[s], V_all.ap()[base:base + 512, :].rearrange("(k p) d -> p k d", p=128),
                                      r=(dVa,), w=(tKV[s],), chan=tKV[s].chan)
                                continue
                            sb_i = srot % 3
                            srot += 1
                            pss, psst = ps[sb_i], tPs[sb_i]
                            S.op("pe", lambda q, v=v, pss=pss: q.matmul(pss[:], lhsT=v[1], rhs=qt[:, t * 512:(t + 1) * 512], start=True, stop=False),
                                 r=(v[3], qtt), w=(psst,), inc=False)
                            S.op("pe", lambda q, v=v, pss=pss: q.matmul(pss[:], lhsT=v[4], rhs=v[5], start=False, stop=True),
                                 r=v[6], w=(psst,), inc=True)
                            pi = prot % 4
                            prot += 1
                            S.op("act", lambda q, pss=pss, pi=pi: q.activation(out=pT[pi], in_=pss[:], func=AF.Exp, scale=SCALE),
                                 r=(psst,), w=(tPT[pi],))
                            pend.append((v, pi))
                            if len(pend) > 2:
                                emit_pv(pend.pop(0), done)
                                done += 1
                        while pend:
                            emit_pv(pend.pop(0), done)
                            done += 1
                        S.op("dve", lambda q: q.reciprocal(out=tmp_t[0][:], in_=ps[4][:]), r=(tPs[4],), w=(tTmp[0],))
                        S.op("dve", lambda q, h=h, t=t: q.tensor_tensor(out=bufA[:, h, t * 512:(t + 1) * 512], in0=ps[3][:], in1=tmp_t[0][:], op=ALU.mult),
                             r=(tPs[3], tTmp[0]), w=(tA,))
                if debug:
                    S.dma("sp", dbg["mixT"].ap().rearrange("j p n -> p j n"), bufA[:], r=(tA,), w=(dDbg,), chan=tA.chan)
                S.barrier()
                ckpt(5)
                for t in range(NT):
                    S.dma("sp", bufB1[:], xsrc[0].ap()[:, :, t * 512:(t + 1) * 512].rearrange("j p n -> p j n"), r=(dX,), w=(tB1,), chan=tB1.chan)
                    for blk in range(4):
                        wv, wtok = load_wblock(wb_out[l], blk * 512, 512, DC, l)
                        for jj in range(4):
                            j = blk * 4 + jj
                            pb, pt_ = next_ps(0, 6)
                            for k in range(DC):
                                S.op("pe", lambda q, k=k, jj=jj: q.matmul(pb[:], lhsT=wv[:, k, jj * 128:(jj + 1) * 128], rhs=bufA[:, k, t * 512:(t + 1) * 512],
                                                                          start=(k == 0), stop=(k == DC - 1)), r=(wtok, tA), w=(pt_,), inc=(k == DC - 1))
                            S.op("act", lambda q, pb=pb, j=j: q.activation(out=bufB0[:, j, :], in_=pb[:], func=AF.Identity), r=(pt_,), w=(tB0,))
                            sumsq_accum(bufB0[:, j, :], tB0, j == 0, j == DC - 1, ps[6], tPs[6])
                    post_residual(bufB0[:], tB0, bufB1[:], tB1, gp1)
                    S.dma("pool", xT_s.ap()[:, :, t * 512:(t + 1) * 512].rearrange("j p n -> p j n"), bufB1[:], r=(tB1,), w=(dX,), chan=tB1.chan)
                    h2v = bufB0[:].rearrange("p j n -> p (j n)").bitcast(BF16)[:, 0:DC * 512].rearrange("p (j n) -> p j n", j=DC)
                    prenorm(bufB1[:], tB1, gsc2, sh2, lambda j: h2v[:, j, :], tB0)
                    S.dma("pool", h2T_s.ap()[:, :, t * 512:(t + 1) * 512].rearrange("j p n -> p j n"), h2v, r=(tB0,), w=(dH2,), chan=tB0.chan)
                xsrc[0] = xT_s
                S.barrier()
                ckpt(6)
                S.dma("sp", bufA[:], h2T_s.ap().rearrange("j p n -> p j n"), r=(dH2,), w=(tA,), chan=tA.chan)
                for fb in range(FC // 4):
                    wg, wgt = load_wblock(wb_gate[l], fb * 512, 512, DC, l)
                    wu, wut = load_wblock(wb_up[l], fb * 512, 512, DC, l)
                    for jj in range(4):
                        f = fb * 4 + jj
                        st, stt = next_stage()
                        for t in range(NT):
                            pg, pgt = next_ps(0, 6)
                            proj_block(wg, wgt, jj, t, pg, pgt)
                            pu, put = next_ps(0, 6)
                            proj_block(wu, wut, jj, t, pu, put)
                            sg, sgt = tmp_t[t % 2], tTmp[t % 2]
                            S.op("act", lambda q, pg=pg, sg=sg: q.activation(out=sg[:], in_=pg[:], func=AF.Silu), r=(pgt,), w=(sgt,))
                            S.op("dve", lambda q, pu=pu, sg=sg, st=st, t=t: q.tensor_tensor(out=st[:, t * 512:(t + 1) * 512], in0=pu[:], in1=sg[:], op=ALU.mult),
                                 r=(put, sgt), w=(stt,))
                        S.dma("pool", actT_s.ap()[f], st, r=(stt,), w=(dAct,), chan=stt.chan)
                S.barrier()
                ckpt(7)
                actv = bufA[:].rearrange("p j n -> p (j n)")[:, 0:FC * 512].rearrange("p (f n) -> p f n", f=FC)
                for t in range(NT):
                    S.dma("sp", actv, actT_s.ap()[:, :, t * 512:(t + 1) * 512].rearrange("f p n -> p f n"), r=(dAct,), w=(tA,), chan=tA.chan)
                    S.dma("sp", bufB1[:], xT_s.ap()[:, :, t * 512:(t + 1) * 512].rearrange("j p n -> p j n"), r=(dX,), w=(tB1,), chan=tB1.chan)
                    for j in range(DC):
                        wi_ = wslot[0] % 3
                        wslot[0] += 1
                        S.dma("sp", wring[wi_][:, 0:FC * 128], wb_down[l].ap()[j], r=(dWb[l],), w=(tW[wi_],), chan=tW[wi_].chan)
                        wv, wtok = wring[wi_][:, 0:FC * 128].rearrange("p (k n) -> p k n", k=FC), tW[wi_]
                        pb, pt_ = next_ps(0, 6)
                        for k in range(FC):
                            S.op("pe", lambda q, k=k: q.matmul(pb[:], lhsT=wv[:, k, :], rhs=actv[:, k, :], start=(k == 0), stop=(k == FC - 1)),
                                 r=(wtok, tA), w=(pt_,), inc=(k == FC - 1))
                        S.op("act", lambda q, pb=pb, j=j: q.activation(out=bufB0[:, j, :], in_=pb[:], func=AF.Identity), r=(pt_,), w=(tB0,))
                        sumsq_accum(bufB0[:, j, :], tB0, j == 0, j == DC - 1, ps[6], tPs[6])
                    post_residual(bufB0[:], tB0, bufB1[:], tB1, gp2)
                    dst = outT if (kind == "B" or last) else xT_s
                    S.dma("pool", dst.ap()[:, :, t * 512:(t + 1) * 512].rearrange("j p n -> p j n"), bufB1[:], r=(tB1,),
                          w=(dOut if (kind == "B" or last) else dX,), chan=tB1.chan)
                S.barrier()
            S.barrier(full=True)

        def run_pass(live, q):
            S = Sched(nc, es, sems)
            qs = {n: (q if n == live else _DummyEng()) for n in ("pe", "act", "dve", "pool", "sp")}
            try:
                program(S, qs["pe"], qs["act"], qs["dve"], qs["pool"], qs["sp"])
            except _Stop:
                pass

        @block.tensor
        def _(q):
            run_pass("pe", q)

        @block.scalar
        def _(q):
            run_pass("act", q)

        @block.vector
        def _(q):
            run_pass("dve", q)

        @block.gpsimd
        def _(q):
            run_pass("pool", q)

        @block.sync
        def _(q):
            run_pass("sp", q)
    return nc


def _consts(core):
    ident = np.eye(128, dtype=np.float32)
    emat = np.zeros((64, 64, 128), np.float32)
    for n in range(64):
        emat[n, n, :] = MASKV
    k = np.arange(128)[:, None]
    q = np.arange(128)[None, :]
    tri = np.where(k <= q, 0.0, -MASKV).astype(np.float32)
    full = np.zeros((128, 128), np.float32)
    ninf = np.full((128, 128), -MASKV, np.float32)
    cm = np.stack([
        np.concatenate([tri, full, ninf, ninf], 1),
        np.concatenate([ninf, tri, ninf, ninf], 1),
        np.concatenate([ninf, ninf, tri, full], 1),
        np.concatenate([ninf, ninf, ninf, tri], 1)], 0)
    cmask = np.ascontiguousarray(cm.transpose(1, 0, 2)).reshape(128, 4 * 512)
    jj = np.arange(64)
    r_ = jj // 8
    lb = jj % 8
    nglob = 2 * (8 * (lb // 2) + r_) + (lb % 2)
    gmask = np.zeros((NT, 4, 64), np.float32)
    for t in range(NT):
        g = 8 * t + core
        for c_ in range(4):
            blk = 2 * g + (1 if c_ >= 2 else 0)
            gmask[t, c_, :] = np.where(nglob < blk, 0.0, NEG)
    gmask = np.broadcast_to(gmask.reshape(1, NT * 256), (128, NT * 256)).copy()
    hs = np.zeros((8,), np.float32)
    if core == 0:
        hs[7] = 1.0
    else:
        hs[core - 1] = 1.0
    hsel = np.broadcast_to(hs[None, :], (128, 8)).copy()
    return ident, emat.reshape(64, 64 * 128).astype(ml_dtypes.bfloat16), cmask.astype(ml_dtypes.bfloat16), gmask, hsel


def _fm(v):
    sh = v.shape
    v = v.reshape(sh[:-1] + (sh[-1] // 128, 128))
    return np.ascontiguousarray(np.moveaxis(v, -1, 0))


_CACHE = {}


def _prog(kind, debug=False, stop=None):
    key = (kind, debug, stop)
    if key not in _CACHE:
        _CACHE[key] = build_program(kind, debug, stop)
    return _CACHE[key]


def _kernel_unfused(x, c, ada_w, ada_b, mix_pre_g, mix_post_g, w_in, conv_w, conv_b, conv_ln_g, conv_ln_b, w_out,
                    ffn_pre_g, ffn_post_g, w_gate, w_up, w_down, _depth=L_FULL, _debug=False, _stop=None):
    L = _depth
    cores = list(range(NCORES))
    f32 = lambda a: np.ascontiguousarray(np.asarray(a, dtype=np.float32))
    x = f32(x).reshape(SEQ, D)
    ada_w = f32(ada_w)
    ada_b = f32(ada_b)
    dbg = {}
    cT = _fm(f32(c).reshape(D))
    in_maps = []
    for core in cores:
        aw = np.ascontiguousarray(ada_w[:, :, core * 1536:(core + 1) * 1536])
        ab = ada_b[:, core * 1536:(core + 1) * 1536].reshape(L_FULL, 12, 128)
        ab = np.ascontiguousarray(ab.transpose(2, 0, 1)).reshape(128, L_FULL * 12)
        in_maps.append({"cT": cT, "adaw": aw, "adab": ab})
    res = run_bass_kernel_spmd(_prog("M"), in_maps, core_ids=cores)
    modall = np.stack([np.asarray(res.results[r]["mod_out"]).reshape(128, L_FULL, 12) for r in cores], axis=1)
    xt = x.reshape(NT, NCORES, 512, D)
    xTs = [np.ascontiguousarray(xt[:, core].reshape(TOK, D).T).reshape(DC, 128, TOK) for core in cores]
    consts = [_consts(core) for core in cores]
    for l in range(L):
        modT = np.ascontiguousarray(modall[:, :, l, :]).reshape(128, NCORES * 12)
        vecs = np.stack([_fm(f32(v)[l]) for v in (mix_pre_g, mix_post_g, ffn_pre_g, ffn_post_g)], axis=1)
        vecs = np.ascontiguousarray(vecs).reshape(128, 4 * DC)
        cv = np.concatenate([f32(conv_w)[l], f32(conv_b)[l][None], f32(conv_ln_g)[l][None], f32(conv_ln_b)[l][None]], axis=0)
        cvec = np.ascontiguousarray(cv.reshape(34, 8, 128).transpose(2, 1, 0)).reshape(128, 8 * 34)
        wi = f32(w_in[l])
        in_maps = [{"xT_in": xTs[core], "modT": modT, "vecs": vecs, "w_in": wi} for core in cores]
        resA = run_bass_kernel_spmd(_prog("A", _debug), in_maps, core_ids=cores).results
        if _debug:
            dbg[("A", l)] = resA
        kT_all = np.concatenate([np.asarray(resA[r]["kT_loc"]) for r in cores], axis=0)
        V_all = np.concatenate([np.asarray(resA[r]["V_loc"]) for r in cores], axis=0)
        sm_all = np.concatenate([np.asarray(resA[r]["sm_loc"]) for r in cores], axis=0)
        wo, wg, wu, wd = f32(w_out[l]), f32(w_gate[l]), f32(w_up[l]), f32(w_down[l])
        in_maps = []
        for core in cores:
            ident, emat, cmask, gmask, hsel = consts[core]
            in_maps.append({"xT_in": xTs[core], "modT": modT, "vecs": vecs, "cvec": cvec,
                            "w_out": wo, "w_gate": wg, "w_up": wu, "w_down": wd,
                            "ident": ident, "emat": emat, "cmask": cmask, "gmask": gmask, "hsel": hsel,
                            "qT_s": np.asarray(resA[core]["qT_s"]), "kT_loc": np.asarray(resA[core]["kT_loc"]),
                            "V_loc": np.asarray(resA[core]["V_loc"]), "kT_all": kT_all, "V_all": V_all, "sm_all": sm_all,
                            "uT_s": np.asarray(resA[core]["uT_s"])})
        resB = run_bass_kernel_spmd(_prog("B", _debug, _stop), in_maps, core_ids=cores).results
        if _debug:
            dbg[("B", l)] = resB
        xTs = [np.asarray(resB[core]["outT"]).reshape(DC, 128, TOK) for core in cores]
    out = np.empty((NT, NCORES, 512, D), np.float32)
    for core in cores:
        out[:, core] = xTs[core].reshape(D, TOK).T.reshape(NT, 512, D)
    if _debug:
        _kernel_unfused._last = dbg
        _kernel_unfused._mod = modall
    return out.reshape(1, SEQ, D)


def _kernel_fused(x, c, ada_w, ada_b, mix_pre_g, mix_post_g, w_in, conv_w, conv_b, conv_ln_g, conv_ln_b, w_out,
                  ffn_pre_g, ffn_post_g, w_gate, w_up, w_down):
    L = L_FULL
    cores = list(range(NCORES))
    f32 = lambda a: np.ascontiguousarray(np.asarray(a, dtype=np.float32))
    x = f32(x).reshape(SEQ, D)
    ada_w = f32(ada_w)
    ada_b = f32(ada_b)
    cT = _fm(f32(c).reshape(D))
    vecs = np.stack([np.stack([_fm(f32(v)[l]) for v in (mix_pre_g, mix_post_g, ffn_pre_g, ffn_post_g)], axis=1).reshape(128, 4 * DC)
                     for l in range(L)], axis=0)
    cvs = []
    for l in range(L):
        cv = np.concatenate([f32(conv_w)[l], f32(conv_b)[l][None], f32(conv_ln_g)[l][None], f32(conv_ln_b)[l][None]], axis=0)
        cvs.append(np.ascontiguousarray(cv.reshape(34, 8, 128).transpose(2, 1, 0)).reshape(128, 8 * 34))
    cvec = np.stack(cvs, axis=0)
    shared = {"cT": cT, "vecs": np.ascontiguousarray(vecs), "cvec": np.ascontiguousarray(cvec),
              "w_in": f32(w_in), "w_out": f32(w_out), "w_gate": f32(w_gate), "w_up": f32(w_up), "w_down": f32(w_down)}
    xt = x.reshape(NT, NCORES, 512, D)
    in_maps = []
    for core in cores:
        ident, emat, cmask, gmask, hsel = _consts(core)
        xT = np.ascontiguousarray(xt[:, core].reshape(TOK, D).T).reshape(DC, 128, TOK)
        aw = np.ascontiguousarray(ada_w[:, :, core * 1536:(core + 1) * 1536])
        ab = ada_b[:, core * 1536:(core + 1) * 1536].reshape(L, 12, 128)
        ab = np.ascontiguousarray(ab.transpose(2, 0, 1)).reshape(128, L * 12)
        m = dict(shared)
        m.update({"xT_in": xT, "adaw": aw, "adab": ab, "ident": ident, "emat": emat, "cmask": cmask, "gmask": gmask, "hsel": hsel})
        in_maps.append(m)
    res = run_bass_kernel_spmd(_prog("F"), in_maps, core_ids=cores)
    out = np.empty((NT, NCORES, 512, D), np.float32)
    for core in cores:
        oT = np.asarray(res.results[core]["outT"]).reshape(D, TOK)
        out[:, core] = oT.T.reshape(NT, 512, D)
    return out.reshape(1, SEQ, D)


def kernel(x, c, ada_w, ada_b, mix_pre_g, mix_post_g, w_in, conv_w, conv_b, conv_ln_g, conv_ln_b, w_out,
           ffn_pre_g, ffn_post_g, w_gate, w_up, w_down):
    return _kernel_unfused(x, c, ada_w, ada_b, mix_pre_g, mix_post_g, w_in, conv_w, conv_b, conv_ln_g, conv_ln_b, w_out,
                           ffn_pre_g, ffn_post_g, w_gate, w_up, w_down)
```
